# Optimizing a Trainium2 kernel written in Bass

```python
import math
import jax, jax.numpy as jnp
from jax import lax
import numpy as np

D_MODEL = 1024
BATCH = 4
SEQ = 8192
DEPTH = 1

N_Q_HEADS = 8
N_KV_HEADS = 2
GROUP = N_Q_HEADS // N_KV_HEADS
HEAD_DIM = 64
ROT_DIM = HEAD_DIM // 4
ROPE_THETA = 500000.0
WINDOW = 128
ATTN_BLOCK = 128
ATTN_WIDTH = N_Q_HEADS * HEAD_DIM
KV_WIDTH = N_KV_HEADS * HEAD_DIM

HGRN_HEADS = 4
HGRN_KEY_DIM = 128
HGRN_VAL_DIM = 128
HGRN_KEY_WIDTH = HGRN_HEADS * HGRN_KEY_DIM
HGRN_WIDTH = HGRN_HEADS * HGRN_VAL_DIM
HGRN_CHUNK = 64

IN_SIZES = (ATTN_WIDTH, KV_WIDTH, KV_WIDTH,
            HGRN_KEY_WIDTH, HGRN_KEY_WIDTH, HGRN_WIDTH, HGRN_WIDTH,
            D_MODEL, D_MODEL)
IN_WIDTH = sum(IN_SIZES)

N_GROUPS = 4
EXPERTS_PER_GROUP = 8
N_EXPERTS = N_GROUPS * EXPERTS_PER_GROUP
TOP_K = 2
EXPERT_FF = 512
MOE_BLOCK = 128

NORM_EPS = 1e-6

kernel_name = "hybrid_swa_hgrn2_hmoe_block"


def rms_norm(x, w):
    xf = x.astype(jnp.float32)
    y = xf * lax.rsqrt(jnp.mean(xf * xf, axis=-1, keepdims=True) + NORM_EPS)
    return (y * w.astype(jnp.float32)).astype(x.dtype)


def rope_tables(positions):
    inv_freq = ROPE_THETA ** (-jnp.arange(0, ROT_DIM, 2, dtype=jnp.float32) / ROT_DIM)
    ang = positions.astype(jnp.float32)[..., None] * inv_freq
    return jnp.cos(ang)[:, :, None, :], jnp.sin(ang)[:, :, None, :]


def partial_rope(x, cos, sin):
    half = ROT_DIM // 2
    xf = x.astype(jnp.float32)
    x1, x2 = xf[..., :half], xf[..., half:ROT_DIM]
    out = jnp.concatenate([x1 * cos - x2 * sin, x2 * cos + x1 * sin, xf[..., ROT_DIM:]], axis=-1)
    return out.astype(x.dtype)


def sliding_window_gqa(q, k, v, sinks):
    B, S = q.shape[0], q.shape[1]
    nb = S // ATTN_BLOCK
    qb = q.reshape(B, nb, ATTN_BLOCK, N_KV_HEADS, GROUP, HEAD_DIM).astype(jnp.float32)
    kb = k.reshape(B, nb, ATTN_BLOCK, N_KV_HEADS, HEAD_DIM).astype(jnp.float32)
    vb = v.reshape(B, nb, ATTN_BLOCK, N_KV_HEADS, HEAD_DIM).astype(jnp.float32)
    pad = ((0, 0), (1, 0), (0, 0), (0, 0), (0, 0))
    kk = jnp.concatenate([jnp.pad(kb[:, :-1], pad), kb], axis=2)
    vv = jnp.concatenate([jnp.pad(vb[:, :-1], pad), vb], axis=2)
    s = jnp.einsum('bnqhgd,bnkhd->bnhgqk', qb, kk) * (HEAD_DIM ** -0.5)
    qi = jnp.arange(ATTN_BLOCK)[:, None] + ATTN_BLOCK
    kj = jnp.arange(2 * ATTN_BLOCK)[None, :]
    band = (kj <= qi) & (qi - kj < WINDOW)
    has_prev = (jnp.arange(nb)[:, None, None] > 0) | (kj[None] >= ATTN_BLOCK)
    valid = band[None] & has_prev
    s = jnp.where(valid[None, :, None, None], s, -jnp.inf)
    sink = sinks.astype(jnp.float32).reshape(N_KV_HEADS, GROUP)[None, None, :, :, None, None]
    m = jnp.maximum(jnp.max(s, axis=-1, keepdims=True), sink)
    p = jnp.exp(s - m)
    denom = jnp.sum(p, axis=-1, keepdims=True) + jnp.exp(sink - m)
    o = jnp.einsum('bnhgqk,bnkhd->bnqhgd', p / denom, vv)
    return o.reshape(B, S, ATTN_WIDTH).astype(q.dtype)


def hgrn2_chunkwise(q, k, v, log_f):
    B, S = q.shape[0], q.shape[1]
    nc = S // HGRN_CHUNK

    def to_chunks(t):
        return t.reshape(B, nc, HGRN_CHUNK, HGRN_HEADS, t.shape[-1]).transpose(1, 0, 3, 2, 4)

    causal = jnp.tril(jnp.ones((HGRN_CHUNK, HGRN_CHUNK), dtype=bool))

    def step(state, inp):
        qc, kc, vc, gc = inp
        b = jnp.cumsum(gc, axis=2)
        o_inter = jnp.einsum('bhtk,bhkv->bhtv', qc * jnp.exp(b), state)
        diff = b[:, :, :, None, :] - b[:, :, None, :, :]
        decay = jnp.exp(jnp.where(causal[:, :, None], diff, -jnp.inf))
        a = jnp.einsum('bhtk,bhsk,bhtsk->bhts', qc, kc, decay)
        o_intra = jnp.einsum('bhts,bhsv->bhtv', a, vc)
        b_last = b[:, :, -1:, :]
        state = jnp.exp(b_last[:, :, 0])[..., None] * state + jnp.einsum(
            'bhsk,bhsv->bhkv', kc * jnp.exp(b_last - b), vc)
        return state, o_inter + o_intra

    init = jnp.zeros((B, HGRN_HEADS, HGRN_KEY_DIM, HGRN_VAL_DIM), jnp.float32)
    _, o = lax.scan(step, init, (to_chunks(q), to_chunks(k), to_chunks(v), to_chunks(log_f)))
    return o.transpose(1, 0, 3, 2, 4).reshape(B, S, HGRN_HEADS, HGRN_VAL_DIM)


def hierarchical_moe(xn, w_rg, b_rg, w_re, b_re, w_gate, w_up, w_down):
    T, D = xn.shape
    lg = (xn @ w_rg).astype(jnp.float32) + b_rg.astype(jnp.float32)
    pg = jax.nn.softmax(lg, axis=-1)
    gsel = jnp.argmax(pg, axis=-1)
    pgsel = jnp.take_along_axis(pg, gsel[:, None], axis=1)[:, 0]
    le = ((xn @ w_re).astype(jnp.float32) + b_re.astype(jnp.float32)).reshape(T, N_GROUPS, EXPERTS_PER_GROUP)
    le_sel = jnp.take_along_axis(le, gsel[:, None, None], axis=1)[:, 0]
    pe = jax.nn.softmax(le_sel, axis=-1)
    topv, topi = lax.top_k(pe, TOP_K)
    gate_w = pgsel[:, None] * topv / jnp.sum(topv, axis=-1, keepdims=True)
    eid = gsel[:, None] * EXPERTS_PER_GROUP + topi

    TK = T * TOP_K
    flat_e = eid.reshape(-1).astype(jnp.int32)
    flat_tok = jnp.repeat(jnp.arange(T, dtype=jnp.int32), TOP_K)
    flat_w = gate_w.reshape(-1)
    order = jnp.argsort(flat_e)
    se = flat_e[order]
    counts = jnp.bincount(flat_e, length=N_EXPERTS)
    starts = jnp.cumsum(counts) - counts
    padded = ((counts + MOE_BLOCK - 1) // MOE_BLOCK) * MOE_BLOCK
    pends = jnp.cumsum(padded)
    pstarts = pends - padded
    dest = pstarts[se] + jnp.arange(TK, dtype=jnp.int32) - starts[se]
    P = TK + N_EXPERTS * MOE_BLOCK
    P = ((P + MOE_BLOCK - 1) // MOE_BLOCK) * MOE_BLOCK
    nblk = P // MOE_BLOCK
    row_tok = jnp.full((P,), T, jnp.int32).at[dest].set(flat_tok[order])
    row_w = jnp.zeros((P,), jnp.float32).at[dest].set(flat_w[order])
    blk_e = jnp.minimum(jnp.searchsorted(pends, jnp.arange(nblk, dtype=jnp.int32) * MOE_BLOCK, side='right'),
                        N_EXPERTS - 1)
    x_pad = jnp.concatenate([xn, jnp.zeros((1, D), xn.dtype)], axis=0)
    x_blocks = x_pad[row_tok].reshape(nblk, MOE_BLOCK, D)

    def expert_block(args):
        xb, e = args
        h = jax.nn.silu(xb @ w_gate[e]) * (xb @ w_up[e])
        return h @ w_down[e]

    y_blocks = lax.map(expert_block, (x_blocks, blk_e))
    y_rows = y_blocks.reshape(P, D) * row_w[:, None].astype(y_blocks.dtype)
    return jax.ops.segment_sum(y_rows, row_tok, num_segments=T + 1)[:T]


def setup_inputs(seed: int = 0) -> dict:
    key = jax.random.key(seed)
    ks = jax.random.split(key, 24)
    f32 = jnp.float32
    nrm = lambda k, shape, scale: jax.random.normal(k, shape, f32) * scale
    x = jax.random.normal(ks[0], (BATCH, SEQ, D_MODEL), f32)
    offs = jax.random.randint(ks[1], (BATCH, 1), 0, 4096, dtype=jnp.int32)
    positions = offs + jnp.arange(SEQ, dtype=jnp.int32)[None, :]
    return {
        "x": x,
        "positions": positions,
        "norm1_w": 1.0 + nrm(ks[2], (DEPTH, D_MODEL), 0.02),
        "w_in": nrm(ks[3], (DEPTH, D_MODEL, IN_WIDTH), D_MODEL ** -0.5),
        "q_norm_w": 1.0 + nrm(ks[4], (DEPTH, HEAD_DIM), 0.02),
        "k_norm_w": 1.0 + nrm(ks[5], (DEPTH, HEAD_DIM), 0.02),
        "attn_sinks": nrm(ks[6], (DEPTH, N_Q_HEADS), 0.5),
        "hgrn_lower_bounds": nrm(ks[7], (DEPTH + 1, HGRN_KEY_WIDTH), 0.5),
        "hgrn_norm_w": 1.0 + nrm(ks[8], (DEPTH, HGRN_VAL_DIM), 0.02),
        "w_branch_attn": nrm(ks[9], (DEPTH, ATTN_WIDTH, D_MODEL), ATTN_WIDTH ** -0.5),
        "w_branch_hgrn": nrm(ks[10], (DEPTH, HGRN_WIDTH, D_MODEL), HGRN_WIDTH ** -0.5),
        "w_out": nrm(ks[11], (DEPTH, D_MODEL, D_MODEL), D_MODEL ** -0.5),
        "norm2_w": 1.0 + nrm(ks[12], (DEPTH, D_MODEL), 0.02),
        "w_router_group": nrm(ks[13], (DEPTH, D_MODEL, N_GROUPS), D_MODEL ** -0.5),
        "b_router_group": nrm(ks[14], (DEPTH, N_GROUPS), 0.01),
        "w_router_expert": nrm(ks[15], (DEPTH, D_MODEL, N_EXPERTS), D_MODEL ** -0.5),
        "b_router_expert": nrm(ks[16], (DEPTH, N_EXPERTS), 0.01),
        "w_gate_experts": nrm(ks[17], (DEPTH, N_EXPERTS, D_MODEL, EXPERT_FF), D_MODEL ** -0.5),
        "w_up_experts": nrm(ks[18], (DEPTH, N_EXPERTS, D_MODEL, EXPERT_FF), D_MODEL ** -0.5),
        "w_down_experts": nrm(ks[19], (DEPTH, N_EXPERTS, EXPERT_FF, D_MODEL), EXPERT_FF ** -0.5),
    }


def reference(x, positions, norm1_w, w_in, q_norm_w, k_norm_w, attn_sinks, hgrn_lower_bounds,
              hgrn_norm_w, w_branch_attn, w_branch_hgrn, w_out, norm2_w, w_router_group,
              b_router_group, w_router_expert, b_router_expert, w_gate_experts, w_up_experts,
              w_down_experts):
    B, S, D = x.shape
    cos, sin = rope_tables(positions)
    split_points = [int(v) for v in np.cumsum(IN_SIZES)[:-1]]
    lower_bounds = jnp.cumsum(jax.nn.softmax(hgrn_lower_bounds.astype(jnp.float32), axis=0), axis=0)
    for l in range(DEPTH):
        xn = rms_norm(x, norm1_w[l])
        proj = xn @ w_in[l]
        q_a, k_a, v_a, q_h, f_h, i_h, g_h, z_a, z_b = jnp.split(proj, split_points, axis=-1)

        q_a = partial_rope(rms_norm(q_a.reshape(B, S, N_Q_HEADS, HEAD_DIM), q_norm_w[l]), cos, sin)
        k_a = partial_rope(rms_norm(k_a.reshape(B, S, N_KV_HEADS, HEAD_DIM), k_norm_w[l]), cos, sin)
        v_a = v_a.reshape(B, S, N_KV_HEADS, HEAD_DIM)
        attn = sliding_window_gqa(q_a, k_a, v_a, attn_sinks[l])

        lb = lower_bounds[l]
        fg = lb + (1.0 - lb) * jax.nn.sigmoid(f_h.astype(jnp.float32))
        hq = jax.nn.silu(q_h.astype(jnp.float32)).reshape(B, S, HGRN_HEADS, HGRN_KEY_DIM)
        hk = (1.0 - fg).reshape(B, S, HGRN_HEADS, HGRN_KEY_DIM)
        hlogf = jnp.log(fg).reshape(B, S, HGRN_HEADS, HGRN_KEY_DIM)
        hv = i_h.astype(jnp.float32).reshape(B, S, HGRN_HEADS, HGRN_VAL_DIM)
        ho = rms_norm(hgrn2_chunkwise(hq, hk, hv, hlogf), hgrn_norm_w[l]).reshape(B, S, HGRN_WIDTH)
        hgrn = (ho * jax.nn.silu(g_h.astype(jnp.float32))).astype(x.dtype)

        mixed = (jax.nn.sigmoid(z_a) * (attn @ w_branch_attn[l])
                 + jax.nn.sigmoid(z_b) * (hgrn @ w_branch_hgrn[l]))
        x = x + mixed @ w_out[l]

        hn = rms_norm(x, norm2_w[l]).reshape(B * S, D)
        moe = hierarchical_moe(hn, w_router_group[l], b_router_group[l], w_router_expert[l],
                               b_router_expert[l], w_gate_experts[l], w_up_experts[l], w_down_experts[l])
        x = x + moe.reshape(B, S, D).astype(x.dtype)
    return x
```

```python
import math
import os
from contextlib import ExitStack
import numpy as np
import concourse.bass as bass
import concourse.mybir as mybir
from concourse.bass_utils import run_bass_kernel_spmd

F32 = mybir.dt.float32
BF16 = mybir.dt.bfloat16
I32 = mybir.dt.int32
ALU = mybir.AluOpType
AF = mybir.ActivationFunctionType
AX = mybir.AxisListType

D = 1024
NEG = -30000.0
EPS = 1e-6
TWO_PI = 2.0 * math.pi


class Buf:
    __slots__ = ("name", "last_w", "readers", "excl")

    def __init__(self, name, excl=False):
        self.name = name
        self.last_w = None
        self.readers = []
        self.excl = excl


class DmaSem:
    def __init__(self, key):
        self.key = key
        self.count = 0


class Prog:
    ENGS = ("pe", "act", "dve", "pool", "sp")
    EMAP = {"pe": "tensor", "act": "scalar", "dve": "vector", "pool": "gpsimd", "sp": "sync"}

    def __init__(self, nc, stack, n_dma_sems=48):
        self.nc = nc
        self.ops = {e: [] for e in self.ENGS}
        self.n = {e: 0 for e in self.ENGS}
        self.waited = {e: {} for e in self.ENGS}
        self.dma_sems = [DmaSem("d%d" % i) for i in range(n_dma_sems)]
        self.sems = {}
        for e in self.ENGS:
            self.sems[e] = stack.enter_context(nc.semaphore("s_" + e))
        for d in self.dma_sems:
            self.sems[d.key] = stack.enter_context(nc.semaphore("s_" + d.key))
        self._next = 0
        self.tfree = {e: 0.0 for e in self.ENGS}
        self.hfin = {}
        self.step_max = 0.0
        self.act_grp = None

    def _time(self, eng, deps, h, cost):
        t = self.tfree[eng]
        for d in deps:
            f = self.hfin.get(d)
            if f is not None:
                f = f + (0.05 if d[0] == eng else 0.2)
                if f > t:
                    t = f
        fin = t + cost
        if h[0] in self.ENGS:
            self.tfree[eng] = fin
        else:
            self.tfree[eng] = t + 0.1
        self.hfin[h] = fin
        if fin > self.step_max:
            self.step_max = fin

    def newsem(self):
        s = self.dma_sems[self._next]
        self._next += 1
        return s

    def _deps(self, eng, reads, writes):
        deps = set()
        for b in reads:
            if b.last_w is not None:
                deps.add(b.last_w)
        for b in writes:
            if b.last_w is not None:
                deps.add(b.last_w)
            deps.update(b.readers)
        w = self.waited[eng]
        best = {}
        for (sk, v) in deps:
            if eng == "pe" and sk == "pe":
                continue
            if w.get(sk, 0) < v and best.get(sk, 0) < v:
                best[sk] = v
        for sk, v in best.items():
            self.ops[eng].append(("wait", sk, v))
            w[sk] = v
        return deps

    def _mark(self, h, reads, writes):
        for b in reads:
            b.readers.append(h)
            if len(b.readers) > 64:
                b.readers = b.readers[-48:]
        for b in writes:
            b.last_w = h
            b.readers = []

    def op(self, eng, fn, reads=(), writes=(), cost=0.3):
        ex = [b for b in reads if b.excl and b not in writes]
        if ex:
            writes = list(writes) + ex
        deps = self._deps(eng, reads, writes)
        self.n[eng] += 1
        h = (eng, self.n[eng])
        self.ops[eng].append(("op", fn))
        self._time(eng, deps, h, cost)
        self._mark(h, reads, writes)
        return h

    def dma(self, eng, fn, sem, reads=(), writes=(), cost=3.0):
        deps = self._deps(eng, reads, writes)
        sem.count += 16
        h = (sem.key, sem.count)
        self.ops[eng].append(("dma", fn, sem.key))
        self._time(eng, deps, h, cost)
        self._mark(h, reads, writes)
        return h

    def wait_all(self, eng, bufs):
        self._deps(eng, bufs, ())

    def barrier(self):
        for e in self.ENGS:
            w = self.waited[e]
            for f in self.ENGS:
                if f != e and self.n[f] > w.get(f, 0):
                    self.ops[e].append(("wait", f, self.n[f]))
                    w[f] = self.n[f]
            for d in self.dma_sems:
                if d.count > w.get(d.key, 0):
                    self.ops[e].append(("wait", d.key, d.count))
                    w[d.key] = d.count

    def emit_block(self):
        sems = self.sems
        with self.nc.Block() as block:
            def mk(e):
                items = self.ops[e]

                def body(engobj):
                    for item in items:
                        if item[0] == "wait":
                            engobj.wait_ge(sems[item[1]], item[2])
                        elif item[0] == "op":
                            item[1](engobj).then_inc(sems[e], 1)
                        else:
                            item[1](engobj).then_inc(sems[item[2]], 16)
                return body
            for e in self.ENGS:
                if self.ops[e]:
                    getattr(block, self.EMAP[e])(mk(e))
        self.ops = {e: [] for e in self.ENGS}


C_Q, C_KV, C_QH, C_FH, C_IH, C_GH, C_ZA, C_ZB = 0, 512, 768, 1280, 1792, 2304, 2816, 3840
NIN = 4864
K_ID, K_TRI, K_SEL2, K_INVF, K_IOTA, K_END = 0, 128, 256, 258, 266, 298
B_ID, B_STRI, B_ONES, B_MCUR, B_MPREV, B_MHALO, B_END = 0, 128, 256, 384, 896, 1408, 1920
Q_N2, Q_QW, Q_KW, Q_HW, Q_SINK, Q_BRT, Q_N1C, Q_N2C, Q_END = 0, 1024, 1536, 1664, 2176, 2184, 2220, 2228, 2236


def build(NT=32, CAP=384, NPRE=None, mode=0):
    if NPRE is None:
        NPRE = NT
    T = NT * 128
    NSLOT = 32 * CAP
    NB = CAP // 128
    nc = bass.Bass("TRN2", target_bir_lowering=False)

    def din(name, shape, dt=F32):
        return nc.dram_tensor(name, shape, dt, kind="ExternalInput").ap()

    x_own = din("x_own", [T, D])
    x_pre = din("x_pre", [NPRE * 128, D])
    pos_d = din("pos", [128, NT + 1], I32)
    cf_d = din("cf32", [128, K_END])
    cb_d = din("cb16", [128, B_END])
    pp_d = din("pp32", [128, Q_END])
    hlb_d = din("hlb", [128, 1024])
    win_d = din("w_in", [D, NIN])
    wba_d = din("w_ba", [512, D])
    wbh_d = din("w_bh", [512, D])
    wout_d = din("w_out", [D, D])
    wr_d = din("w_r", [D, 36])
    NEX = 32 if mode == 0 else 1
    wg_d = din("w_g", [NEX, D, 512])
    wu_d = din("w_u", [NEX, D, 512])
    wd_d = din("w_d", [NEX, 512, D])
    out_d = nc.dram_tensor("out", [T, D], F32, kind="ExternalOutput").ap()
    xbuf_d = nc.dram_tensor("xbuf", [NSLOT, D], BF16, kind="Internal").ap()
    ybuf_d = nc.dram_tensor("ybuf", [NSLOT, D], F32, kind="Internal").ap()
    wgb_d = nc.dram_tensor("wgb", [NEX, D, 512], BF16, kind="Internal").ap()
    wub_d = nc.dram_tensor("wub", [NEX, D, 512], BF16, kind="Internal").ap()
    wdb_d = nc.dram_tensor("wdb", [NEX, 512, D], BF16, kind="Internal").ap()

    with ExitStack() as top:
        P = Prog(nc, top)

        def SB(st, name, shape, dt):
            return st.enter_context(nc.sbuf_tensor(name, shape, dt))

        def PS(st, name, shape, dt):
            return st.enter_context(nc.psum_tensor(name, shape, dt))

        def nfree(ap):
            n = 1
            for d in ap.shape[1:]:
                n *= d
            return n

        def ecost(eng, ap):
            n = nfree(ap)
            if eng == "pool":
                return 0.12 + n * 0.0022
            if eng == "act":
                return 0.2 + n * 0.00095
            return 0.07 + n * 0.00123

        AGRP = {AF.Exp: 1, AF.Ln: 1, AF.Sigmoid: 2, AF.Silu: 3, AF.Sin: 4}

        def mm(out, lhsT, rhs, start, stop, r, w):
            c = max(0.064, 0.00042 * nfree(rhs))
            if lhsT.dtype == F32:
                c *= 4
            return P.op("pe", lambda e: e.matmul(out=out, lhsT=lhsT, rhs=rhs, start=start, stop=stop), r, w, cost=c)

        def tr(out, in_, ident, r, w):
            c = 0.11 * (4 if in_.dtype == F32 else 1)
            return P.op("pe", lambda e: e.transpose(out=out, in_=in_, identity=ident), r, w, cost=c)

        def act(out, in_, func, r, w, **kw):
            c = ecost("act", in_)
            g = AGRP.get(func)
            if g is not None and g != P.act_grp:
                c += 1.3
                P.act_grp = g
            return P.op("act", lambda e: e.activation(out=out, in_=in_, func=func, **kw), r, w, cost=c)

        def tt(eng, out, in0, in1, op, r, w):
            return P.op(eng, lambda e: e.tensor_tensor(out=out, in0=in0, in1=in1, op=op), r, w, cost=ecost(eng, out))

        def ts(eng, out, in0, s1, s2, op0, op1, r, w):
            if op1 is None:
                return P.op(eng, lambda e: e.tensor_scalar(out=out, in0=in0, scalar1=s1, scalar2=None, op0=op0), r, w, cost=ecost(eng, out))
            return P.op(eng, lambda e: e.tensor_scalar(out=out, in0=in0, scalar1=s1, scalar2=s2, op0=op0, op1=op1), r, w, cost=ecost(eng, out))

        def stt(eng, out, in0, scalar, in1, op0, op1, r, w):
            return P.op(eng, lambda e: e.scalar_tensor_tensor(out=out, in0=in0, scalar=scalar, in1=in1, op0=op0, op1=op1), r, w, cost=ecost(eng, out))

        def cp(eng, out, in_, r, w):
            if eng == "act":
                return P.op("act", lambda e: e.copy(out=out, in_=in_), r, w, cost=ecost(eng, out))
            return P.op(eng, lambda e: e.tensor_copy(out=out, in_=in_), r, w, cost=ecost(eng, out))

        def red(eng, out, in_, op, r, w):
            return P.op(eng, lambda e: e.tensor_reduce(out=out, in_=in_, axis=AX.X, op=op), r, w, cost=ecost(eng, in_))

        def mset(eng, ap, val, w):
            return P.op(eng, lambda e: e.memset(ap, val), (), w, cost=ecost(eng, ap))

        def dma(eng, out, in_, sem, r, w):
            return P.dma(eng, lambda e: e.dma_start(out=out, in_=in_), sem, r, w)

        def bc(ap, shape, axis):
            return ap.unsqueeze(axis).to_broadcast(shape)

        idb = SB(top, "idb", [128, B_END], BF16); b_cb = Buf("cb")
        cf = SB(top, "cf", [128, K_END], F32); b_cf = Buf("cf")
        dest = SB(top, "dest", [128, 2, NT], I32); b_dest = Buf("dest")
        gate = SB(top, "gate", [128, 2, NT], F32); b_gate = Buf("gate")
        b_xbuf = Buf("xbuf"); b_ybuf = Buf("ybuf"); b_out = Buf("out")
        ident_f = cf[:, K_ID:K_ID + 128]
        tri_f = cf[:, K_TRI:K_TRI + 128]
        sel2 = cf[:, K_SEL2:K_SEL2 + 2]
        invf = cf[:, K_INVF:K_INVF + 8]
        iotae = cf[:, K_IOTA:K_IOTA + 32]
        ident_b = idb[:, B_ID:B_ID + 128]
        stri_b = idb[:, B_STRI:B_STRI + 128]
        ones_b = idb[:, B_ONES:B_ONES + 128]
        mask_cur = idb[:, B_MCUR:B_MCUR + 512]
        mask_prev = idb[:, B_MPREV:B_MPREV + 512]
        mask_halo = idb[:, B_MHALO:B_MHALO + 512]

        sem_c = [P.newsem() for _ in range(4)]
        dma("sp", cf[:], cf_d, sem_c[0], [], [b_cf])
        dma("pool", idb[:], cb_d, sem_c[1], [], [b_cb])

        with ExitStack() as s1:
            Wp = SB(s1, "Wp", [128, 8, NIN], BF16); b_Wp = Buf("Wp")
            wba = SB(s1, "wba", [128, 4, D], BF16); b_wba = Buf("wba")
            wbh = SB(s1, "wbh", [128, 4, D], BF16); b_wbh = Buf("wbh")
            wout = SB(s1, "wout", [128, 8, D], BF16); b_wout = Buf("wout")
            wr = SB(s1, "wr", [128, 8, 36], F32); b_wr = Buf("wr")
            pp = SB(s1, "pp", [128, Q_END], F32); b_pp = Buf("pp")
            lbt = SB(s1, "lbt", [128, 2, 512], F32); b_lb = Buf("lb")
            esink = SB(s1, "esink", [128, 8], F32); b_esink = Buf("esink")
            posi = SB(s1, "posi", [128, NT + 1], I32); b_posi = Buf("posi")
            NA = (NT + 1) * 8
            trig = SB(s1, "trig", [128, 2, NA], F32); b_trig = Buf("trig")
            cst = SB(s1, "cst", [128, 4], F32); b_cst = Buf("cst")
            cos_t = trig[:, 0, :].rearrange("p (i j) -> p i j", j=8)
            sin_t = trig[:, 1, :].rearrange("p (i j) -> p i j", j=8)

            n2w = pp[:, Q_N2:Q_N2 + 1024]
            qw = pp[:, Q_QW:Q_QW + 512]
            kw = pp[:, Q_KW:Q_KW + 128]
            hw = pp[:, Q_HW:Q_HW + 512]
            brt = pp[:, Q_BRT:Q_BRT + 36]

            dma("sp", pp[:], pp_d, sem_c[2], [], [b_pp])
            dma("sp", posi[:], pos_d, sem_c[3], [], [b_posi])
            WG = [(C_FH, C_GH), (C_KV, C_QH), (C_Q, C_KV), (C_QH, C_FH), (C_GH, C_ZA), (C_ZA, NIN)]
            b_Wg = [Buf("Wg%d" % g) for g in range(len(WG))]
            win3 = win_d.rearrange("(c p) n -> p c n", p=128)
            for g, (c0, c1) in enumerate(WG):
                dma("pool", Wp[:, :, c0:c1], win3[:, :, c0:c1], P.newsem(), [], [b_Wg[g]])

            def wbuf(c0):
                for g, (a0, a1) in enumerate(WG):
                    if a0 <= c0 < a1:
                        return b_Wg[g]
                raise ValueError(c0)
            sem_w = P.newsem()
            dma("pool", wba[:], wba_d.rearrange("(c p) n -> p c n", p=128), sem_w, [], [Buf("wtmp")])
            dma("pool", wbh[:], wbh_d.rearrange("(c p) n -> p c n", p=128), sem_w, [], [Buf("wtmp")])
            hW = dma("pool", wout[:], wout_d.rearrange("(c p) n -> p c n", p=128), sem_w, [], [Buf("wtmp")])
            for b in (b_wba, b_wbh, b_wout):
                b.last_w = hW
            sem_wr = P.newsem()
            dma("sp", wr[:], wr_d.rearrange("(c p) n -> p c n", p=128), sem_wr, [], [b_wr])

            xt = [SB(s1, "xt%d" % k, [128, D], F32) for k in range(3)]; b_xt = [Buf("xt%d" % k) for k in range(3)]
            sem_x = [P.newsem() for _ in range(3)]
            junkF = SB(s1, "junkF", [128, D], BF16); b_junkF = Buf("junkF")
            junkY = SB(s1, "junkY", [128, D], BF16); b_junkY = Buf("junkY")
            stF = SB(s1, "stF", [128, 8], F32); b_stF = Buf("stF")
            stA = SB(s1, "stA", [128, 64], F32); b_stA = Buf("stA")
            stH = SB(s1, "stH", [128, 16], F32); b_stH = Buf("stH")
            stY = SB(s1, "stY", [128, 8], F32); b_stY = Buf("stY")
            xb = SB(s1, "xb", [128, D], BF16); b_xb = Buf("xb")
            xT = SB(s1, "xT", [128, 8, 128], BF16); b_xT = Buf("xT")
            qa = SB(s1, "qa", [128, 512], F32); b_qa = Buf("qa")
            kv = SB(s1, "kvf", [128, 128], F32); b_kv = Buf("kv")
            r16 = SB(s1, "r16", [128, 6, 8, 8], F32); b_r16 = Buf("r16")
            qr = SB(s1, "qr", [128, 512], BF16); b_qr = Buf("qr")
            kr = SB(s1, "kr", [128, 128], BF16); b_kr = Buf("kr")
            qT = SB(s1, "qT", [128, 4, 128], BF16); b_qT = Buf("qT")
            kTs = [SB(s1, "kT%d" % k, [128, 128], BF16) for k in range(2)]; b_kT = [Buf("kT0"), Buf("kT1")]
            vx = [SB(s1, "vx%d" % k, [128, 2, 80], BF16) for k in range(2)]; b_vx = [Buf("vx0"), Buf("vx1")]
            pTb = [SB(s1, "pT%d" % k, [128, 512], BF16) for k in range(2)]; b_pTb = [Buf("pT0"), Buf("pT1")]
            attn = SB(s1, "attn", [128, 512], BF16); b_attn = Buf("attn")
            aT = SB(s1, "aT", [128, 4, 128], BF16); b_aT = Buf("aT")
            qh = SB(s1, "qh", [128, 512], F32); b_qh = Buf("qh")
            hA = SB(s1, "hA", [128, 512], F32); b_hA = Buf("hA")
            hk = SB(s1, "hk", [128, 512], F32); b_hk = Buf("hk")
            eb = SB(s1, "eb", [128, 512], F32); b_eb = Buf("eb")
            iv = SB(s1, "iv", [128, 512], BF16); b_iv = Buf("iv")
            gs = SB(s1, "gs", [128, 512], F32); b_gs = Buf("gs")
            qt = SB(s1, "qt", [128, 512], BF16); b_qt = Buf("qt")
            kt = SB(s1, "kt", [128, 512], BF16); b_kt = Buf("kt")
            qlh = SB(s1, "qlh", [128, 4, 2, 128], BF16); b_qlh = Buf("qlh")
            ktT = SB(s1, "ktT", [128, 4, 128], BF16); b_ktT = Buf("ktT")
            Am = [SB(s1, "Am%d" % k, [128, 128], BF16) for k in range(2)]; b_Am = [Buf("Am0"), Buf("Am1")]
            S = SB(s1, "S", [128, 4, 128], F32); b_S = Buf("S")
            Sb = [SB(s1, "Sb%d" % k, [128, 4, 128], BF16) for k in range(2)]; b_Sb = [Buf("Sb0"), Buf("Sb1")]
            Dsb = SB(s1, "Dsb", [128, 4, 2], F32); b_Dsb = Buf("Dsb")
            hg = SB(s1, "hg", [128, 512], BF16); b_hg = Buf("hg")
            hgT = SB(s1, "hgT", [128, 4, 128], BF16); b_hgT = Buf("hgT")
            sza = SB(s1, "sza", [128, D], BF16); b_sza = Buf("sza")
            szb = SB(s1, "szb", [128, D], BF16); b_szb = Buf("szb")
            m1 = SB(s1, "m1", [128, D], F32); b_m1 = Buf("m1")
            mixed = SB(s1, "mixed", [128, D], BF16); b_mixed = Buf("mixed")
            mT = SB(s1, "mT", [128, 8, 128], BF16); b_mT = Buf("mT")
            hb = SB(s1, "hb", [128, D], BF16); b_hb = Buf("hb")
            rt = SB(s1, "rt", [128, 256], F32); b_rt = Buf("rt")
            cums = SB(s1, "cums", [128, 32], F32); b_cums = Buf("cums")
            cumsb = SB(s1, "cumsb", [128, 32], BF16); b_cumsb = Buf("cumsb")
            selb = SB(s1, "selb", [128, 32], BF16); b_selb = Buf("selb")
            sem_x1 = P.newsem()
            sem_sc = P.newsem()

            tp = PS(s1, "tp", [128, 1024], BF16); b_tp = Buf("tp", True)
            mmA = PS(s1, "mmA", [128, 512], F32); mmB = PS(s1, "mmB", [128, 512], F32)
            b_mm = [Buf("mmA", True), Buf("mmB", True)]; mmP = [mmA, mmB]
            sc = [PS(s1, "sc0", [128, 512], F32)]; b_sc = [Buf("sc0", True)]
            tpB = PS(s1, "tpB", [128, 1024], BF16); b_tpB = Buf("tpB", True)
            pv = [PS(s1, "pv0", [128, 512], F32), PS(s1, "pv1", [128, 512], F32)]; b_pv = [Buf("pv0", True), Buf("pv1", True)]
            misc = PS(s1, "misc", [128, 512], F32); b_misc = Buf("misc", True)

            mset("dve", qlh[:], 0.0, [b_qlh])
            mset("dve", S[:], 0.0, [b_S])
            mset("dve", Sb[0][:], 0.0, [b_Sb[0]])
            mset("dve", cums[:], 0.0, [b_cums])
            mset("dve", cumsb[:], 0.0, [b_cumsb])
            for k in range(2):
                mset("dve", vx[k][:], 0.0, [b_vx[k]])
                mset("dve", vx[k][:, :, 64:65], 1.0, [b_vx[k]])
            def scale_w(g):
                c0, c1 = WG[g]
                tt("dve", Wp[:, :, c0:c1], Wp[:, :, c0:c1], bc(pp[:, Q_N1C:Q_N1C + 8], [128, 8, c1 - c0], 2), ALU.mult,
                   [b_Wg[g], b_pp], [b_Wg[g]])
            scale_w(0)
            scale_w(1)
            pending_w = [2, 3, 4, 5]
            for c in range(8):
                ts("dve", wr[:, c, :], wr[:, c, :], pp[:, Q_N2C + c:Q_N2C + c + 1], None, ALU.mult, None, [b_wr, b_pp], [b_wr])
            zt = hb[:]; b_zt = b_hb
            ZC = 1024
            mset("pool", zt, 0.0, [b_zt])
            sem_z = P.newsem()
            xz = xbuf_d.rearrange("(p r) d -> p (r d)", p=128)
            tot = (NSLOT // 128) * D
            zchunks = [(k0, min(tot, k0 + ZC)) for k0 in range(0, tot, ZC)]

            def zero_some(n):
                for _ in range(n):
                    if zchunks:
                        k0, k1 = zchunks.pop(0)
                        b_xbuf.last_w = dma("sp", xz[:, k0:k1], zt[:, 0:k1 - k0], sem_z, [b_zt], [Buf("ztmp")])

            mset("dve", cst[:, 0:1], math.pi / 2, [b_cst])
            mset("dve", cst[:, 1:2], EPS, [b_cst])
            dma("sp", m1[:], hlb_d, P.newsem(), [], [b_m1])
            hl = m1[:].rearrange("p (a c) -> p a c", a=2)
            tt("dve", lbt[:, 0, :], hl[:, 0, :], hl[:, 1, :], ALU.subtract, [b_m1], [b_lb])
            act(lbt[:, 0, :], lbt[:, 0, :], AF.Sigmoid, [b_lb], [b_lb])
            ts("dve", lbt[:, 1, :], lbt[:, 0, :], -1.0, 1.0, ALU.mult, ALU.add, [b_lb], [b_lb])
            act(esink[:], pp[:, Q_SINK:Q_SINK + 8], AF.Exp, [b_pp], [b_esink])
            b_tg = Buf("trigtmp")
            tA = xt[1][:, 0:2 * NA].rearrange("p (a n) -> p a n", a=2)
            tB = xt[2][:, 0:2 * NA].rearrange("p (a n) -> p a n", a=2)
            TR = [b_tg, b_xt[1], b_xt[2]]
            trg = {0: tA[:, 0, :], 1: tA[:, 1, :], 2: tB[:, 0, :], 3: tB[:, 1, :], 4: trig[:, 0, :], 5: trig[:, 1, :]}
            posf = trg[0][:, 0:NT + 1]
            cp("dve", posf, posi[:], [b_posi], TR)
            ang = trg[1].rearrange("p (i j) -> p i j", j=8)
            tt("dve", ang, bc(posf, [128, NT + 1, 8], 2), bc(invf, [128, NT + 1, 8], 1), ALU.mult, TR + [b_cf], TR)
            kf = trg[2]
            ts("dve", kf, trg[1], 1.0 / TWO_PI, None, ALU.mult, None, TR, TR)
            ki = trg[3].bitcast(I32)
            cp("dve", ki, kf, TR, TR)
            cp("dve", kf, ki, TR, TR)
            r_ = trg[1]
            stt("dve", r_, kf, -TWO_PI, r_, ALU.mult, ALU.add, TR, TR)
            s4 = trg[2]
            c4 = trg[3]
            act(s4, r_, AF.Sin, TR, TR, scale=0.25)
            act(c4, r_, AF.Sin, TR + [b_cst], TR, scale=0.25, bias=cst[:, 0:1])
            s2 = trg[0]
            c2 = trg[1]
            stt("dve", s2, s4, 2.0, c4, ALU.mult, ALU.mult, TR, TR)
            tt("dve", c2, s4, s4, ALU.mult, TR, TR)
            ts("dve", c2, c2, -2.0, 1.0, ALU.mult, ALU.add, TR, TR)
            stt("dve", trg[5], s2, 2.0, c2, ALU.mult, ALU.mult, TR, [b_trig])
            tt("dve", trg[4], s2, s2, ALU.mult, TR, [b_trig])
            ts("dve", trg[4], trg[4], -2.0, 1.0, ALU.mult, ALU.add, [b_trig], [b_trig])


            mmc = [0]

            def next_mm():
                k = mmc[0] % 2
                mmc[0] += 1
                return mmP[k], b_mm[k]

            def rstd_of(stt_, b_s, col, src_col, scale=1.0):
                act(stt_[:, col], stt_[:, src_col], AF.Ln, [b_s, b_cst], [b_s], bias=cst[:, 1:2], scale=scale)
                act(stt_[:, col], stt_[:, col], AF.Exp, [b_s], [b_s], scale=-0.5)

            def load_x(src, i, slot):
                dma("sp", xt[slot][:], src[i * 128:(i + 1) * 128, :], sem_x[slot], [], [b_xt[slot]])

            rstd1 = stF[:, 1:2]

            def front(slot):
                mset("dve", stF[:, 0:1], 0.0, [b_stF])
                act(junkF[:], xt[slot][:], AF.Square, [b_xt[slot]], [b_junkF, b_stF], scale=1.0 / 32, accum_out=stF[:, 0:1])
                rstd_of(stF, b_stF, slice(1, 2), slice(0, 1))
                cp("act", xb[:], xt[slot][:], [b_xt[slot]], [b_xb])
                for c in range(8):
                    tr(tp[:, c * 128:(c + 1) * 128], xb[:, c * 128:(c + 1) * 128], ident_b, [b_xb, b_cb], [b_tp])
                cp("dve", xT[:].rearrange("p c t -> p (c t)"), tp[:], [b_tp], [b_xT])

            def proj(c0, n):
                ps_, b_ps = next_mm()
                for c in range(8):
                    mm(ps_[:, 0:n], xT[:, c, :], Wp[:, c, c0:c0 + n], c == 0, c == 7, [b_xT, wbuf(c0)], [b_ps])
                return ps_, b_ps

            def qknorm_rope(src, b_src, nh, wtile, dst, b_dst, ti):
                n = nh * 64
                s3 = src.rearrange("p (h d) -> p h d", d=64)
                d3 = dst.rearrange("p (h d) -> p h d", d=64)
                w3 = wtile.rearrange("p (h d) -> p h d", d=64)
                tt("pool", dst, src, src, ALU.mult, [b_src], [b_dst])
                red("dve", stA[:, 8:8 + nh], d3, ALU.add, [b_dst], [b_stA])
                rstd_of(stA, b_stA, slice(16, 16 + nh), slice(8, 8 + nh), scale=1.0 / 64)
                tt("dve", s3, s3, bc(stA[:, 16:16 + nh], [128, nh, 64], 2), ALU.mult, [b_src, b_stA], [b_src])
                tt("dve", dst, src, wtile, ALU.mult, [b_src, b_pp], [b_dst])
                x1 = r16[:, 0, 0:nh, :]; x2 = r16[:, 1, 0:nh, :]
                tt("dve", x1, s3[:, :, 0:8], w3[:, :, 0:8], ALU.mult, [b_src, b_pp], [b_r16])
                tt("dve", x2, s3[:, :, 8:16], w3[:, :, 8:16], ALU.mult, [b_src, b_pp], [b_r16])
                cs = bc(cos_t[:, ti, :], [128, nh, 8], 1)
                sn = bc(sin_t[:, ti, :], [128, nh, 8], 1)
                a_ = r16[:, 2, 0:nh, :]; b_ = r16[:, 3, 0:nh, :]; c_ = r16[:, 4, 0:nh, :]; d_ = r16[:, 5, 0:nh, :]
                tt("dve", a_, x1, cs, ALU.mult, [b_r16, b_trig], [b_r16])
                tt("dve", b_, x2, sn, ALU.mult, [b_r16, b_trig], [b_r16])
                tt("dve", c_, x2, cs, ALU.mult, [b_r16, b_trig], [b_r16])
                tt("dve", d_, x1, sn, ALU.mult, [b_r16, b_trig], [b_r16])
                tt("dve", d3[:, :, 0:8], a_, b_, ALU.subtract, [b_r16], [b_dst])
                tt("dve", d3[:, :, 8:16], c_, d_, ALU.add, [b_r16], [b_dst])

            def kv_proj(slot):
                ps_, b_ps = proj(C_KV, 256)
                act(kv[:], ps_[:, 0:128], AF.Copy, [b_ps, b_stF], [b_kv], scale=rstd1)
                act(vx[slot][:, :, 0:64], ps_[:, 128:256].rearrange("p (m d) -> p m d", d=64), AF.Copy, [b_ps, b_stF], [b_vx[slot]], scale=rstd1)

            def k_finish(ti, slot):
                qknorm_rope(kv[:], b_kv, 2, kw, kr[:], b_kr, ti)
                tr(tp[:, 0:128], kr[:], ident_b, [b_kr, b_cb], [b_tp])
                cp("dve", kTs[slot][:], tp[:, 0:128], [b_tp], [b_kT[slot]])

            HS = [(hA, b_hA, hk, b_hk, eb, b_eb, iv, b_iv, kt, b_kt),
                  (qa, b_qa, qh, b_qh, gs, b_gs, qr, b_qr, attn, b_attn)]

            def hgrn_proj(hs=0):
                hA, b_hA, hk, b_hk, eb, b_eb, iv, b_iv, kt, b_kt = HS[hs]
                ps_, b_ps = proj(C_FH, 512)
                act(hA[:], ps_[:], AF.Sigmoid, [b_ps, b_stF], [b_hA], scale=rstd1)
                ps2, b_ps2 = proj(C_IH, 512)
                act(iv[:], ps2[:], AF.Copy, [b_ps2, b_stF], [b_iv], scale=rstd1)

            def hgrn_chain(hs=0):
                hA, b_hA, hk, b_hk, eb, b_eb, iv, b_iv, kt, b_kt = HS[hs]
                tt("pool", hA[:], hA[:], lbt[:, 1, :], ALU.mult, [b_hA, b_lb], [b_hA])
                tt("pool", hA[:], hA[:], lbt[:, 0, :], ALU.add, [b_hA, b_lb], [b_hA])
                yield
                ts("dve", hk[:], hA[:], -1.0, 1.0, ALU.mult, ALU.add, [b_hA], [b_hk])
                act(hA[:], hA[:], AF.Ln, [b_hA], [b_hA])
                yield
                mm(pv[1][:], tri_f, hA[:], True, True, [b_cf, b_hA], [b_pv[1]])
                act(eb[:], pv[1][:], AF.Exp, [b_pv[1]], [b_eb])
                act(hA[:], pv[1][:], AF.Exp, [b_pv[1]], [b_hA], scale=-1.0)
                yield
                tt("dve", kt[:], hk[:], hA[:], ALU.mult, [b_hk, b_hA], [b_kt])
                for hh in range(4):
                    mm(misc[:, hh * 2:hh * 2 + 2], eb[:, hh * 128:(hh + 1) * 128], sel2, True, True, [b_eb, b_cf], [b_misc])
                cp("dve", Dsb[:].rearrange("p h c -> p (h c)"), misc[:, 0:8], [b_misc], [b_Dsb])
                yield

            def state_update(c, dst, hs=0):
                hA, b_hA, hk, b_hk, eb, b_eb, iv, b_iv, kt, b_kt = HS[hs]
                for hh in range(4):
                    mm(misc[:, hh * 128:(hh + 1) * 128], kt[c * 64:(c + 1) * 64, hh * 128:(hh + 1) * 128],
                       iv[c * 64:(c + 1) * 64, hh * 128:(hh + 1) * 128], True, True, [b_kt, b_iv], [b_misc])
                Sf = S[:].rearrange("p h d -> p (h d)")
                tt("dve", Sf, Sf, misc[:], ALU.add, [b_S, b_misc], [b_S])
                tt("dve", S[:], S[:], bc(Dsb[:, :, c], [128, 4, 128], 2), ALU.mult, [b_S, b_Dsb], [b_S])
                cp("act", Sb[dst][:], S[:], [b_S], [b_Sb[dst]])

            flags = set()

            def run_threads(threads):
                active = [[g, 0.0, None] for g in threads]
                while active:
                    cand = [a for a in active if a[2] is None or a[2] in flags]
                    assert cand, "scheduler deadlock"
                    a = cand[0]
                    active.remove(a)
                    active.append(a)
                    a[2] = None
                    P.step_max = 0.0
                    try:
                        r = next(a[0])
                    except StopIteration:
                        active.remove(a)
                        continue
                    if P.step_max > 0.0:
                        a[1] = P.step_max
                    if r is None:
                        continue
                    if isinstance(r, tuple):
                        if r[0] == "need":
                            a[2] = r[1]
                        else:
                            flags.add(r[1])
                    else:
                        active.append([r, a[1], None])

            load_x(x_pre if NPRE > 0 else x_own, 0, 0)
            def thr_PXF(j):
                front(j % 3)
                yield
                hgrn_proj(j % 2)
                yield
                if j == NPRE - 1:
                    kv_proj(1)
                    yield
                    k_finish(NT, 1)

            def thr_PH(j):
                for _ in hgrn_chain(j % 2):
                    yield
                state_update(0, 1, j % 2)
                yield
                state_update(1, 0, j % 2)

            for j in range(NPRE + 1):
                if j < NPRE:
                    if j + 1 < NPRE:
                        load_x(x_pre, j + 1, (j + 1) % 3)
                    else:
                        load_x(x_own, 0, (j + 1) % 3)
                thr = []
                if j > 0:
                    thr.append(thr_PH(j - 1))
                if j < NPRE:
                    thr.append(thr_PXF(j))
                run_threads(thr)
                if pending_w and j >= 3 and j % 2 == 1:
                    scale_w(pending_w.pop(0))
                if j >= 1:
                    zero_some(4)
            while pending_w:
                scale_w(pending_w.pop(0))
            zero_some(len(zchunks))

            def thr_A(i):
                ks = i % 2
                k_finish(i, ks)
                yield
                qknorm_rope(qa[:], b_qa, 8, qw, qr[:], b_qr, i)
                yield
                for j in range(4):
                    tr(tp[:, j * 128:(j + 1) * 128], qr[:, j * 128:(j + 1) * 128], ident_b, [b_qr, b_cb], [b_tp])
                cp("dve", qT[:].rearrange("p j t -> p (j t)"), tp[:, 0:512], [b_tp], [b_qT])
                yield
                for m in range(2):
                    for kb in range(2):
                        ksl = (1 - ks) if kb == 0 else ks
                        msk = (mask_halo if i == 0 else mask_prev) if kb == 0 else mask_cur
                        mm(sc[0][:], ident_b, msk, True, False, [b_cb], [b_sc[0]])
                        mm(sc[0][:], kTs[ksl][m * 64:(m + 1) * 64, :], qT[m * 64:(m + 1) * 64, :, :].rearrange("p j t -> p (j t)"),
                           False, True, [b_kT[ksl], b_qT], [b_sc[0]])
                        act(pTb[kb][:], sc[0][:], AF.Exp, [b_sc[0]], [b_pTb[kb]], scale=0.125)
                        yield
                    for j in range(4):
                        for kb in range(2):
                            ksl = (1 - ks) if kb == 0 else ks
                            mm(pv[0][:, j * 80:(j + 1) * 80], pTb[kb][:, j * 128:(j + 1) * 128], vx[ksl][:, m, :],
                               kb == 0, kb == 1, [b_pTb[kb], b_vx[ksl]], [b_pv[0]])
                    pv3 = pv[0][:, 0:320].rearrange("p (j d) -> p j d", d=80)
                    den = stA[:, 24 + m * 4:28 + m * 4]
                    tt("dve", den, pv3[:, :, 64], esink[:, m * 4:(m + 1) * 4], ALU.add, [b_pv[0], b_esink], [b_stA])
                    P.op("dve", lambda e, den=den: e.reciprocal(out=den, in_=den), [b_stA], [b_stA])
                    tt("dve", attn[:, m * 256:(m + 1) * 256].rearrange("p (j d) -> p j d", d=64), pv3[:, :, 0:64],
                       bc(den, [128, 4, 64], 2), ALU.mult, [b_pv[0], b_stA], [b_attn])
                    yield
                if i > 0:
                    yield ("need", ("aT", i - 1))
                for j in range(4):
                    tr(tp[:, j * 128:(j + 1) * 128], attn[:, j * 128:(j + 1) * 128], ident_b, [b_attn, b_cb], [b_tp])
                cp("dve", aT[:].rearrange("p j t -> p (j t)"), tp[:, 0:512], [b_tp], [b_aT])

            def thr_H(i):
                for _ in hgrn_chain():
                    yield
                tt("dve", qt[:], qh[:], eb[:], ALU.mult, [b_qh, b_eb], [b_qt])
                tt("pool", gs[:], gs[:], hw, ALU.mult, [b_gs, b_pp], [b_gs])
                yield
                for hh in range(4):
                    tr(tpB[:, hh * 128:(hh + 1) * 128], qt[:, hh * 128:(hh + 1) * 128], ident_b, [b_qt, b_cb], [b_tpB])
                    tr(tpB[:, 512 + hh * 128:512 + (hh + 1) * 128], kt[:, hh * 128:(hh + 1) * 128], ident_b, [b_kt, b_cb], [b_tpB])
                tq = tpB[:, 0:512].rearrange("p (h t) -> p h t", t=128)
                cp("dve", qlh[:, :, 0, 0:64], tq[:, :, 0:64], [b_tpB], [b_qlh])
                cp("dve", qlh[:, :, 1, 64:128], tq[:, :, 64:128], [b_tpB], [b_qlh])
                cp("act", ktT[:].rearrange("p h t -> p (h t)"), tpB[:, 512:1024], [b_tpB], [b_ktT])
                yield
                state_update(0, 1)
                yield
                for hh in range(4):
                    a_ = hh % 2
                    sca = misc[:, 128 + a_ * 128:256 + a_ * 128]
                    mm(sca, ktT[:, hh, :], qlh[:, hh, 0, :], True, False, [b_ktT, b_qlh], [b_misc])
                    mm(sca, ktT[:, hh, :], qlh[:, hh, 1, :], False, True, [b_ktT, b_qlh], [b_misc])
                    tt("dve", Am[a_][:], sca, tri_f, ALU.mult, [b_misc, b_cf], [b_Am[a_]])
                    o_ = pv[1][:, hh * 128:(hh + 1) * 128]
                    mm(o_, Am[a_][:], iv[:, hh * 128:(hh + 1) * 128], True, False, [b_Am[a_], b_iv], [b_pv[1]])
                    mm(o_, qlh[:, hh, 0, :], Sb[0][:, hh, :], False, False, [b_qlh, b_Sb[0]], [b_pv[1]])
                    mm(o_, qlh[:, hh, 1, :], Sb[1][:, hh, :], False, True, [b_qlh, b_Sb[1]], [b_pv[1]])
                    yield
                state_update(1, 0)
                yield
                o3 = pv[1][:].rearrange("p (h d) -> p h d", d=128)
                act(hk[:], pv[1][:], AF.Square, [b_pv[1]], [b_hk])
                red("dve", stH[:, 0:4], hk[:].rearrange("p (h d) -> p h d", d=128), ALU.add, [b_hk], [b_stH])
                rstd_of(stH, b_stH, slice(4, 8), slice(0, 4), scale=1.0 / 128)
                tt("dve", qh[:].rearrange("p (h d) -> p h d", d=128), o3, bc(stH[:, 4:8], [128, 4, 128], 2), ALU.mult,
                   [b_pv[1], b_stH], [b_qh])
                yield
                tt("dve", hg[:], qh[:], gs[:], ALU.mult, [b_qh, b_gs], [b_hg])
                if i > 0:
                    yield ("need", ("hgT", i - 1))
                for j in range(4):
                    tr(tpB[:, j * 128:(j + 1) * 128], hg[:, j * 128:(j + 1) * 128], ident_b, [b_hg, b_cb], [b_tpB])
                cp("dve", hgT[:].rearrange("p j t -> p (j t)"), tpB[:, 0:512], [b_tpB], [b_hgT])

            def thr_XF(i, slot):
                front(slot)
                yield
                ps_, b_ps = proj(C_Q, 512)
                act(qa[:], ps_[:], AF.Copy, [b_ps, b_stF], [b_qa], scale=rstd1)
                kv_proj(i % 2)
                yield thr_A(i)
                hgrn_proj()
                yield
                ps_, b_ps = proj(C_QH, 512)
                act(qh[:], ps_[:], AF.Silu, [b_ps, b_stF], [b_qh], scale=rstd1)
                ps_, b_ps = proj(C_GH, 512)
                act(gs[:], ps_[:], AF.Silu, [b_ps, b_stF], [b_gs], scale=rstd1)
                yield thr_H(i)
                if i > 0:
                    yield ("need", ("hgT", i - 1))
                for half in range(2):
                    ps_, b_ps = proj(C_ZA + half * 512, 512)
                    act(sza[:, half * 512:(half + 1) * 512], ps_[:], AF.Sigmoid, [b_ps, b_stF], [b_sza], scale=rstd1)
                    yield
                for half in range(2):
                    ps_, b_ps = proj(C_ZB + half * 512, 512)
                    act(szb[:, half * 512:(half + 1) * 512], ps_[:], AF.Sigmoid, [b_ps, b_stF], [b_szb], scale=rstd1)
                    yield

            def thr_Y(i, slot):
                for half in range(2):
                    hs = slice(half * 512, (half + 1) * 512)
                    for c in range(4):
                        mm(mmP[half][:], aT[:, c, :], wba[:, c, hs], c == 0, c == 3, [b_aT, b_wba], [b_mm[half]])
                    tt("dve", m1[:, hs], mmP[half][:], sza[:, hs], ALU.mult, [b_mm[half], b_sza], [b_m1])
                    yield
                yield ("set", ("aT", i))
                for half in range(2):
                    hs = slice(half * 512, (half + 1) * 512)
                    for c in range(4):
                        mm(mmP[half][:], hgT[:, c, :], wbh[:, c, hs], c == 0, c == 3, [b_hgT, b_wbh], [b_mm[half]])
                    tt("dve", junkY[:, hs], mmP[half][:], szb[:, hs], ALU.mult, [b_mm[half], b_szb], [b_junkY])
                    tt("pool", mixed[:, hs], junkY[:, hs], m1[:, hs], ALU.add, [b_junkY, b_m1], [b_mixed])
                    yield
                yield ("set", ("hgT", i))
                for c in range(8):
                    tr(tpB[:, c * 128:(c + 1) * 128], mixed[:, c * 128:(c + 1) * 128], ident_b, [b_mixed, b_cb], [b_tpB])
                cp("dve", mT[:].rearrange("p c t -> p (c t)"), tpB[:], [b_tpB], [b_mT])
                yield
                for half in range(2):
                    hs = slice(half * 512, (half + 1) * 512)
                    for c in range(8):
                        mm(mmP[half][:], mT[:, c, :], wout[:, c, hs], c == 0, c == 7, [b_mT, b_wout], [b_mm[half]])
                    tt("dve", xt[slot][:, hs], xt[slot][:, hs], mmP[half][:], ALU.add, [b_xt[slot], b_mm[half]], [b_xt[slot]])
                    yield
                dma("sp", out_d[i * 128:(i + 1) * 128, :], xt[slot][:], sem_x1, [b_xt[slot]], [b_out])
                mset("dve", stY[:, 0:1], 0.0, [b_stY])
                act(junkY[:], xt[slot][:], AF.Square, [b_xt[slot]], [b_junkY, b_stY], scale=1.0 / 32, accum_out=stY[:, 0:1])
                rstd_of(stY, b_stY, slice(1, 2), slice(0, 1))
                rstd2 = stY[:, 1:2]
                stt("dve", hb[:], xt[slot][:], rstd2, n2w, ALU.mult, ALU.mult, [b_xt[slot], b_stY, b_pp], [b_hb])
                yield
                x1T = m1[:].rearrange("p (c t) -> p c t", t=128)
                for g4 in range(2):
                    for c in range(4):
                        tr(mmP[g4][:, c * 128:(c + 1) * 128], xt[slot][:, (g4 * 4 + c) * 128:(g4 * 4 + c + 1) * 128], ident_f,
                           [b_xt[slot], b_cf], [b_mm[g4]])
                    cp("act" if g4 == 0 else "dve", m1[:, g4 * 512:(g4 + 1) * 512], mmP[g4][:], [b_mm[g4]], [b_m1])
                    yield
                for c in range(8):
                    mm(mmP[0][:, 0:36], x1T[:, c, :], wr[:, c, :], c == 0, c == 7, [b_m1, b_wr], [b_mm[0]])
                L = rt[:, 0:36]
                stt("dve", L, mmP[0][:, 0:36], rstd2, brt, ALU.mult, ALU.add, [b_mm[0], b_stY, b_pp], [b_rt])
                yield
                R = [b_rt]
                gmax = rt[:, 36:37]; ngmax = rt[:, 37:38]; gsum = rt[:, 38:39]; pg = rt[:, 39:40]
                red("dve", gmax, L[:, 0:4], ALU.max, R, R)
                ts("dve", ngmax, gmax, -1.0, None, ALU.mult, None, R, R)
                mset("dve", gsum, 0.0, R)
                act(rt[:, 40:44], L[:, 0:4], AF.Exp, R, R, bias=ngmax, accum_out=gsum)
                P.op("dve", lambda e, pg=pg, gsum=gsum: e.reciprocal(out=pg, in_=gsum), R, R)
                gm = rt[:, 44:48]
                ts("dve", gm, L[:, 0:4], gmax, None, ALU.is_ge, None, R, R)
                ts("dve", gm, gm, 1e30, -1e30, ALU.mult, ALU.add, R, R)
                yield
                lem = rt[:, 48:80]
                tt("dve", lem.rearrange("p (g j) -> p g j", j=8), L[:, 4:36].rearrange("p (g j) -> p g j", j=8),
                   bc(gm, [128, 4, 8], 2), ALU.add, R, R)
                top8 = rt[:, 80:88]
                P.op("dve", lambda e, top8=top8, lem=lem: e.max(out=top8, in_=lem), R, R)
                sel = rt[:, 88:120]; is1 = rt[:, 120:152]; isB = rt[:, 152:184]
                ts("dve", sel, lem, top8[:, 1:2], None, ALU.is_ge, None, R, R)
                ts("dve", is1, lem, top8[:, 0:1], None, ALU.is_ge, None, R, R)
                tt("dve", isB, sel, is1, ALU.subtract, R, R)
                yield
                dd = rt[:, 184:185]; s1_ = rt[:, 185:186]
                tt("dve", dd, top8[:, 0:1], top8[:, 1:2], ALU.subtract, R, R)
                act(s1_, dd, AF.Sigmoid, R, R)
                tt("dve", gate[:, 0, i:i + 1], pg, s1_, ALU.mult, R, [b_gate])
                tt("dve", gate[:, 1, i:i + 1], pg, gate[:, 0, i:i + 1], ALU.subtract, R + [b_gate], [b_gate])
                cp("dve", selb[:], sel, R, [b_selb])
                yield
                mm(mmP[1][:, 0:32], stri_b, selb[:], True, False, [b_cb, b_selb], [b_mm[1]])
                mm(mmP[1][:, 0:32], ones_b, cumsb[:], False, True, [b_cb, b_cumsb], [b_mm[1]])
                tt("dve", cums[:], cums[:], sel, ALU.add, R + [b_cums], [b_cums])
                cp("dve", cumsb[:], cums[:], [b_cums], [b_cumsb])
                posc = rt[:, 192:224]
                ts("dve", posc, mmP[1][:, 0:32], float(CAP - 1), None, ALU.min, None, [b_mm[1]], R)
                yield
                tt("dve", posc, posc, iotae, ALU.add, R + [b_cf], R)
                tmpm = rt[:, 224:256]
                dA = rt[:, 186:187]; dB = rt[:, 187:188]
                tt("dve", tmpm, posc, is1, ALU.mult, R, R)
                red("dve", dA, tmpm, ALU.add, R, R)
                tt("dve", tmpm, posc, isB, ALU.mult, R, R)
                red("dve", dB, tmpm, ALU.add, R, R)
                cp("dve", dest[:, 0, i:i + 1], dA, R, [b_dest])
                cp("dve", dest[:, 1, i:i + 1], dB, R, [b_dest])
                yield
                for k in range(2):
                    off = dest[:, k, i:i + 1]
                    P.dma("pool", lambda e, off=off: e.indirect_dma_start(
                        out=xbuf_d, out_offset=bass.IndirectOffsetOnAxis(ap=off, axis=0), in_=hb[:, :], in_offset=None),
                        sem_sc, [b_hb, b_dest, b_xbuf], [Buf("scat")])

            sem_cv = P.newsem()
            cv_next = [0]

            def convert_experts(upto):
                while cv_next[0] < min(upto, NEX):
                    e = cv_next[0]
                    cv_next[0] += 1
                    for src, dst in ((wg_d, wgb_d), (wu_d, wub_d), (wd_d, wdb_d)):
                        s2_ = src[e].rearrange("a b -> (a b)").rearrange("(p f) -> p f", p=128)
                        d2_ = dst[e].rearrange("a b -> (a b)").rearrange("(p f) -> p f", p=128)
                        dma("pool", d2_, s2_, sem_cv, [], [Buf("cv")])

            for i in range(NT):
                gi = NPRE + i
                if i + 1 < NT:
                    load_x(x_own, i + 1, (gi + 1) % 3)
                convert_experts(((i + 1) * 32 + NT - 1) // NT)
                thr = []
                if i > 0:
                    thr.append(thr_Y(i - 1, (gi - 1) % 3))
                thr.append(thr_XF(i, gi % 3))
                run_threads(thr)
            run_threads([thr_Y(NT - 1, (NPRE + NT - 1) % 3)])
            P.barrier()
            P.emit_block()
        if mode in (1, 2, 3, 4):
            return nc

        with ExitStack() as s2:
            NWB = 3
            wg = [SB(s2, "wg%d" % k, [128, 8, 512], BF16) for k in range(NWB)]
            wu = [SB(s2, "wu%d" % k, [128, 8, 512], BF16) for k in range(NWB)]
            wd = [SB(s2, "wd%d" % k, [128, 4, D], BF16) for k in range(NWB)]
            b_w = [Buf("w%d" % k) for k in range(NWB)]
            sem_e = [P.newsem() for _ in range(NWB)]
            xg = [SB(s2, "xg%d" % k, [128, NB, D], BF16) for k in range(2)]; b_xg = [Buf("xg0"), Buf("xg1")]
            sem_xg = [P.newsem(), P.newsem()]
            xgTs = [SB(s2, "xgT%d" % k, [128, 8, CAP], BF16) for k in range(2)]; b_xgTs = [Buf("xgT0"), Buf("xgT1")]
            sg = [SB(s2, "sg%d" % k, [128, CAP], F32) for k in range(2)]; b_sg = [Buf("sg0"), Buf("sg1")]
            hT = SB(s2, "hT", [128, 4, CAP], BF16); b_hT = Buf("hT")
            ysb = [SB(s2, "ysb%d" % k, [128, D], F32) for k in range(2)]; b_ysb = [Buf("ysb0"), Buf("ysb1")]
            sem_y = [P.newsem(), P.newsem()]
            NP3 = 4
            x1t = [SB(s2, "x1t%d" % k, [128, D], F32) for k in range(NP3)]; b_x1t = [Buf("x1t%d" % k) for k in range(NP3)]
            yA = [SB(s2, "yA%d" % k, [128, D], F32) for k in range(NP3)]; b_yA = [Buf("yA%d" % k) for k in range(NP3)]
            yB = [SB(s2, "yB%d" % k, [128, D], F32) for k in range(NP3)]; b_yB = [Buf("yB%d" % k) for k in range(NP3)]
            sem_l = [P.newsem() for _ in range(NP3)]
            sem_ga = [P.newsem() for _ in range(NP3)]
            sem_gb = [P.newsem() for _ in range(NP3)]
            sem_o = [P.newsem() for _ in range(NP3)]
            tp2 = PS(s2, "tp2", [128, 1024], BF16); b_tp2 = Buf("tp2", True)
            pg_ = [PS(s2, "pg%d" % k, [128, 512], F32) for k in range(2)]; b_pg = [Buf("pg0", True), Buf("pg1", True)]
            pu_ = [PS(s2, "pu%d" % k, [128, 512], F32) for k in range(2)]; b_pu = [Buf("pu0", True), Buf("pu1", True)]
            py_ = [PS(s2, "py%d" % k, [128, 512], F32) for k in range(2)]; b_py = [Buf("py0", True), Buf("py1", True)]

            def load_w(e):
                k = e % NWB
                hws = []
                dma("sp", wg[k][:], wgb_d[e].rearrange("(c p) n -> p c n", p=128), sem_e[k], [], [b_w[k]])
                P.dma("sp", lambda en, k=k, e=e: en.dma_start(out=wu[k][:], in_=wub_d[e].rearrange("(c p) n -> p c n", p=128)), sem_e[k], [], [Buf("t")])
                h = P.dma("sp", lambda en, k=k, e=e: en.dma_start(out=wd[k][:], in_=wdb_d[e].rearrange("(c p) n -> p c n", p=128)), sem_e[k], [], [Buf("t")])
                b_w[k].last_w = h

            def load_xg(e):
                k = e % 2
                dma("sp", xg[k][:], xbuf_d[e * CAP:(e + 1) * CAP, :].rearrange("(b p) d -> p b d", p=128), sem_xg[k], [b_xbuf], [b_xg[k]])

            load_w(0)
            load_xg(0)
            load_w(1)
            ycount = [0]

            def ex_T(e):
                kx = e % 2
                xgT = xgTs[kx]
                for sb_ in range(NB):
                    for c in range(8):
                        tr(tp2[:, c * 128:(c + 1) * 128], xg[kx][:, sb_, c * 128:(c + 1) * 128], ident_b, [b_xg[kx], b_cb], [b_tp2])
                    cp("dve" if sb_ % 2 == 0 else "act", xgT[:, :, sb_ * 128:(sb_ + 1) * 128], tp2[:].rearrange("p (c t) -> p c t", t=128), [b_tp2], [b_xgTs[kx]])

            def ex_H(e):
                k = e % NWB
                xgT = xgTs[e % 2]; b_xgT = b_xgTs[e % 2]
                for fc in range(4):
                    a_ = fc % 2
                    for c in range(8):
                        mm(pg_[a_][:, 0:CAP], wg[k][:, c, fc * 128:(fc + 1) * 128], xgT[:, c, :], c == 0, c == 7, [b_w[k], b_xgT], [b_pg[a_]])
                    for c in range(8):
                        mm(pu_[a_][:, 0:CAP], wu[k][:, c, fc * 128:(fc + 1) * 128], xgT[:, c, :], c == 0, c == 7, [b_w[k], b_xgT], [b_pu[a_]])
                    act(sg[a_][:], pg_[a_][:, 0:CAP], AF.Silu, [b_pg[a_]], [b_sg[a_]])
                    tt("dve", hT[:, fc, :], sg[a_][:], pu_[a_][:, 0:CAP], ALU.mult, [b_sg[a_], b_pu[a_]], [b_hT])

            def ex_Y(e):
                k = e % NWB
                for sb_ in range(NB):
                    ky = ycount[0] % 2
                    ycount[0] += 1
                    for nh in range(2):
                        for fc in range(4):
                            mm(py_[nh][:], hT[:, fc, sb_ * 128:(sb_ + 1) * 128], wd[k][:, fc, nh * 512:(nh + 1) * 512], fc == 0, fc == 3,
                               [b_hT, b_w[k]], [b_py[nh]])
                        cp("act" if nh == 0 else "dve", ysb[ky][:, nh * 512:(nh + 1) * 512], py_[nh][:], [b_py[nh]], [b_ysb[ky]])
                    r0 = e * CAP + sb_ * 128
                    dma("sp", ybuf_d[r0:r0 + 128, :], ysb[ky][:], sem_y[ky], [b_ysb[ky]], [Buf("yst%d" % ky)])

            ex_T(0)
            for e in range(32):
                if e + 2 < 32:
                    load_w(e + 2)
                if e + 1 < 32:
                    load_xg(e + 1)
                ex_H(e)
                if e + 1 < 32:
                    ex_T(e + 1)
                ex_Y(e)
            P.barrier()

            def p3_fetch(i):
                k = i % NP3
                dma("sp", x1t[k][:], out_d[i * 128:(i + 1) * 128, :], sem_l[k], [b_out], [b_x1t[k]])
                offA = dest[:, 0, i:i + 1]
                offB = dest[:, 1, i:i + 1]
                P.dma("pool", lambda en, off=offA, dst=yA[k]: en.indirect_dma_start(
                    out=dst[:, :], out_offset=None, in_=ybuf_d, in_offset=bass.IndirectOffsetOnAxis(ap=off, axis=0)),
                    sem_ga[k], [b_dest], [b_yA[k]])
                P.dma("pool", lambda en, off=offB, dst=yB[k]: en.indirect_dma_start(
                    out=dst[:, :], out_offset=None, in_=ybuf_d, in_offset=bass.IndirectOffsetOnAxis(ap=off, axis=0)),
                    sem_gb[k], [b_dest], [b_yB[k]])

            for i in range(min(NP3 - 1, NT)):
                p3_fetch(i)
            for i in range(NT):
                k = i % NP3
                if i + NP3 - 1 < NT:
                    p3_fetch(i + NP3 - 1)
                stt("dve", x1t[k][:], yA[k][:], gate[:, 0, i:i + 1], x1t[k][:], ALU.mult, ALU.add, [b_yA[k], b_gate, b_x1t[k]], [b_x1t[k]])
                stt("dve", x1t[k][:], yB[k][:], gate[:, 1, i:i + 1], x1t[k][:], ALU.mult, ALU.add, [b_yB[k], b_gate, b_x1t[k]], [b_x1t[k]])
                dma("act", out_d[i * 128:(i + 1) * 128, :], x1t[k][:], sem_o[k], [b_x1t[k]], [Buf("ost%d" % k)])
            P.barrier()
            P.emit_block()
    return nc


def _consts(CAP):
    cf = np.zeros((128, K_END), np.float32)
    cf[:, K_ID:K_ID + 128] = np.eye(128, dtype=np.float32)
    s = np.arange(128)[:, None]
    t = np.arange(128)[None, :]
    cf[:, K_TRI:K_TRI + 128] = ((s <= t) & ((s // 64) == (t // 64))).astype(np.float32)
    cf[63, K_SEL2] = 1.0
    cf[127, K_SEL2 + 1] = 1.0
    invf = (500000.0 ** (-np.arange(0, 16, 2, dtype=np.float32) / 16)).astype(np.float32)
    cf[:, K_INVF:K_INVF + 8] = invf[None, :]
    cf[:, K_IOTA:K_IOTA + 32] = (np.arange(32, dtype=np.float32) * CAP)[None, :]
    cb = np.zeros((128, B_END), np.float32)
    cb[:, B_ID:B_ID + 128] = np.eye(128, dtype=np.float32)
    cb[:, B_STRI:B_STRI + 128] = (s < t).astype(np.float32)
    cb[:, B_ONES:B_ONES + 128] = 1.0
    mcur = np.where(s <= t, 0.0, NEG).astype(np.float32)
    mprev = np.where(s > t, 0.0, NEG).astype(np.float32)
    cb[:, B_MCUR:B_MCUR + 512] = np.tile(mcur, (1, 4))
    cb[:, B_MPREV:B_MPREV + 512] = np.tile(mprev, (1, 4))
    return cf, cb, mprev


def make_in_maps(inp, NT, CAP, n_cores=8):
    x = np.asarray(inp["x"], np.float32)
    B, S, _ = x.shape
    T = NT * 128
    assert S == 2 * T and B * 2 == n_cores
    pos = np.asarray(inp["positions"]).astype(np.int32)
    cf, cb0, mprev = _consts(CAP)
    w_in = np.asarray(inp["w_in"], np.float32)[0]
    qperm = np.concatenate([np.arange(64) + (g * 4 + j) * 64 for j in range(4) for g in range(2)])
    w_in_p = np.ascontiguousarray(np.concatenate([w_in[:, :512][:, qperm], w_in[:, 512:]], axis=1))
    f = lambda k: np.ascontiguousarray(np.asarray(inp[k], np.float32)[0])
    pp = np.zeros((128, Q_END), np.float32)
    pp[:, Q_N1C:Q_N1C + 8] = f("norm1_w").reshape(8, 128).T
    pp[:, Q_N2C:Q_N2C + 8] = f("norm2_w").reshape(8, 128).T
    pp[:, Q_N2:Q_N2 + 1024] = f("norm2_w")[None, :]
    pp[:, Q_QW:Q_QW + 512] = np.tile(f("q_norm_w"), 8)[None, :]
    pp[:, Q_KW:Q_KW + 128] = np.tile(f("k_norm_w"), 2)[None, :]
    pp[:, Q_HW:Q_HW + 512] = np.tile(f("hgrn_norm_w"), 4)[None, :]
    hlb = np.asarray(inp["hgrn_lower_bounds"], np.float32)
    hlbt = np.ascontiguousarray(np.tile(hlb.reshape(1, 1024), (128, 1)))
    pp[:, Q_SINK:Q_SINK + 8] = f("attn_sinks")[None, :]
    pp[:, Q_BRT:Q_BRT + 4] = f("b_router_group")[None, :]
    pp[:, Q_BRT + 4:Q_BRT + 36] = f("b_router_expert")[None, :]
    w_r = np.ascontiguousarray(np.concatenate([f("w_router_group"), f("w_router_expert")], axis=1))
    shared = {
        "cf32": cf, "pp32": pp, "hlb": hlbt, "w_in": w_in_p, "w_ba": f("w_branch_attn"), "w_bh": f("w_branch_hgrn"),
        "w_out": f("w_out"), "w_r": w_r, "w_g": f("w_gate_experts"), "w_u": f("w_up_experts"), "w_d": f("w_down_experts"),
    }
    maps = []
    for c in range(n_cores):
        b, half = c // 2, c % 2
        cb = cb0.copy()
        if half == 1:
            cb[:, B_MHALO:B_MHALO + 512] = np.tile(mprev, (1, 4))
            x_pre = np.ascontiguousarray(x[b, 0:T])
            halo_pos = pos[b, T - 128:T]
        else:
            cb[:, B_MHALO:B_MHALO + 512] = NEG
            x_pre = np.zeros((T, D), np.float32)
            halo_pos = np.zeros(128, np.int32)
        own = slice(half * T, (half + 1) * T)
        pt = np.concatenate([pos[b, own].reshape(NT, 128).T, halo_pos[:, None]], axis=1).astype(np.int32)
        m = dict(shared)
        m.update({"x_own": np.ascontiguousarray(x[b, own]), "x_pre": x_pre, "pos": np.ascontiguousarray(pt), "cb16": cb})
        maps.append(m)
    return maps


_NC_CACHE = {}


def run(inp, NT, CAP, mode=0, raw=False):
    key = (NT, CAP, mode)
    if key not in _NC_CACHE:
        _NC_CACHE[key] = build(NT, CAP, mode=mode)
    nc = _NC_CACHE[key]
    maps = make_in_maps(inp, NT, CAP)
    if mode != 0:
        for m in maps:
            for k in ("w_g", "w_u", "w_d"):
                m[k] = np.ascontiguousarray(m[k][:1])
    res = run_bass_kernel_spmd(nc, maps, core_ids=list(range(8)))
    if raw:
        return res.results
    x = np.asarray(inp["x"])
    B, S, _ = x.shape
    T = NT * 128
    out = np.empty((B, S, D), np.float32)
    for c in range(8):
        b, half = c // 2, c % 2
        out[b, half * T:(half + 1) * T] = res.results[c]["out"]
    return out


def kernel(**inputs):
    return run(inputs, 32, 384)
```

```python
import math
import os
from contextlib import ExitStack
import numpy as np
import concourse.bass as bass
import concourse.mybir as mybir
from concourse.bass_utils import run_bass_kernel_spmd

F32 = mybir.dt.float32
BF16 = mybir.dt.bfloat16
I32 = mybir.dt.int32
ALU = mybir.AluOpType
AF = mybir.ActivationFunctionType
AX = mybir.AxisListType

D = 1024
NEG = -30000.0
EPS = 1e-6
TWO_PI = 2.0 * math.pi


class Buf:
    __slots__ = ("name", "last_w", "readers", "excl")

    def __init__(self, name, excl=False):
        self.name = name
        self.last_w = None
        self.readers = []
        self.excl = excl


class DmaSem:
    def __init__(self, key):
        self.key = key
        self.count = 0


class Prog:
    ENGS = ("pe", "act", "dve", "pool", "sp")
    EMAP = {"pe": "tensor", "act": "scalar", "dve": "vector", "pool": "gpsimd", "sp": "sync"}

    def __init__(self, nc, stack, n_dma_sems=48):
        self.nc = nc
        self.ops = {e: [] for e in self.ENGS}
        self.n = {e: 0 for e in self.ENGS}
        self.waited = {e: {} for e in self.ENGS}
        self.dma_sems = [DmaSem("d%d" % i) for i in range(n_dma_sems)]
        self.sems = {}
        for e in self.ENGS:
            self.sems[e] = stack.enter_context(nc.semaphore("s_" + e))
        for d in self.dma_sems:
            self.sems[d.key] = stack.enter_context(nc.semaphore("s_" + d.key))
        self._next = 0
        self.tfree = {e: 0.0 for e in self.ENGS}
        self.hfin = {}
        self.step_max = 0.0
        self.act_grp = None

    def _time(self, eng, deps, h, cost):
        t = self.tfree[eng]
        for d in deps:
            f = self.hfin.get(d)
            if f is not None:
                f = f + (0.05 if d[0] == eng else 0.2)
                if f > t:
                    t = f
        fin = t + cost
        if h[0] in self.ENGS:
            self.tfree[eng] = fin
        else:
            self.tfree[eng] = t + 0.1
        self.hfin[h] = fin
        if fin > self.step_max:
            self.step_max = fin

    def newsem(self):
        s = self.dma_sems[self._next]
        self._next += 1
        return s

    def _deps(self, eng, reads, writes):
        deps = set()
        for b in reads:
            if b.last_w is not None:
                deps.add(b.last_w)
        for b in writes:
            if b.last_w is not None:
                deps.add(b.last_w)
            deps.update(b.readers)
        w = self.waited[eng]
        best = {}
        for (sk, v) in deps:
            if eng == "pe" and sk == "pe":
                continue
            if w.get(sk, 0) < v and best.get(sk, 0) < v:
                best[sk] = v
        for sk, v in best.items():
            self.ops[eng].append(("wait", sk, v))
            w[sk] = v
        return deps

    def _mark(self, h, reads, writes):
        for b in reads:
            b.readers.append(h)
            if len(b.readers) > 64:
                b.readers = b.readers[-48:]
        for b in writes:
            b.last_w = h
            b.readers = []

    def op(self, eng, fn, reads=(), writes=(), cost=0.3):
        ex = [b for b in reads if b.excl and b not in writes]
        if ex:
            writes = list(writes) + ex
        deps = self._deps(eng, reads, writes)
        self.n[eng] += 1
        h = (eng, self.n[eng])
        self.ops[eng].append(("op", fn))
        self._time(eng, deps, h, cost)
        self._mark(h, reads, writes)
        return h

    def dma(self, eng, fn, sem, reads=(), writes=(), cost=3.0):
        deps = self._deps(eng, reads, writes)
        sem.count += 16
        h = (sem.key, sem.count)
        self.ops[eng].append(("dma", fn, sem.key))
        self._time(eng, deps, h, cost)
        self._mark(h, reads, writes)
        return h

    def wait_all(self, eng, bufs):
        self._deps(eng, bufs, ())

    def barrier(self):
        for e in self.ENGS:
            w = self.waited[e]
            for f in self.ENGS:
                if f != e and self.n[f] > w.get(f, 0):
                    self.ops[e].append(("wait", f, self.n[f]))
                    w[f] = self.n[f]
            for d in self.dma_sems:
                if d.count > w.get(d.key, 0):
                    self.ops[e].append(("wait", d.key, d.count))
                    w[d.key] = d.count

    def emit_block(self):
        sems = self.sems
        with self.nc.Block() as block:
            def mk(e):
                items = self.ops[e]

                def body(engobj):
                    for item in items:
                        if item[0] == "wait":
                            engobj.wait_ge(sems[item[1]], item[2])
                        elif item[0] == "op":
                            item[1](engobj).then_inc(sems[e], 1)
                        else:
                            item[1](engobj).then_inc(sems[item[2]], 16)
                return body
            for e in self.ENGS:
                if self.ops[e]:
                    getattr(block, self.EMAP[e])(mk(e))
        self.ops = {e: [] for e in self.ENGS}


C_Q, C_KV, C_QH, C_FH, C_IH, C_GH, C_ZA, C_ZB = 0, 512, 768, 1280, 1792, 2304, 2816, 3840
NIN = 4864
K_ID, K_TRI, K_SEL2, K_INVF, K_IOTA, K_END = 0, 128, 256, 258, 266, 298
B_ID, B_STRI, B_ONES, B_MCUR, B_MPREV, B_MHALO, B_END = 0, 128, 256, 384, 896, 1408, 1920
Q_N2, Q_QW, Q_KW, Q_HW, Q_SINK, Q_BRT, Q_N1C, Q_N2C, Q_END = 0, 1024, 1536, 1664, 2176, 2184, 2220, 2228, 2236


def build(NT=32, CAP=384, NPRE=None, mode=0):
    if NPRE is None:
        NPRE = NT
    T = NT * 128
    NSLOT = 32 * CAP
    NB = CAP // 128
    nc = bass.Bass("TRN2", target_bir_lowering=False)

    def din(name, shape, dt=F32):
        return nc.dram_tensor(name, shape, dt, kind="ExternalInput").ap()

    x_own = din("x_own", [T, D])
    x_pre = din("x_pre", [NPRE * 128, D])
    pos_d = din("pos", [128, NT + 1], I32)
    cf_d = din("cf32", [128, K_END])
    cb_d = din("cb16", [128, B_END])
    pp_d = din("pp32", [128, Q_END])
    hlb_d = din("hlb", [128, 1024])
    win_d = din("w_in", [D, NIN])
    wba_d = din("w_ba", [512, D])
    wbh_d = din("w_bh", [512, D])
    wout_d = din("w_out", [D, D])
    wr_d = din("w_r", [D, 36])
    NEX = 32 if mode == 0 else 1
    wg_d = din("w_g", [NEX, D, 512])
    wu_d = din("w_u", [NEX, D, 512])
    wd_d = din("w_d", [NEX, 512, D])
    out_d = nc.dram_tensor("out", [T, D], F32, kind="ExternalOutput").ap()
    xbuf_d = nc.dram_tensor("xbuf", [NSLOT, D], BF16, kind="Internal").ap()
    ybuf_d = nc.dram_tensor("ybuf", [NSLOT, D], F32, kind="Internal").ap()
    wgb_d = nc.dram_tensor("wgb", [NEX, D, 512], BF16, kind="Internal").ap()
    wub_d = nc.dram_tensor("wub", [NEX, D, 512], BF16, kind="Internal").ap()
    wdb_d = nc.dram_tensor("wdb", [NEX, 512, D], BF16, kind="Internal").ap()

    with ExitStack() as top:
        P = Prog(nc, top)

        def SB(st, name, shape, dt):
            return st.enter_context(nc.sbuf_tensor(name, shape, dt))

        def PS(st, name, shape, dt):
            return st.enter_context(nc.psum_tensor(name, shape, dt))

        def nfree(ap):
            n = 1
            for d in ap.shape[1:]:
                n *= d
            return n

        def ecost(eng, ap):
            n = nfree(ap)
            if eng == "pool":
                return 0.12 + n * 0.0022
            if eng == "act":
                return 0.2 + n * 0.00095
            return 0.07 + n * 0.00123

        AGRP = {AF.Exp: 1, AF.Ln: 1, AF.Sigmoid: 2, AF.Silu: 3, AF.Sin: 4}

        def mm(out, lhsT, rhs, start, stop, r, w):
            c = max(0.064, 0.00042 * nfree(rhs))
            if lhsT.dtype == F32:
                c *= 4
            return P.op("pe", lambda e: e.matmul(out=out, lhsT=lhsT, rhs=rhs, start=start, stop=stop), r, w, cost=c)

        def tr(out, in_, ident, r, w):
            c = 0.11 * (4 if in_.dtype == F32 else 1)
            return P.op("pe", lambda e: e.transpose(out=out, in_=in_, identity=ident), r, w, cost=c)

        def act(out, in_, func, r, w, **kw):
            c = ecost("act", in_)
            g = AGRP.get(func)
            if g is not None and g != P.act_grp:
                c += 1.3
                P.act_grp = g
            return P.op("act", lambda e: e.activation(out=out, in_=in_, func=func, **kw), r, w, cost=c)

        def tt(eng, out, in0, in1, op, r, w):
            return P.op(eng, lambda e: e.tensor_tensor(out=out, in0=in0, in1=in1, op=op), r, w, cost=ecost(eng, out))

        def ts(eng, out, in0, s1, s2, op0, op1, r, w):
            if op1 is None:
                return P.op(eng, lambda e: e.tensor_scalar(out=out, in0=in0, scalar1=s1, scalar2=None, op0=op0), r, w, cost=ecost(eng, out))
            return P.op(eng, lambda e: e.tensor_scalar(out=out, in0=in0, scalar1=s1, scalar2=s2, op0=op0, op1=op1), r, w, cost=ecost(eng, out))

        def stt(eng, out, in0, scalar, in1, op0, op1, r, w):
            return P.op(eng, lambda e: e.scalar_tensor_tensor(out=out, in0=in0, scalar=scalar, in1=in1, op0=op0, op1=op1), r, w, cost=ecost(eng, out))

        def cp(eng, out, in_, r, w):
            if eng == "act":
                return P.op("act", lambda e: e.copy(out=out, in_=in_), r, w, cost=ecost(eng, out))
            return P.op(eng, lambda e: e.tensor_copy(out=out, in_=in_), r, w, cost=ecost(eng, out))

        def red(eng, out, in_, op, r, w):
            return P.op(eng, lambda e: e.tensor_reduce(out=out, in_=in_, axis=AX.X, op=op), r, w, cost=ecost(eng, in_))

        def mset(eng, ap, val, w):
            return P.op(eng, lambda e: e.memset(ap, val), (), w, cost=ecost(eng, ap))

        def dma(eng, out, in_, sem, r, w):
            return P.dma(eng, lambda e: e.dma_start(out=out, in_=in_), sem, r, w)

        def bc(ap, shape, axis):
            return ap.unsqueeze(axis).to_broadcast(shape)

        idb = SB(top, "idb", [128, B_END], BF16); b_cb = Buf("cb")
        cf = SB(top, "cf", [128, K_END], F32); b_cf = Buf("cf")
        dest = SB(top, "dest", [128, 2, NT], I32); b_dest = Buf("dest")
        gate = SB(top, "gate", [128, 2, NT], F32); b_gate = Buf("gate")
        b_xbuf = Buf("xbuf"); b_ybuf = Buf("ybuf"); b_out = Buf("out")
        ident_f = cf[:, K_ID:K_ID + 128]
        tri_f = cf[:, K_TRI:K_TRI + 128]
        sel2 = cf[:, K_SEL2:K_SEL2 + 2]
        invf = cf[:, K_INVF:K_INVF + 8]
        iotae = cf[:, K_IOTA:K_IOTA + 32]
        ident_b = idb[:, B_ID:B_ID + 128]
        stri_b = idb[:, B_STRI:B_STRI + 128]
        ones_b = idb[:, B_ONES:B_ONES + 128]
        mask_cur = idb[:, B_MCUR:B_MCUR + 512]
        mask_prev = idb[:, B_MPREV:B_MPREV + 512]
        mask_halo = idb[:, B_MHALO:B_MHALO + 512]

        sem_c = [P.newsem() for _ in range(4)]
        dma("sp", cf[:], cf_d, sem_c[0], [], [b_cf])
        dma("pool", idb[:], cb_d, sem_c[1], [], [b_cb])

        with ExitStack() as s1:
            Wp = SB(s1, "Wp", [128, 8, NIN], BF16); b_Wp = Buf("Wp")
            wba = SB(s1, "wba", [128, 4, D], BF16); b_wba = Buf("wba")
            wbh = SB(s1, "wbh", [128, 4, D], BF16); b_wbh = Buf("wbh")
            wout = SB(s1, "wout", [128, 8, D], BF16); b_wout = Buf("wout")
            wr = SB(s1, "wr", [128, 8, 36], F32); b_wr = Buf("wr")
            pp = SB(s1, "pp", [128, Q_END], F32); b_pp = Buf("pp")
            lbt = SB(s1, "lbt", [128, 2, 512], F32); b_lb = Buf("lb")
            esink = SB(s1, "esink", [128, 8], F32); b_esink = Buf("esink")
            posi = SB(s1, "posi", [128, NT + 1], I32); b_posi = Buf("posi")
            NA = (NT + 1) * 8
            trig = SB(s1, "trig", [128, 2, NA], F32); b_trig = Buf("trig")
            cst = SB(s1, "cst", [128, 4], F32); b_cst = Buf("cst")
            cos_t = trig[:, 0, :].rearrange("p (i j) -> p i j", j=8)
            sin_t = trig[:, 1, :].rearrange("p (i j) -> p i j", j=8)

            n2w = pp[:, Q_N2:Q_N2 + 1024]
            qw = pp[:, Q_QW:Q_QW + 512]
            kw = pp[:, Q_KW:Q_KW + 128]
            hw = pp[:, Q_HW:Q_HW + 512]
            brt = pp[:, Q_BRT:Q_BRT + 36]

            dma("sp", pp[:], pp_d, sem_c[2], [], [b_pp])
            dma("sp", posi[:], pos_d, sem_c[3], [], [b_posi])
            WG = [(C_FH, C_GH), (C_KV, C_QH), (C_Q, C_KV), (C_QH, C_FH), (C_GH, C_ZA), (C_ZA, NIN)]
            b_Wg = [Buf("Wg%d" % g) for g in range(len(WG))]
            win3 = win_d.rearrange("(c p) n -> p c n", p=128)
            for g, (c0, c1) in enumerate(WG):
                dma("pool", Wp[:, :, c0:c1], win3[:, :, c0:c1], P.newsem(), [], [b_Wg[g]])

            def wbuf(c0):
                for g, (a0, a1) in enumerate(WG):
                    if a0 <= c0 < a1:
                        return b_Wg[g]
                raise ValueError(c0)
            sem_w = P.newsem()
            dma("pool", wba[:], wba_d.rearrange("(c p) n -> p c n", p=128), sem_w, [], [Buf("wtmp")])
            dma("pool", wbh[:], wbh_d.rearrange("(c p) n -> p c n", p=128), sem_w, [], [Buf("wtmp")])
            hW = dma("pool", wout[:], wout_d.rearrange("(c p) n -> p c n", p=128), sem_w, [], [Buf("wtmp")])
            for b in (b_wba, b_wbh, b_wout):
                b.last_w = hW
            sem_wr = P.newsem()
            dma("sp", wr[:], wr_d.rearrange("(c p) n -> p c n", p=128), sem_wr, [], [b_wr])

            xt = [SB(s1, "xt%d" % k, [128, D], F32) for k in range(3)]; b_xt = [Buf("xt%d" % k) for k in range(3)]
            sem_x = [P.newsem() for _ in range(3)]
            junkF = SB(s1, "junkF", [128, D], BF16); b_junkF = Buf("junkF")
            junkY = SB(s1, "junkY", [128, D], BF16); b_junkY = Buf("junkY")
            stF = SB(s1, "stF", [128, 8], F32); b_stF = Buf("stF")
            stA = SB(s1, "stA", [128, 64], F32); b_stA = Buf("stA")
            stH = SB(s1, "stH", [128, 16], F32); b_stH = Buf("stH")
            stY = SB(s1, "stY", [128, 8], F32); b_stY = Buf("stY")
            xb = SB(s1, "xb", [128, D], BF16); b_xb = Buf("xb")
            xT = SB(s1, "xT", [128, 8, 128], BF16); b_xT = Buf("xT")
            qa = SB(s1, "qa", [128, 512], F32); b_qa = Buf("qa")
            kv = SB(s1, "kvf", [128, 128], F32); b_kv = Buf("kv")
            r16 = SB(s1, "r16", [128, 6, 8, 8], F32); b_r16 = Buf("r16")
            qr = SB(s1, "qr", [128, 512], BF16); b_qr = Buf("qr")
            kr = SB(s1, "kr", [128, 128], BF16); b_kr = Buf("kr")
            qT = SB(s1, "qT", [128, 4, 128], BF16); b_qT = Buf("qT")
            kTs = [SB(s1, "kT%d" % k, [128, 128], BF16) for k in range(2)]; b_kT = [Buf("kT0"), Buf("kT1")]
            vx = [SB(s1, "vx%d" % k, [128, 2, 80], BF16) for k in range(2)]; b_vx = [Buf("vx0"), Buf("vx1")]
            pTb = [SB(s1, "pT%d" % k, [128, 512], BF16) for k in range(2)]; b_pTb = [Buf("pT0"), Buf("pT1")]
            attn = SB(s1, "attn", [128, 512], BF16); b_attn = Buf("attn")
            aT = SB(s1, "aT", [128, 4, 128], BF16); b_aT = Buf("aT")
            qh = SB(s1, "qh", [128, 512], F32); b_qh = Buf("qh")
            hA = SB(s1, "hA", [128, 512], F32); b_hA = Buf("hA")
            hk = SB(s1, "hk", [128, 512], F32); b_hk = Buf("hk")
            eb = SB(s1, "eb", [128, 512], F32); b_eb = Buf("eb")
            iv = SB(s1, "iv", [128, 512], BF16); b_iv = Buf("iv")
            gs = SB(s1, "gs", [128, 512], F32); b_gs = Buf("gs")
            qt = SB(s1, "qt", [128, 512], BF16); b_qt = Buf("qt")
            kt = SB(s1, "kt", [128, 512], BF16); b_kt = Buf("kt")
            qlh = SB(s1, "qlh", [128, 4, 2, 128], BF16); b_qlh = Buf("qlh")
            ktT = SB(s1, "ktT", [128, 4, 128], BF16); b_ktT = Buf("ktT")
            Am = [SB(s1, "Am%d" % k, [128, 128], BF16) for k in range(2)]; b_Am = [Buf("Am0"), Buf("Am1")]
            S = SB(s1, "S", [128, 4, 128], F32); b_S = Buf("S")
            Sb = [SB(s1, "Sb%d" % k, [128, 4, 128], BF16) for k in range(2)]; b_Sb = [Buf("Sb0"), Buf("Sb1")]
            Dsb = SB(s1, "Dsb", [128, 4, 2], F32); b_Dsb = Buf("Dsb")
            hg = SB(s1, "hg", [128, 512], BF16); b_hg = Buf("hg")
            hgT = SB(s1, "hgT", [128, 4, 128], BF16); b_hgT = Buf("hgT")
            sza = SB(s1, "sza", [128, D], BF16); b_sza = Buf("sza")
            szb = SB(s1, "szb", [128, D], BF16); b_szb = Buf("szb")
            m1 = SB(s1, "m1", [128, D], F32); b_m1 = Buf("m1")
            mixed = SB(s1, "mixed", [128, D], BF16); b_mixed = Buf("mixed")
            mT = SB(s1, "mT", [128, 8, 128], BF16); b_mT = Buf("mT")
            hb = SB(s1, "hb", [128, D], BF16); b_hb = Buf("hb")
            rt = SB(s1, "rt", [128, 256], F32); b_rt = Buf("rt")
            cums = SB(s1, "cums", [128, 32], F32); b_cums = Buf("cums")
            cumsb = SB(s1, "cumsb", [128, 32], BF16); b_cumsb = Buf("cumsb")
            selb = SB(s1, "selb", [128, 32], BF16); b_selb = Buf("selb")
            sem_x1 = P.newsem()
            sem_sc = P.newsem()

            tp = PS(s1, "tp", [128, 1024], BF16); b_tp = Buf("tp", True)
            mmA = PS(s1, "mmA", [128, 512], F32); mmB = PS(s1, "mmB", [128, 512], F32)
            b_mm = [Buf("mmA", True), Buf("mmB", True)]; mmP = [mmA, mmB]
            sc = [PS(s1, "sc0", [128, 512], F32)]; b_sc = [Buf("sc0", True)]
            tpB = PS(s1, "tpB", [128, 1024], BF16); b_tpB = Buf("tpB", True)
            pv = [PS(s1, "pv0", [128, 512], F32), PS(s1, "pv1", [128, 512], F32)]; b_pv = [Buf("pv0", True), Buf("pv1", True)]
            misc = PS(s1, "misc", [128, 512], F32); b_misc = Buf("misc", True)

            mset("dve", qlh[:], 0.0, [b_qlh])
            mset("dve", S[:], 0.0, [b_S])
            mset("dve", Sb[0][:], 0.0, [b_Sb[0]])
            mset("dve", cums[:], 0.0, [b_cums])
            mset("dve", cumsb[:], 0.0, [b_cumsb])
            for k in range(2):
                mset("dve", vx[k][:], 0.0, [b_vx[k]])
                mset("dve", vx[k][:, :, 64:65], 1.0, [b_vx[k]])
            def scale_w(g):
                c0, c1 = WG[g]
                tt("dve", Wp[:, :, c0:c1], Wp[:, :, c0:c1], bc(pp[:, Q_N1C:Q_N1C + 8], [128, 8, c1 - c0], 2), ALU.mult,
                   [b_Wg[g], b_pp], [b_Wg[g]])
            scale_w(0)
            scale_w(1)
            pending_w = [2, 3, 4, 5]
            for c in range(8):
                ts("dve", wr[:, c, :], wr[:, c, :], pp[:, Q_N2C + c:Q_N2C + c + 1], None, ALU.mult, None, [b_wr, b_pp], [b_wr])
            zt = hb[:]; b_zt = b_hb
            ZC = 1024
            mset("pool", zt, 0.0, [b_zt])
            sem_z = P.newsem()
            xz = xbuf_d.rearrange("(p r) d -> p (r d)", p=128)
            tot = (NSLOT // 128) * D
            zchunks = [(k0, min(tot, k0 + ZC)) for k0 in range(0, tot, ZC)]

            def zero_some(n):
                for _ in range(n):
                    if zchunks:
                        k0, k1 = zchunks.pop(0)
                        b_xbuf.last_w = dma("sp", xz[:, k0:k1], zt[:, 0:k1 - k0], sem_z, [b_zt], [Buf("ztmp")])

            mset("dve", cst[:, 0:1], math.pi / 2, [b_cst])
            mset("dve", cst[:, 1:2], EPS, [b_cst])
            dma("sp", m1[:], hlb_d, P.newsem(), [], [b_m1])
            hl = m1[:].rearrange("p (a c) -> p a c", a=2)
            tt("dve", lbt[:, 0, :], hl[:, 0, :], hl[:, 1, :], ALU.subtract, [b_m1], [b_lb])
            act(lbt[:, 0, :], lbt[:, 0, :], AF.Sigmoid, [b_lb], [b_lb])
            ts("dve", lbt[:, 1, :], lbt[:, 0, :], -1.0, 1.0, ALU.mult, ALU.add, [b_lb], [b_lb])
            act(esink[:], pp[:, Q_SINK:Q_SINK + 8], AF.Exp, [b_pp], [b_esink])
            b_tg = Buf("trigtmp")
            tA = xt[1][:, 0:2 * NA].rearrange("p (a n) -> p a n", a=2)
            tB = xt[2][:, 0:2 * NA].rearrange("p (a n) -> p a n", a=2)
            TR = [b_tg, b_xt[1], b_xt[2]]
            trg = {0: tA[:, 0, :], 1: tA[:, 1, :], 2: tB[:, 0, :], 3: tB[:, 1, :], 4: trig[:, 0, :], 5: trig[:, 1, :]}
            posf = trg[0][:, 0:NT + 1]
            cp("dve", posf, posi[:], [b_posi], TR)
            ang = trg[1].rearrange("p (i j) -> p i j", j=8)
            tt("dve", ang, bc(posf, [128, NT + 1, 8], 2), bc(invf, [128, NT + 1, 8], 1), ALU.mult, TR + [b_cf], TR)
            kf = trg[2]
            ts("dve", kf, trg[1], 1.0 / TWO_PI, None, ALU.mult, None, TR, TR)
            ki = trg[3].bitcast(I32)
            cp("dve", ki, kf, TR, TR)
            cp("dve", kf, ki, TR, TR)
            r_ = trg[1]
            stt("dve", r_, kf, -TWO_PI, r_, ALU.mult, ALU.add, TR, TR)
            s4 = trg[2]
            c4 = trg[3]
            act(s4, r_, AF.Sin, TR, TR, scale=0.25)
            act(c4, r_, AF.Sin, TR + [b_cst], TR, scale=0.25, bias=cst[:, 0:1])
            s2 = trg[0]
            c2 = trg[1]
            stt("dve", s2, s4, 2.0, c4, ALU.mult, ALU.mult, TR, TR)
            tt("dve", c2, s4, s4, ALU.mult, TR, TR)
            ts("dve", c2, c2, -2.0, 1.0, ALU.mult, ALU.add, TR, TR)
            stt("dve", trg[5], s2, 2.0, c2, ALU.mult, ALU.mult, TR, [b_trig])
            tt("dve", trg[4], s2, s2, ALU.mult, TR, [b_trig])
            ts("dve", trg[4], trg[4], -2.0, 1.0, ALU.mult, ALU.add, [b_trig], [b_trig])


            mmc = [0]

            def next_mm():
                k = mmc[0] % 2
                mmc[0] += 1
                return mmP[k], b_mm[k]

            def rstd_of(stt_, b_s, col, src_col, scale=1.0):
                act(stt_[:, col], stt_[:, src_col], AF.Ln, [b_s, b_cst], [b_s], bias=cst[:, 1:2], scale=scale)
                act(stt_[:, col], stt_[:, col], AF.Exp, [b_s], [b_s], scale=-0.5)

            def load_x(src, i, slot):
                dma("sp", xt[slot][:], src[i * 128:(i + 1) * 128, :], sem_x[slot], [], [b_xt[slot]])

            rstd1 = stF[:, 1:2]

            def front(slot):
                mset("dve", stF[:, 0:1], 0.0, [b_stF])
                act(junkF[:], xt[slot][:], AF.Square, [b_xt[slot]], [b_junkF, b_stF], scale=1.0 / 32, accum_out=stF[:, 0:1])
                rstd_of(stF, b_stF, slice(1, 2), slice(0, 1))
                cp("act", xb[:], xt[slot][:], [b_xt[slot]], [b_xb])
                for c in range(8):
                    tr(tp[:, c * 128:(c + 1) * 128], xb[:, c * 128:(c + 1) * 128], ident_b, [b_xb, b_cb], [b_tp])
                cp("dve", xT[:].rearrange("p c t -> p (c t)"), tp[:], [b_tp], [b_xT])

            def proj(c0, n):
                ps_, b_ps = next_mm()
                for c in range(8):
                    mm(ps_[:, 0:n], xT[:, c, :], Wp[:, c, c0:c0 + n], c == 0, c == 7, [b_xT, wbuf(c0)], [b_ps])
                return ps_, b_ps

            def qknorm_rope(src, b_src, nh, wtile, dst, b_dst, ti):
                n = nh * 64
                s3 = src.rearrange("p (h d) -> p h d", d=64)
                d3 = dst.rearrange("p (h d) -> p h d", d=64)
                w3 = wtile.rearrange("p (h d) -> p h d", d=64)
                tt("pool", dst, src, src, ALU.mult, [b_src], [b_dst])
                red("dve", stA[:, 8:8 + nh], d3, ALU.add, [b_dst], [b_stA])
                rstd_of(stA, b_stA, slice(16, 16 + nh), slice(8, 8 + nh), scale=1.0 / 64)
                tt("dve", s3, s3, bc(stA[:, 16:16 + nh], [128, nh, 64], 2), ALU.mult, [b_src, b_stA], [b_src])
                tt("dve", dst, src, wtile, ALU.mult, [b_src, b_pp], [b_dst])
                x1 = r16[:, 0, 0:nh, :]; x2 = r16[:, 1, 0:nh, :]
                tt("dve", x1, s3[:, :, 0:8], w3[:, :, 0:8], ALU.mult, [b_src, b_pp], [b_r16])
                tt("dve", x2, s3[:, :, 8:16], w3[:, :, 8:16], ALU.mult, [b_src, b_pp], [b_r16])
                cs = bc(cos_t[:, ti, :], [128, nh, 8], 1)
                sn = bc(sin_t[:, ti, :], [128, nh, 8], 1)
                a_ = r16[:, 2, 0:nh, :]; b_ = r16[:, 3, 0:nh, :]; c_ = r16[:, 4, 0:nh, :]; d_ = r16[:, 5, 0:nh, :]
                tt("dve", a_, x1, cs, ALU.mult, [b_r16, b_trig], [b_r16])
                tt("dve", b_, x2, sn, ALU.mult, [b_r16, b_trig], [b_r16])
                tt("dve", c_, x2, cs, ALU.mult, [b_r16, b_trig], [b_r16])
                tt("dve", d_, x1, sn, ALU.mult, [b_r16, b_trig], [b_r16])
                tt("dve", d3[:, :, 0:8], a_, b_, ALU.subtract, [b_r16], [b_dst])
                tt("dve", d3[:, :, 8:16], c_, d_, ALU.add, [b_r16], [b_dst])

            def kv_proj(slot):
                ps_, b_ps = proj(C_KV, 256)
                act(kv[:], ps_[:, 0:128], AF.Copy, [b_ps, b_stF], [b_kv], scale=rstd1)
                act(vx[slot][:, :, 0:64], ps_[:, 128:256].rearrange("p (m d) -> p m d", d=64), AF.Copy, [b_ps, b_stF], [b_vx[slot]], scale=rstd1)

            def k_finish(ti, slot):
                qknorm_rope(kv[:], b_kv, 2, kw, kr[:], b_kr, ti)
                tr(tp[:, 0:128], kr[:], ident_b, [b_kr, b_cb], [b_tp])
                cp("dve", kTs[slot][:], tp[:, 0:128], [b_tp], [b_kT[slot]])

            HS = [(hA, b_hA, hk, b_hk, eb, b_eb, iv, b_iv, kt, b_kt),
                  (qa, b_qa, qh, b_qh, gs, b_gs, qr, b_qr, attn, b_attn)]

            def hgrn_proj(hs=0):
                hA, b_hA, hk, b_hk, eb, b_eb, iv, b_iv, kt, b_kt = HS[hs]
                ps_, b_ps = proj(C_FH, 512)
                act(hA[:], ps_[:], AF.Sigmoid, [b_ps, b_stF], [b_hA], scale=rstd1)
                ps2, b_ps2 = proj(C_IH, 512)
                act(iv[:], ps2[:], AF.Copy, [b_ps2, b_stF], [b_iv], scale=rstd1)

            def hgrn_chain(hs=0):
                hA, b_hA, hk, b_hk, eb, b_eb, iv, b_iv, kt, b_kt = HS[hs]
                tt("pool", hA[:], hA[:], lbt[:, 1, :], ALU.mult, [b_hA, b_lb], [b_hA])
                tt("pool", hA[:], hA[:], lbt[:, 0, :], ALU.add, [b_hA, b_lb], [b_hA])
                yield
                ts("dve", hk[:], hA[:], -1.0, 1.0, ALU.mult, ALU.add, [b_hA], [b_hk])
                act(hA[:], hA[:], AF.Ln, [b_hA], [b_hA])
                yield
                mm(pv[1][:], tri_f, hA[:], True, True, [b_cf, b_hA], [b_pv[1]])
                act(eb[:], pv[1][:], AF.Exp, [b_pv[1]], [b_eb])
                act(hA[:], pv[1][:], AF.Exp, [b_pv[1]], [b_hA], scale=-1.0)
                yield
                tt("dve", kt[:], hk[:], hA[:], ALU.mult, [b_hk, b_hA], [b_kt])
                for hh in range(4):
                    mm(misc[:, hh * 2:hh * 2 + 2], eb[:, hh * 128:(hh + 1) * 128], sel2, True, True, [b_eb, b_cf], [b_misc])
                cp("dve", Dsb[:].rearrange("p h c -> p (h c)"), misc[:, 0:8], [b_misc], [b_Dsb])
                yield

            def state_update(c, dst, hs=0):
                hA, b_hA, hk, b_hk, eb, b_eb, iv, b_iv, kt, b_kt = HS[hs]
                for hh in range(4):
                    mm(misc[:, hh * 128:(hh + 1) * 128], kt[c * 64:(c + 1) * 64, hh * 128:(hh + 1) * 128],
                       iv[c * 64:(c + 1) * 64, hh * 128:(hh + 1) * 128], True, True, [b_kt, b_iv], [b_misc])
                Sf = S[:].rearrange("p h d -> p (h d)")
                tt("dve", Sf, Sf, misc[:], ALU.add, [b_S, b_misc], [b_S])
                tt("dve", S[:], S[:], bc(Dsb[:, :, c], [128, 4, 128], 2), ALU.mult, [b_S, b_Dsb], [b_S])
                cp("act", Sb[dst][:], S[:], [b_S], [b_Sb[dst]])

            flags = set()

            def run_threads(threads):
                active = [[g, 0.0, None] for g in threads]
                while active:
                    cand = [a for a in active if a[2] is None or a[2] in flags]
                    assert cand, "scheduler deadlock"
                    a = cand[0]
                    active.remove(a)
                    active.append(a)
                    a[2] = None
                    P.step_max = 0.0
                    try:
                        r = next(a[0])
                    except StopIteration:
                        active.remove(a)
                        continue
                    if P.step_max > 0.0:
                        a[1] = P.step_max
                    if r is None:
                        continue
                    if isinstance(r, tuple):
                        if r[0] == "need":
                            a[2] = r[1]
                        else:
                            flags.add(r[1])
                    else:
                        active.append([r, a[1], None])

            load_x(x_pre if NPRE > 0 else x_own, 0, 0)
            def thr_PXF(j):
                front(j % 3)
                yield
                hgrn_proj(j % 2)
                yield
                if j == NPRE - 1:
                    kv_proj(1)
                    yield
                    k_finish(NT, 1)

            def thr_PH(j):
                for _ in hgrn_chain(j % 2):
                    yield
                state_update(0, 1, j % 2)
                yield
                state_update(1, 0, j % 2)

            for j in range(NPRE + 1):
                if j < NPRE:
                    if j + 1 < NPRE:
                        load_x(x_pre, j + 1, (j + 1) % 3)
                    else:
                        load_x(x_own, 0, (j + 1) % 3)
                thr = []
                if j > 0:
                    thr.append(thr_PH(j - 1))
                if j < NPRE:
                    thr.append(thr_PXF(j))
                run_threads(thr)
                if pending_w and j >= 3 and j % 2 == 1:
                    scale_w(pending_w.pop(0))
                if j >= 1:
                    zero_some(4)
            while pending_w:
                scale_w(pending_w.pop(0))
            zero_some(len(zchunks))

            def thr_A(i):
                ks = i % 2
                k_finish(i, ks)
                yield
                qknorm_rope(qa[:], b_qa, 8, qw, qr[:], b_qr, i)
                yield
                for j in range(4):
                    tr(tp[:, j * 128:(j + 1) * 128], qr[:, j * 128:(j + 1) * 128], ident_b, [b_qr, b_cb], [b_tp])
                cp("dve", qT[:].rearrange("p j t -> p (j t)"), tp[:, 0:512], [b_tp], [b_qT])
                yield
                for m in range(2):
                    for kb in range(2):
                        ksl = (1 - ks) if kb == 0 else ks
                        msk = (mask_halo if i == 0 else mask_prev) if kb == 0 else mask_cur
                        mm(sc[0][:], ident_b, msk, True, False, [b_cb], [b_sc[0]])
                        mm(sc[0][:], kTs[ksl][m * 64:(m + 1) * 64, :], qT[m * 64:(m + 1) * 64, :, :].rearrange("p j t -> p (j t)"),
                           False, True, [b_kT[ksl], b_qT], [b_sc[0]])
                        act(pTb[kb][:], sc[0][:], AF.Exp, [b_sc[0]], [b_pTb[kb]], scale=0.125)
                        yield
                    for j in range(4):
                        for kb in range(2):
                            ksl = (1 - ks) if kb == 0 else ks
                            mm(pv[0][:, j * 80:(j + 1) * 80], pTb[kb][:, j * 128:(j + 1) * 128], vx[ksl][:, m, :],
                               kb == 0, kb == 1, [b_pTb[kb], b_vx[ksl]], [b_pv[0]])
                    pv3 = pv[0][:, 0:320].rearrange("p (j d) -> p j d", d=80)
                    den = stA[:, 24 + m * 4:28 + m * 4]
                    tt("dve", den, pv3[:, :, 64], esink[:, m * 4:(m + 1) * 4], ALU.add, [b_pv[0], b_esink], [b_stA])
                    P.op("dve", lambda e, den=den: e.reciprocal(out=den, in_=den), [b_stA], [b_stA])
                    tt("dve", attn[:, m * 256:(m + 1) * 256].rearrange("p (j d) -> p j d", d=64), pv3[:, :, 0:64],
                       bc(den, [128, 4, 64], 2), ALU.mult, [b_pv[0], b_stA], [b_attn])
                    yield
                if i > 0:
                    yield ("need", ("aT", i - 1))
                for j in range(4):
                    tr(tp[:, j * 128:(j + 1) * 128], attn[:, j * 128:(j + 1) * 128], ident_b, [b_attn, b_cb], [b_tp])
                cp("dve", aT[:].rearrange("p j t -> p (j t)"), tp[:, 0:512], [b_tp], [b_aT])

            def thr_H(i):
                for _ in hgrn_chain():
                    yield
                tt("dve", qt[:], qh[:], eb[:], ALU.mult, [b_qh, b_eb], [b_qt])
                tt("pool", gs[:], gs[:], hw, ALU.mult, [b_gs, b_pp], [b_gs])
                yield
                for hh in range(4):
                    tr(tpB[:, hh * 128:(hh + 1) * 128], qt[:, hh * 128:(hh + 1) * 128], ident_b, [b_qt, b_cb], [b_tpB])
                    tr(tpB[:, 512 + hh * 128:512 + (hh + 1) * 128], kt[:, hh * 128:(hh + 1) * 128], ident_b, [b_kt, b_cb], [b_tpB])
                tq = tpB[:, 0:512].rearrange("p (h t) -> p h t", t=128)
                cp("dve", qlh[:, :, 0, 0:64], tq[:, :, 0:64], [b_tpB], [b_qlh])
                cp("dve", qlh[:, :, 1, 64:128], tq[:, :, 64:128], [b_tpB], [b_qlh])
                cp("act", ktT[:].rearrange("p h t -> p (h t)"), tpB[:, 512:1024], [b_tpB], [b_ktT])
                yield
                state_update(0, 1)
                yield
                for hh in range(4):
                    a_ = hh % 2
                    sca = misc[:, 128 + a_ * 128:256 + a_ * 128]
                    mm(sca, ktT[:, hh, :], qlh[:, hh, 0, :], True, False, [b_ktT, b_qlh], [b_misc])
                    mm(sca, ktT[:, hh, :], qlh[:, hh, 1, :], False, True, [b_ktT, b_qlh], [b_misc])
                    tt("dve", Am[a_][:], sca, tri_f, ALU.mult, [b_misc, b_cf], [b_Am[a_]])
                    o_ = pv[1][:, hh * 128:(hh + 1) * 128]
                    mm(o_, Am[a_][:], iv[:, hh * 128:(hh + 1) * 128], True, False, [b_Am[a_], b_iv], [b_pv[1]])
                    mm(o_, qlh[:, hh, 0, :], Sb[0][:, hh, :], False, False, [b_qlh, b_Sb[0]], [b_pv[1]])
                    mm(o_, qlh[:, hh, 1, :], Sb[1][:, hh, :], False, True, [b_qlh, b_Sb[1]], [b_pv[1]])
                    yield
                state_update(1, 0)
                yield
                o3 = pv[1][:].rearrange("p (h d) -> p h d", d=128)
                act(hk[:], pv[1][:], AF.Square, [b_pv[1]], [b_hk])
                red("dve", stH[:, 0:4], hk[:].rearrange("p (h d) -> p h d", d=128), ALU.add, [b_hk], [b_stH])
                rstd_of(stH, b_stH, slice(4, 8), slice(0, 4), scale=1.0 / 128)
                tt("dve", qh[:].rearrange("p (h d) -> p h d", d=128), o3, bc(stH[:, 4:8], [128, 4, 128], 2), ALU.mult,
                   [b_pv[1], b_stH], [b_qh])
                yield
                tt("dve", hg[:], qh[:], gs[:], ALU.mult, [b_qh, b_gs], [b_hg])
                if i > 0:
                    yield ("need", ("hgT", i - 1))
                for j in range(4):
                    tr(tpB[:, j * 128:(j + 1) * 128], hg[:, j * 128:(j + 1) * 128], ident_b, [b_hg, b_cb], [b_tpB])
                cp("dve", hgT[:].rearrange("p j t -> p (j t)"), tpB[:, 0:512], [b_tpB], [b_hgT])

            def thr_XF(i, slot):
                front(slot)
                yield
                ps_, b_ps = proj(C_Q, 512)
                act(qa[:], ps_[:], AF.Copy, [b_ps, b_stF], [b_qa], scale=rstd1)
                kv_proj(i % 2)
                yield thr_A(i)
                hgrn_proj()
                yield
                ps_, b_ps = proj(C_QH, 512)
                act(qh[:], ps_[:], AF.Silu, [b_ps, b_stF], [b_qh], scale=rstd1)
                ps_, b_ps = proj(C_GH, 512)
                act(gs[:], ps_[:], AF.Silu, [b_ps, b_stF], [b_gs], scale=rstd1)
                yield thr_H(i)
                if i > 0:
                    yield ("need", ("hgT", i - 1))
                for half in range(2):
                    ps_, b_ps = proj(C_ZA + half * 512, 512)
                    act(sza[:, half * 512:(half + 1) * 512], ps_[:], AF.Sigmoid, [b_ps, b_stF], [b_sza], scale=rstd1)
                    yield
                for half in range(2):
                    ps_, b_ps = proj(C_ZB + half * 512, 512)
                    act(szb[:, half * 512:(half + 1) * 512], ps_[:], AF.Sigmoid, [b_ps, b_stF], [b_szb], scale=rstd1)
                    yield

            def thr_Y(i, slot):
                for half in range(2):
                    hs = slice(half * 512, (half + 1) * 512)
                    for c in range(4):
                        mm(mmP[half][:], aT[:, c, :], wba[:, c, hs], c == 0, c == 3, [b_aT, b_wba], [b_mm[half]])
                    tt("dve", m1[:, hs], mmP[half][:], sza[:, hs], ALU.mult, [b_mm[half], b_sza], [b_m1])
                    yield
                yield ("set", ("aT", i))
                for half in range(2):
                    hs = slice(half * 512, (half + 1) * 512)
                    for c in range(4):
                        mm(mmP[half][:], hgT[:, c, :], wbh[:, c, hs], c == 0, c == 3, [b_hgT, b_wbh], [b_mm[half]])
                    tt("dve", junkY[:, hs], mmP[half][:], szb[:, hs], ALU.mult, [b_mm[half], b_szb], [b_junkY])
                    tt("pool", mixed[:, hs], junkY[:, hs], m1[:, hs], ALU.add, [b_junkY, b_m1], [b_mixed])
                    yield
                yield ("set", ("hgT", i))
                for c in range(8):
                    tr(tpB[:, c * 128:(c + 1) * 128], mixed[:, c * 128:(c + 1) * 128], ident_b, [b_mixed, b_cb], [b_tpB])
                cp("dve", mT[:].rearrange("p c t -> p (c t)"), tpB[:], [b_tpB], [b_mT])
                yield
                for half in range(2):
                    hs = slice(half * 512, (half + 1) * 512)
                    for c in range(8):
                        mm(mmP[half][:], mT[:, c, :], wout[:, c, hs], c == 0, c == 7, [b_mT, b_wout], [b_mm[half]])
                    tt("dve", xt[slot][:, hs], xt[slot][:, hs], mmP[half][:], ALU.add, [b_xt[slot], b_mm[half]], [b_xt[slot]])
                    yield
                dma("sp", out_d[i * 128:(i + 1) * 128, :], xt[slot][:], sem_x1, [b_xt[slot]], [b_out])
                mset("dve", stY[:, 0:1], 0.0, [b_stY])
                act(junkY[:], xt[slot][:], AF.Square, [b_xt[slot]], [b_junkY, b_stY], scale=1.0 / 32, accum_out=stY[:, 0:1])
                rstd_of(stY, b_stY, slice(1, 2), slice(0, 1))
                rstd2 = stY[:, 1:2]
                stt("dve", hb[:], xt[slot][:], rstd2, n2w, ALU.mult, ALU.mult, [b_xt[slot], b_stY, b_pp], [b_hb])
                yield
                x1T = m1[:].rearrange("p (c t) -> p c t", t=128)
                for g4 in range(2):
                    for c in range(4):
                        tr(mmP[g4][:, c * 128:(c + 1) * 128], xt[slot][:, (g4 * 4 + c) * 128:(g4 * 4 + c + 1) * 128], ident_f,
                           [b_xt[slot], b_cf], [b_mm[g4]])
                    cp("act" if g4 == 0 else "dve", m1[:, g4 * 512:(g4 + 1) * 512], mmP[g4][:], [b_mm[g4]], [b_m1])
                    yield
                for c in range(8):
                    mm(mmP[0][:, 0:36], x1T[:, c, :], wr[:, c, :], c == 0, c == 7, [b_m1, b_wr], [b_mm[0]])
                L = rt[:, 0:36]
                stt("dve", L, mmP[0][:, 0:36], rstd2, brt, ALU.mult, ALU.add, [b_mm[0], b_stY, b_pp], [b_rt])
                yield
                R = [b_rt]
                gmax = rt[:, 36:37]; ngmax = rt[:, 37:38]; gsum = rt[:, 38:39]; pg = rt[:, 39:40]
                red("dve", gmax, L[:, 0:4], ALU.max, R, R)
                ts("dve", ngmax, gmax, -1.0, None, ALU.mult, None, R, R)
                mset("dve", gsum, 0.0, R)
                act(rt[:, 40:44], L[:, 0:4], AF.Exp, R, R, bias=ngmax, accum_out=gsum)
                P.op("dve", lambda e, pg=pg, gsum=gsum: e.reciprocal(out=pg, in_=gsum), R, R)
                gm = rt[:, 44:48]
                ts("dve", gm, L[:, 0:4], gmax, None, ALU.is_ge, None, R, R)
                ts("dve", gm, gm, 1e30, -1e30, ALU.mult, ALU.add, R, R)
                yield
                lem = rt[:, 48:80]
                tt("dve", lem.rearrange("p (g j) -> p g j", j=8), L[:, 4:36].rearrange("p (g j) -> p g j", j=8),
                   bc(gm, [128, 4, 8], 2), ALU.add, R, R)
                top8 = rt[:, 80:88]
                P.op("dve", lambda e, top8=top8, lem=lem: e.max(out=top8, in_=lem), R, R)
                sel = rt[:, 88:120]; is1 = rt[:, 120:152]; isB = rt[:, 152:184]
                ts("dve", sel, lem, top8[:, 1:2], None, ALU.is_ge, None, R, R)
                ts("dve", is1, lem, top8[:, 0:1], None, ALU.is_ge, None, R, R)
                tt("dve", isB, sel, is1, ALU.subtract, R, R)
                yield
                dd = rt[:, 184:185]; s1_ = rt[:, 185:186]
                tt("dve", dd, top8[:, 0:1], top8[:, 1:2], ALU.subtract, R, R)
                act(s1_, dd, AF.Sigmoid, R, R)
                tt("dve", gate[:, 0, i:i + 1], pg, s1_, ALU.mult, R, [b_gate])
                tt("dve", gate[:, 1, i:i + 1], pg, gate[:, 0, i:i + 1], ALU.subtract, R + [b_gate], [b_gate])
                cp("dve", selb[:], sel, R, [b_selb])
                yield
                mm(mmP[1][:, 0:32], stri_b, selb[:], True, False, [b_cb, b_selb], [b_mm[1]])
                mm(mmP[1][:, 0:32], ones_b, cumsb[:], False, True, [b_cb, b_cumsb], [b_mm[1]])
                tt("dve", cums[:], cums[:], sel, ALU.add, R + [b_cums], [b_cums])
                cp("dve", cumsb[:], cums[:], [b_cums], [b_cumsb])
                posc = rt[:, 192:224]
                ts("dve", posc, mmP[1][:, 0:32], float(CAP - 1), None, ALU.min, None, [b_mm[1]], R)
                yield
                tt("dve", posc, posc, iotae, ALU.add, R + [b_cf], R)
                tmpm = rt[:, 224:256]
                dA = rt[:, 186:187]; dB = rt[:, 187:188]
                tt("dve", tmpm, posc, is1, ALU.mult, R, R)
                red("dve", dA, tmpm, ALU.add, R, R)
                tt("dve", tmpm, posc, isB, ALU.mult, R, R)
                red("dve", dB, tmpm, ALU.add, R, R)
                cp("dve", dest[:, 0, i:i + 1], dA, R, [b_dest])
                cp("dve", dest[:, 1, i:i + 1], dB, R, [b_dest])
                yield
                for k in range(2):
                    off = dest[:, k, i:i + 1]
                    P.dma("pool", lambda e, off=off: e.indirect_dma_start(
                        out=xbuf_d, out_offset=bass.IndirectOffsetOnAxis(ap=off, axis=0), in_=hb[:, :], in_offset=None),
                        sem_sc, [b_hb, b_dest, b_xbuf], [Buf("scat")])

            sem_cv = P.newsem()
            cv_next = [0]

            def convert_experts(upto):
                while cv_next[0] < min(upto, NEX):
                    e = cv_next[0]
                    cv_next[0] += 1
                    for src, dst in ((wg_d, wgb_d), (wu_d, wub_d), (wd_d, wdb_d)):
                        s2_ = src[e].rearrange("a b -> (a b)").rearrange("(p f) -> p f", p=128)
                        d2_ = dst[e].rearrange("a b -> (a b)").rearrange("(p f) -> p f", p=128)
                        dma("pool", d2_, s2_, sem_cv, [], [Buf("cv")])

            for i in range(NT):
                gi = NPRE + i
                if i + 1 < NT:
                    load_x(x_own, i + 1, (gi + 1) % 3)
                convert_experts(((i + 1) * 32 + NT - 1) // NT)
                thr = []
                if i > 0:
                    thr.append(thr_Y(i - 1, (gi - 1) % 3))
                thr.append(thr_XF(i, gi % 3))
                run_threads(thr)
            run_threads([thr_Y(NT - 1, (NPRE + NT - 1) % 3)])
            P.barrier()
            P.emit_block()
        if mode in (1, 2, 3, 4):
            return nc

        with ExitStack() as s2:
            NWB = 2
            wg = [SB(s2, "wg%d" % k, [128, 8, 512], BF16) for k in range(NWB)]
            wu = [SB(s2, "wu%d" % k, [128, 8, 512], BF16) for k in range(NWB)]
            wd = [SB(s2, "wd%d" % k, [128, 4, D], BF16) for k in range(NWB)]
            b_w = [Buf("w%d" % k) for k in range(NWB)]
            sem_e = [P.newsem() for _ in range(NWB)]
            xg = [SB(s2, "xg%d" % k, [128, NB, D], BF16) for k in range(2)]; b_xg = [Buf("xg0"), Buf("xg1")]
            sem_xg = [P.newsem(), P.newsem()]
            xgT = SB(s2, "xgT", [128, 8, CAP], BF16); b_xgT = Buf("xgT")
            sg = [SB(s2, "sg%d" % k, [128, CAP], F32) for k in range(2)]; b_sg = [Buf("sg0"), Buf("sg1")]
            hT = SB(s2, "hT", [128, 4, CAP], BF16); b_hT = Buf("hT")
            ysb = [SB(s2, "ysb%d" % k, [128, D], F32) for k in range(2)]; b_ysb = [Buf("ysb0"), Buf("ysb1")]
            sem_y = [P.newsem(), P.newsem()]
            NP3 = 4
            x1t = [SB(s2, "x1t%d" % k, [128, D], F32) for k in range(NP3)]; b_x1t = [Buf("x1t%d" % k) for k in range(NP3)]
            yA = [SB(s2, "yA%d" % k, [128, D], F32) for k in range(NP3)]; b_yA = [Buf("yA%d" % k) for k in range(NP3)]
            yB = [SB(s2, "yB%d" % k, [128, D], F32) for k in range(NP3)]; b_yB = [Buf("yB%d" % k) for k in range(NP3)]
            sem_l = [P.newsem() for _ in range(NP3)]
            sem_ga = [P.newsem() for _ in range(NP3)]
            sem_gb = [P.newsem() for _ in range(NP3)]
            sem_o = [P.newsem() for _ in range(NP3)]
            tp2s = [PS(s2, "tp2%d" % k, [128, 1024], BF16) for k in range(2)]; b_tp2s = [Buf("tp20", True), Buf("tp21", True)]
            tcount = [0]
            pg_ = [PS(s2, "pg%d" % k, [128, 512], F32) for k in range(2)]; b_pg = [Buf("pg0", True), Buf("pg1", True)]
            pu_ = [PS(s2, "pu%d" % k, [128, 512], F32) for k in range(2)]; b_pu = [Buf("pu0", True), Buf("pu1", True)]
            py_ = [PS(s2, "py%d" % k, [128, 512], F32) for k in range(2)]; b_py = [Buf("py0", True), Buf("py1", True)]

            def load_w(e):
                k = e % NWB
                hws = []
                dma("pool", wg[k][:], wgb_d[e].rearrange("(c p) n -> p c n", p=128), sem_e[k], [], [b_w[k]])
                P.dma("pool", lambda en, k=k, e=e: en.dma_start(out=wu[k][:], in_=wub_d[e].rearrange("(c p) n -> p c n", p=128)), sem_e[k], [], [Buf("t")])
                h = P.dma("pool", lambda en, k=k, e=e: en.dma_start(out=wd[k][:], in_=wdb_d[e].rearrange("(c p) n -> p c n", p=128)), sem_e[k], [], [Buf("t")])
                b_w[k].last_w = h

            def load_xg(e):
                k = e % 2
                dma("sp", xg[k][:], xbuf_d[e * CAP:(e + 1) * CAP, :].rearrange("(b p) d -> p b d", p=128), sem_xg[k], [b_xbuf], [b_xg[k]])

            load_w(0)
            load_xg(0)
            ycount = 0
            for e in range(32):
                k = e % NWB
                kx = e % 2
                if e + 1 < 32:
                    load_w(e + 1)
                    load_xg(e + 1)
                for sb_ in range(NB):
                    tp2 = tp2s[tcount[0] % 2]; b_tp2 = b_tp2s[tcount[0] % 2]
                    tcount[0] += 1
                    for c in range(8):
                        tr(tp2[:, c * 128:(c + 1) * 128], xg[kx][:, sb_, c * 128:(c + 1) * 128], ident_b, [b_xg[kx], b_cb], [b_tp2])
                    cp("dve" if sb_ % 2 == 0 else "act", xgT[:, :, sb_ * 128:(sb_ + 1) * 128], tp2[:].rearrange("p (c t) -> p c t", t=128), [b_tp2], [b_xgT])
                for fc in range(4):
                    a_ = fc % 2
                    for c in range(8):
                        mm(pg_[a_][:, 0:CAP], wg[k][:, c, fc * 128:(fc + 1) * 128], xgT[:, c, :], c == 0, c == 7, [b_w[k], b_xgT], [b_pg[a_]])
                    for c in range(8):
                        mm(pu_[a_][:, 0:CAP], wu[k][:, c, fc * 128:(fc + 1) * 128], xgT[:, c, :], c == 0, c == 7, [b_w[k], b_xgT], [b_pu[a_]])
                    act(sg[a_][:], pg_[a_][:, 0:CAP], AF.Silu, [b_pg[a_]], [b_sg[a_]])
                    tt("dve", hT[:, fc, :], sg[a_][:], pu_[a_][:, 0:CAP], ALU.mult, [b_sg[a_], b_pu[a_]], [b_hT])
                for sb_ in range(NB):
                    ky = ycount % 2
                    ycount += 1
                    for nh in range(2):
                        for fc in range(4):
                            mm(py_[nh][:], hT[:, fc, sb_ * 128:(sb_ + 1) * 128], wd[k][:, fc, nh * 512:(nh + 1) * 512], fc == 0, fc == 3,
                               [b_hT, b_w[k]], [b_py[nh]])
                        cp("act" if nh == 0 else "dve", ysb[ky][:, nh * 512:(nh + 1) * 512], py_[nh][:], [b_py[nh]], [b_ysb[ky]])
                    r0 = e * CAP + sb_ * 128
                    dma("sp", ybuf_d[r0:r0 + 128, :], ysb[ky][:], sem_y[ky], [b_ysb[ky]], [Buf("yst%d" % ky)])
            P.barrier()

            def p3_fetch(i):
                k = i % NP3
                dma("sp", x1t[k][:], out_d[i * 128:(i + 1) * 128, :], sem_l[k], [b_out], [b_x1t[k]])
                offA = dest[:, 0, i:i + 1]
                offB = dest[:, 1, i:i + 1]
                P.dma("pool", lambda en, off=offA, dst=yA[k]: en.indirect_dma_start(
                    out=dst[:, :], out_offset=None, in_=ybuf_d, in_offset=bass.IndirectOffsetOnAxis(ap=off, axis=0)),
                    sem_ga[k], [b_dest], [b_yA[k]])
                P.dma("pool", lambda en, off=offB, dst=yB[k]: en.indirect_dma_start(
                    out=dst[:, :], out_offset=None, in_=ybuf_d, in_offset=bass.IndirectOffsetOnAxis(ap=off, axis=0)),
                    sem_gb[k], [b_dest], [b_yB[k]])

            for i in range(min(NP3 - 1, NT)):
                p3_fetch(i)
            for i in range(NT):
                k = i % NP3
                if i + NP3 - 1 < NT:
                    p3_fetch(i + NP3 - 1)
                stt("dve", x1t[k][:], yA[k][:], gate[:, 0, i:i + 1], x1t[k][:], ALU.mult, ALU.add, [b_yA[k], b_gate, b_x1t[k]], [b_x1t[k]])
                stt("dve", x1t[k][:], yB[k][:], gate[:, 1, i:i + 1], x1t[k][:], ALU.mult, ALU.add, [b_yB[k], b_gate, b_x1t[k]], [b_x1t[k]])
                dma("act", out_d[i * 128:(i + 1) * 128, :], x1t[k][:], sem_o[k], [b_x1t[k]], [Buf("ost%d" % k)])
            P.barrier()
            P.emit_block()
    return nc


def _consts(CAP):
    cf = np.zeros((128, K_END), np.float32)
    cf[:, K_ID:K_ID + 128] = np.eye(128, dtype=np.float32)
    s = np.arange(128)[:, None]
    t = np.arange(128)[None, :]
    cf[:, K_TRI:K_TRI + 128] = ((s <= t) & ((s // 64) == (t // 64))).astype(np.float32)
    cf[63, K_SEL2] = 1.0
    cf[127, K_SEL2 + 1] = 1.0
    invf = (500000.0 ** (-np.arange(0, 16, 2, dtype=np.float32) / 16)).astype(np.float32)
    cf[:, K_INVF:K_INVF + 8] = invf[None, :]
    cf[:, K_IOTA:K_IOTA + 32] = (np.arange(32, dtype=np.float32) * CAP)[None, :]
    cb = np.zeros((128, B_END), np.float32)
    cb[:, B_ID:B_ID + 128] = np.eye(128, dtype=np.float32)
    cb[:, B_STRI:B_STRI + 128] = (s < t).astype(np.float32)
    cb[:, B_ONES:B_ONES + 128] = 1.0
    mcur = np.where(s <= t, 0.0, NEG).astype(np.float32)
    mprev = np.where(s > t, 0.0, NEG).astype(np.float32)
    cb[:, B_MCUR:B_MCUR + 512] = np.tile(mcur, (1, 4))
    cb[:, B_MPREV:B_MPREV + 512] = np.tile(mprev, (1, 4))
    return cf, cb, mprev


def make_in_maps(inp, NT, CAP, n_cores=8):
    x = np.asarray(inp["x"], np.float32)
    B, S, _ = x.shape
    T = NT * 128
    assert S == 2 * T and B * 2 == n_cores
    pos = np.asarray(inp["positions"]).astype(np.int32)
    cf, cb0, mprev = _consts(CAP)
    w_in = np.asarray(inp["w_in"], np.float32)[0]
    qperm = np.concatenate([np.arange(64) + (g * 4 + j) * 64 for j in range(4) for g in range(2)])
    w_in_p = np.ascontiguousarray(np.concatenate([w_in[:, :512][:, qperm], w_in[:, 512:]], axis=1))
    f = lambda k: np.ascontiguousarray(np.asarray(inp[k], np.float32)[0])
    pp = np.zeros((128, Q_END), np.float32)
    pp[:, Q_N1C:Q_N1C + 8] = f("norm1_w").reshape(8, 128).T
    pp[:, Q_N2C:Q_N2C + 8] = f("norm2_w").reshape(8, 128).T
    pp[:, Q_N2:Q_N2 + 1024] = f("norm2_w")[None, :]
    pp[:, Q_QW:Q_QW + 512] = np.tile(f("q_norm_w"), 8)[None, :]
    pp[:, Q_KW:Q_KW + 128] = np.tile(f("k_norm_w"), 2)[None, :]
    pp[:, Q_HW:Q_HW + 512] = np.tile(f("hgrn_norm_w"), 4)[None, :]
    hlb = np.asarray(inp["hgrn_lower_bounds"], np.float32)
    hlbt = np.ascontiguousarray(np.tile(hlb.reshape(1, 1024), (128, 1)))
    pp[:, Q_SINK:Q_SINK + 8] = f("attn_sinks")[None, :]
    pp[:, Q_BRT:Q_BRT + 4] = f("b_router_group")[None, :]
    pp[:, Q_BRT + 4:Q_BRT + 36] = f("b_router_expert")[None, :]
    w_r = np.ascontiguousarray(np.concatenate([f("w_router_group"), f("w_router_expert")], axis=1))
    shared = {
        "cf32": cf, "pp32": pp, "hlb": hlbt, "w_in": w_in_p, "w_ba": f("w_branch_attn"), "w_bh": f("w_branch_hgrn"),
        "w_out": f("w_out"), "w_r": w_r, "w_g": f("w_gate_experts"), "w_u": f("w_up_experts"), "w_d": f("w_down_experts"),
    }
    maps = []
    for c in range(n_cores):
        b, half = c // 2, c % 2
        cb = cb0.copy()
        if half == 1:
            cb[:, B_MHALO:B_MHALO + 512] = np.tile(mprev, (1, 4))
            x_pre = np.ascontiguousarray(x[b, 0:T])
            halo_pos = pos[b, T - 128:T]
        else:
            cb[:, B_MHALO:B_MHALO + 512] = NEG
            x_pre = np.zeros((T, D), np.float32)
            halo_pos = np.zeros(128, np.int32)
        own = slice(half * T, (half + 1) * T)
        pt = np.concatenate([pos[b, own].reshape(NT, 128).T, halo_pos[:, None]], axis=1).astype(np.int32)
        m = dict(shared)
        m.update({"x_own": np.ascontiguousarray(x[b, own]), "x_pre": x_pre, "pos": np.ascontiguousarray(pt), "cb16": cb})
        maps.append(m)
    return maps


_NC_CACHE = {}


def run(inp, NT, CAP, mode=0, raw=False):
    key = (NT, CAP, mode)
    if key not in _NC_CACHE:
        _NC_CACHE[key] = build(NT, CAP, mode=mode)
    nc = _NC_CACHE[key]
    maps = make_in_maps(inp, NT, CAP)
    if mode != 0:
        for m in maps:
            for k in ("w_g", "w_u", "w_d"):
                m[k] = np.ascontiguousarray(m[k][:1])
    res = run_bass_kernel_spmd(nc, maps, core_ids=list(range(8)))
    if raw:
        return res.results
    x = np.asarray(inp["x"])
    B, S, _ = x.shape
    T = NT * 128
    out = np.empty((B, S, D), np.float32)
    for c in range(8):
        b, half = c // 2, c % 2
        out[b, half * T:(half + 1) * T] = res.results[c]["out"]
    return out


def kernel(**inputs):
    return run(inputs, 32, 384)
```

```python
import math
import os
from contextlib import ExitStack
import numpy as np
import concourse.bass as bass
import concourse.mybir as mybir
from concourse.bass_utils import run_bass_kernel_spmd

F32 = mybir.dt.float32
BF16 = mybir.dt.bfloat16
I32 = mybir.dt.int32
ALU = mybir.AluOpType
AF = mybir.ActivationFunctionType
AX = mybir.AxisListType

D = 1024
NEG = -30000.0
EPS = 1e-6
TWO_PI = 2.0 * math.pi


class Buf:
    __slots__ = ("name", "last_w", "readers", "excl")

    def __init__(self, name, excl=False):
        self.name = name
        self.last_w = None
        self.readers = []
        self.excl = excl


class DmaSem:
    def __init__(self, key):
        self.key = key
        self.count = 0


class Prog:
    ENGS = ("pe", "act", "dve", "pool", "sp")
    EMAP = {"pe": "tensor", "act": "scalar", "dve": "vector", "pool": "gpsimd", "sp": "sync"}

    def __init__(self, nc, stack, n_dma_sems=64):
        self.nc = nc
        self.ops = {e: [] for e in self.ENGS}
        self.n = {e: 0 for e in self.ENGS}
        self.waited = {e: {} for e in self.ENGS}
        self.dma_sems = [DmaSem("d%d" % i) for i in range(n_dma_sems)]
        self.sems = {}
        for e in self.ENGS:
            self.sems[e] = stack.enter_context(nc.semaphore("s_" + e))
        for d in self.dma_sems:
            self.sems[d.key] = stack.enter_context(nc.semaphore("s_" + d.key))
        self._next = 0
        self.tfree = {e: 0.0 for e in self.ENGS}
        self.hfin = {}
        self.step_max = 0.0
        self.act_grp = None

    def _time(self, eng, deps, h, cost):
        t = self.tfree[eng]
        for d in deps:
            f = self.hfin.get(d)
            if f is not None:
                f = f + (0.05 if d[0] == eng else 0.2)
                if f > t:
                    t = f
        fin = t + cost
        if h[0] in self.ENGS:
            self.tfree[eng] = fin
        else:
            self.tfree[eng] = t + 0.1
        self.hfin[h] = fin
        if fin > self.step_max:
            self.step_max = fin

    def newsem(self):
        s = self.dma_sems[self._next]
        self._next += 1
        return s

    def _deps(self, eng, reads, writes):
        deps = set()
        for b in reads:
            if b.last_w is not None:
                deps.add(b.last_w)
        for b in writes:
            if b.last_w is not None:
                deps.add(b.last_w)
            deps.update(b.readers)
        w = self.waited[eng]
        best = {}
        for (sk, v) in deps:
            if eng == "pe" and sk == "pe":
                continue
            if w.get(sk, 0) < v and best.get(sk, 0) < v:
                best[sk] = v
        for sk, v in best.items():
            self.ops[eng].append(("wait", sk, v))
            w[sk] = v
        return deps

    def _mark(self, h, reads, writes):
        for b in reads:
            b.readers.append(h)
            if len(b.readers) > 64:
                b.readers = b.readers[-48:]
        for b in writes:
            b.last_w = h
            b.readers = []

    def op(self, eng, fn, reads=(), writes=(), cost=0.3):
        ex = [b for b in reads if b.excl and b not in writes]
        if ex:
            writes = list(writes) + ex
        deps = self._deps(eng, reads, writes)
        self.n[eng] += 1
        h = (eng, self.n[eng])
        self.ops[eng].append(("op", fn))
        self._time(eng, deps, h, cost)
        self._mark(h, reads, writes)
        return h

    def dma(self, eng, fn, sem, reads=(), writes=(), cost=3.0):
        deps = self._deps(eng, reads, writes)
        sem.count += 16
        h = (sem.key, sem.count)
        self.ops[eng].append(("dma", fn, sem.key))
        self._time(eng, deps, h, cost)
        self._mark(h, reads, writes)
        return h

    def wait_all(self, eng, bufs):
        self._deps(eng, bufs, ())

    def barrier(self):
        for e in self.ENGS:
            w = self.waited[e]
            for f in self.ENGS:
                if f != e and self.n[f] > w.get(f, 0):
                    self.ops[e].append(("wait", f, self.n[f]))
                    w[f] = self.n[f]
            for d in self.dma_sems:
                if d.count > w.get(d.key, 0):
                    self.ops[e].append(("wait", d.key, d.count))
                    w[d.key] = d.count

    def emit_block(self):
        sems = self.sems
        with self.nc.Block() as block:
            def mk(e):
                items = self.ops[e]

                def body(engobj):
                    for item in items:
                        if item[0] == "wait":
                            engobj.wait_ge(sems[item[1]], item[2])
                        elif item[0] == "op":
                            item[1](engobj).then_inc(sems[e], 1)
                        else:
                            item[1](engobj).then_inc(sems[item[2]], 16)
                return body
            for e in self.ENGS:
                if self.ops[e]:
                    getattr(block, self.EMAP[e])(mk(e))
        self.ops = {e: [] for e in self.ENGS}


C_Q, C_KV, C_QH, C_FH, C_IH, C_GH, C_ZA, C_ZB = 0, 512, 768, 1280, 1792, 2304, 2816, 3840
NIN = 4864
K_ID, K_TRI, K_SEL2, K_INVF, K_IOTA, K_END = 0, 128, 256, 258, 266, 298
B_ID, B_STRI, B_ONES, B_MCUR, B_MPREV, B_MHALO, B_END = 0, 128, 256, 384, 896, 1408, 1920
Q_N2, Q_QW, Q_KW, Q_HW, Q_SINK, Q_BRT, Q_N1C, Q_N2C, Q_END = 0, 1024, 1536, 1664, 2176, 2184, 2220, 2228, 2236


def build(NT=32, CAP=384, NPRE=None, mode=0):
    if NPRE is None:
        NPRE = NT
    T = NT * 128
    NSLOT = 32 * CAP
    NB = CAP // 128
    nc = bass.Bass("TRN2", target_bir_lowering=False)

    def din(name, shape, dt=F32):
        return nc.dram_tensor(name, shape, dt, kind="ExternalInput").ap()

    x_own = din("x_own", [T, D])
    x_pre = din("x_pre", [NPRE * 128, D])
    pos_d = din("pos", [128, NT + 1], I32)
    cf_d = din("cf32", [128, K_END])
    cb_d = din("cb16", [128, B_END])
    pp_d = din("pp32", [128, Q_END])
    hlb_d = din("hlb", [128, 1024])
    win_d = din("w_in", [D, NIN])
    wba_d = din("w_ba", [512, D])
    wbh_d = din("w_bh", [512, D])
    wout_d = din("w_out", [D, D])
    wr_d = din("w_r", [D, 36])
    NEX = 32 if mode == 0 else 1
    wg_d = din("w_g", [NEX, D, 512])
    wu_d = din("w_u", [NEX, D, 512])
    wd_d = din("w_d", [NEX, 512, D])
    out_d = nc.dram_tensor("out", [T, D], F32, kind="ExternalOutput").ap()
    xbuf_d = nc.dram_tensor("xbuf", [NSLOT, D], BF16, kind="Internal").ap()
    ybuf_d = nc.dram_tensor("ybuf", [NSLOT, D], F32, kind="Internal").ap()
    wgb_d = nc.dram_tensor("wgb", [NEX, D, 512], BF16, kind="Internal").ap()
    wub_d = nc.dram_tensor("wub", [NEX, D, 512], BF16, kind="Internal").ap()
    wdb_d = nc.dram_tensor("wdb", [NEX, 512, D], BF16, kind="Internal").ap()

    with ExitStack() as top:
        P = Prog(nc, top)

        def SB(st, name, shape, dt):
            return st.enter_context(nc.sbuf_tensor(name, shape, dt))

        def PS(st, name, shape, dt):
            return st.enter_context(nc.psum_tensor(name, shape, dt))

        def nfree(ap):
            n = 1
            for d in ap.shape[1:]:
                n *= d
            return n

        def ecost(eng, ap):
            n = nfree(ap)
            if eng == "pool":
                return 0.12 + n * 0.0022
            if eng == "act":
                return 0.2 + n * 0.00095
            return 0.07 + n * 0.00123

        AGRP = {AF.Exp: 1, AF.Ln: 1, AF.Sigmoid: 2, AF.Silu: 3, AF.Sin: 4}

        def mm(out, lhsT, rhs, start, stop, r, w):
            c = max(0.064, 0.00042 * nfree(rhs))
            if lhsT.dtype == F32:
                c *= 4
            return P.op("pe", lambda e: e.matmul(out=out, lhsT=lhsT, rhs=rhs, start=start, stop=stop), r, w, cost=c)

        def tr(out, in_, ident, r, w):
            c = 0.11 * (4 if in_.dtype == F32 else 1)
            return P.op("pe", lambda e: e.transpose(out=out, in_=in_, identity=ident), r, w, cost=c)

        def act(out, in_, func, r, w, **kw):
            c = ecost("act", in_)
            g = AGRP.get(func)
            if g is not None and g != P.act_grp:
                c += 1.3
                P.act_grp = g
            return P.op("act", lambda e: e.activation(out=out, in_=in_, func=func, **kw), r, w, cost=c)

        def tt(eng, out, in0, in1, op, r, w):
            return P.op(eng, lambda e: e.tensor_tensor(out=out, in0=in0, in1=in1, op=op), r, w, cost=ecost(eng, out))

        def ts(eng, out, in0, s1, s2, op0, op1, r, w):
            if op1 is None:
                return P.op(eng, lambda e: e.tensor_scalar(out=out, in0=in0, scalar1=s1, scalar2=None, op0=op0), r, w, cost=ecost(eng, out))
            return P.op(eng, lambda e: e.tensor_scalar(out=out, in0=in0, scalar1=s1, scalar2=s2, op0=op0, op1=op1), r, w, cost=ecost(eng, out))

        def stt(eng, out, in0, scalar, in1, op0, op1, r, w):
            return P.op(eng, lambda e: e.scalar_tensor_tensor(out=out, in0=in0, scalar=scalar, in1=in1, op0=op0, op1=op1), r, w, cost=ecost(eng, out))

        def cp(eng, out, in_, r, w):
            if eng == "act":
                return P.op("act", lambda e: e.copy(out=out, in_=in_), r, w, cost=ecost(eng, out))
            return P.op(eng, lambda e: e.tensor_copy(out=out, in_=in_), r, w, cost=ecost(eng, out))

        def red(eng, out, in_, op, r, w):
            return P.op(eng, lambda e: e.tensor_reduce(out=out, in_=in_, axis=AX.X, op=op), r, w, cost=ecost(eng, in_))

        def mset(eng, ap, val, w):
            return P.op(eng, lambda e: e.memset(ap, val), (), w, cost=ecost(eng, ap))

        def dma(eng, out, in_, sem, r, w):
            return P.dma(eng, lambda e: e.dma_start(out=out, in_=in_), sem, r, w)

        def bc(ap, shape, axis):
            return ap.unsqueeze(axis).to_broadcast(shape)

        idb = SB(top, "idb", [128, B_END], BF16); b_cb = Buf("cb")
        cf = SB(top, "cf", [128, K_END], F32); b_cf = Buf("cf")
        dest = SB(top, "dest", [128, 2, NT], I32); b_dest = Buf("dest")
        gate = SB(top, "gate", [128, 2, NT], F32); b_gate = Buf("gate")
        b_xbuf = Buf("xbuf"); b_ybuf = Buf("ybuf"); b_out = Buf("out")
        ident_f = cf[:, K_ID:K_ID + 128]
        tri_f = cf[:, K_TRI:K_TRI + 128]
        sel2 = cf[:, K_SEL2:K_SEL2 + 2]
        invf = cf[:, K_INVF:K_INVF + 8]
        iotae = cf[:, K_IOTA:K_IOTA + 32]
        ident_b = idb[:, B_ID:B_ID + 128]
        stri_b = idb[:, B_STRI:B_STRI + 128]
        ones_b = idb[:, B_ONES:B_ONES + 128]
        mask_cur = idb[:, B_MCUR:B_MCUR + 512]
        mask_prev = idb[:, B_MPREV:B_MPREV + 512]
        mask_halo = idb[:, B_MHALO:B_MHALO + 512]

        sem_c = [P.newsem() for _ in range(4)]
        dma("sp", cf[:], cf_d, sem_c[0], [], [b_cf])
        dma("pool", idb[:], cb_d, sem_c[1], [], [b_cb])

        with ExitStack() as s1:
            Wp = SB(s1, "Wp", [128, 8, NIN], BF16); b_Wp = Buf("Wp")
            wba = SB(s1, "wba", [128, 4, D], BF16); b_wba = Buf("wba")
            wbh = SB(s1, "wbh", [128, 4, D], BF16); b_wbh = Buf("wbh")
            wout = SB(s1, "wout", [128, 8, D], BF16); b_wout = Buf("wout")
            wr = SB(s1, "wr", [128, 8, 36], F32); b_wr = Buf("wr")
            pp = SB(s1, "pp", [128, Q_END], F32); b_pp = Buf("pp")
            lbt = SB(s1, "lbt", [128, 2, 512], F32); b_lb = Buf("lb")
            esink = SB(s1, "esink", [128, 8], F32); b_esink = Buf("esink")
            posi = SB(s1, "posi", [128, NT + 1], I32); b_posi = Buf("posi")
            NA = (NT + 1) * 8
            trig = SB(s1, "trig", [128, 2, NA], F32); b_trig = Buf("trig")
            cst = SB(s1, "cst", [128, 4], F32); b_cst = Buf("cst")
            cos_t = trig[:, 0, :].rearrange("p (i j) -> p i j", j=8)
            sin_t = trig[:, 1, :].rearrange("p (i j) -> p i j", j=8)

            n2w = pp[:, Q_N2:Q_N2 + 1024]
            qw = pp[:, Q_QW:Q_QW + 512]
            kw = pp[:, Q_KW:Q_KW + 128]
            hw = pp[:, Q_HW:Q_HW + 512]
            brt = pp[:, Q_BRT:Q_BRT + 36]

            dma("sp", pp[:], pp_d, sem_c[2], [], [b_pp])
            dma("sp", posi[:], pos_d, sem_c[3], [], [b_posi])
            WG = [(C_FH, C_GH), (C_KV, C_QH), (C_Q, C_KV), (C_QH, C_FH), (C_GH, C_ZA), (C_ZA, NIN)]
            b_Wg = [Buf("Wg%d" % g) for g in range(len(WG))]
            win3 = win_d.rearrange("(c p) n -> p c n", p=128)
            for g, (c0, c1) in enumerate(WG):
                dma("pool", Wp[:, :, c0:c1], win3[:, :, c0:c1], P.newsem(), [], [b_Wg[g]])

            def wbuf(c0):
                for g, (a0, a1) in enumerate(WG):
                    if a0 <= c0 < a1:
                        return b_Wg[g]
                raise ValueError(c0)
            sem_w = P.newsem()
            dma("pool", wba[:], wba_d.rearrange("(c p) n -> p c n", p=128), sem_w, [], [Buf("wtmp")])
            dma("pool", wbh[:], wbh_d.rearrange("(c p) n -> p c n", p=128), sem_w, [], [Buf("wtmp")])
            hW = dma("pool", wout[:], wout_d.rearrange("(c p) n -> p c n", p=128), sem_w, [], [Buf("wtmp")])
            for b in (b_wba, b_wbh, b_wout):
                b.last_w = hW
            sem_wr = P.newsem()
            dma("sp", wr[:], wr_d.rearrange("(c p) n -> p c n", p=128), sem_wr, [], [b_wr])

            xt = [SB(s1, "xt%d" % k, [128, D], F32) for k in range(3)]; b_xt = [Buf("xt%d" % k) for k in range(3)]
            sem_x = [P.newsem() for _ in range(3)]
            junkF = SB(s1, "junkF", [128, D], BF16); b_junkF = Buf("junkF")
            junkY = SB(s1, "junkY", [128, D], BF16); b_junkY = Buf("junkY")
            stF = SB(s1, "stF", [128, 8], F32); b_stF = Buf("stF")
            stA = SB(s1, "stA", [128, 64], F32); b_stA = Buf("stA")
            stH = SB(s1, "stH", [128, 16], F32); b_stH = Buf("stH")
            stY = SB(s1, "stY", [128, 8], F32); b_stY = Buf("stY")
            xb = SB(s1, "xb", [128, D], BF16); b_xb = Buf("xb")
            xT = SB(s1, "xT", [128, 8, 128], BF16); b_xT = Buf("xT")
            qa = SB(s1, "qa", [128, 512], F32); b_qa = Buf("qa")
            kv = SB(s1, "kvf", [128, 128], F32); b_kv = Buf("kv")
            r16 = SB(s1, "r16", [128, 6, 8, 8], F32); b_r16 = Buf("r16")
            qr = SB(s1, "qr", [128, 512], BF16); b_qr = Buf("qr")
            kr = SB(s1, "kr", [128, 128], BF16); b_kr = Buf("kr")
            qT = SB(s1, "qT", [128, 4, 128], BF16); b_qT = Buf("qT")
            kTs = [SB(s1, "kT%d" % k, [128, 128], BF16) for k in range(2)]; b_kT = [Buf("kT0"), Buf("kT1")]
            vx = [SB(s1, "vx%d" % k, [128, 2, 80], BF16) for k in range(2)]; b_vx = [Buf("vx0"), Buf("vx1")]
            pTb = [SB(s1, "pT%d" % k, [128, 512], BF16) for k in range(2)]; b_pTb = [Buf("pT0"), Buf("pT1")]
            attn = SB(s1, "attn", [128, 512], BF16); b_attn = Buf("attn")
            aT = SB(s1, "aT", [128, 4, 128], BF16); b_aT = Buf("aT")
            qh = SB(s1, "qh", [128, 512], F32); b_qh = Buf("qh")
            hA = SB(s1, "hA", [128, 512], F32); b_hA = Buf("hA")
            hk = SB(s1, "hk", [128, 512], F32); b_hk = Buf("hk")
            eb = SB(s1, "eb", [128, 512], F32); b_eb = Buf("eb")
            iv = SB(s1, "iv", [128, 512], BF16); b_iv = Buf("iv")
            gs = SB(s1, "gs", [128, 512], F32); b_gs = Buf("gs")
            qt = SB(s1, "qt", [128, 512], BF16); b_qt = Buf("qt")
            kt = SB(s1, "kt", [128, 512], BF16); b_kt = Buf("kt")
            qlh = SB(s1, "qlh", [128, 4, 2, 128], BF16); b_qlh = Buf("qlh")
            ktT = SB(s1, "ktT", [128, 4, 128], BF16); b_ktT = Buf("ktT")
            Am = [SB(s1, "Am%d" % k, [128, 128], BF16) for k in range(2)]; b_Am = [Buf("Am0"), Buf("Am1")]
            S = SB(s1, "S", [128, 4, 128], F32); b_S = Buf("S")
            Sb = [SB(s1, "Sb%d" % k, [128, 4, 128], BF16) for k in range(2)]; b_Sb = [Buf("Sb0"), Buf("Sb1")]
            Dsb = SB(s1, "Dsb", [128, 4, 2], F32); b_Dsb = Buf("Dsb")
            hg = SB(s1, "hg", [128, 512], BF16); b_hg = Buf("hg")
            hgT = SB(s1, "hgT", [128, 4, 128], BF16); b_hgT = Buf("hgT")
            sza = SB(s1, "sza", [128, D], BF16); b_sza = Buf("sza")
            szb = SB(s1, "szb", [128, D], BF16); b_szb = Buf("szb")
            m1 = SB(s1, "m1", [128, D], F32); b_m1 = Buf("m1")
            mixed = SB(s1, "mixed", [128, D], BF16); b_mixed = Buf("mixed")
            mT = SB(s1, "mT", [128, 8, 128], BF16); b_mT = Buf("mT")
            hb = SB(s1, "hb", [128, D], BF16); b_hb = Buf("hb")
            rt = SB(s1, "rt", [128, 256], F32); b_rt = Buf("rt")
            cums = SB(s1, "cums", [128, 32], F32); b_cums = Buf("cums")
            cumsb = SB(s1, "cumsb", [128, 32], BF16); b_cumsb = Buf("cumsb")
            selb = SB(s1, "selb", [128, 32], BF16); b_selb = Buf("selb")
            sem_x1 = P.newsem()
            sem_sc = P.newsem()

            tp = PS(s1, "tp", [128, 1024], BF16); b_tp = Buf("tp", True)
            mmA = PS(s1, "mmA", [128, 512], F32); mmB = PS(s1, "mmB", [128, 512], F32)
            b_mm = [Buf("mmA", True), Buf("mmB", True)]; mmP = [mmA, mmB]
            sc = [PS(s1, "sc0", [128, 512], F32)]; b_sc = [Buf("sc0", True)]
            tpB = PS(s1, "tpB", [128, 1024], BF16); b_tpB = Buf("tpB", True)
            pv = [PS(s1, "pv0", [128, 512], F32), PS(s1, "pv1", [128, 512], F32)]; b_pv = [Buf("pv0", True), Buf("pv1", True)]
            misc = PS(s1, "misc", [128, 512], F32); b_misc = Buf("misc", True)

            mset("dve", qlh[:], 0.0, [b_qlh])
            mset("dve", S[:], 0.0, [b_S])
            mset("dve", Sb[0][:], 0.0, [b_Sb[0]])
            mset("dve", cums[:], 0.0, [b_cums])
            mset("dve", cumsb[:], 0.0, [b_cumsb])
            for k in range(2):
                mset("dve", vx[k][:], 0.0, [b_vx[k]])
                mset("dve", vx[k][:, :, 64:65], 1.0, [b_vx[k]])
            def scale_w(g):
                c0, c1 = WG[g]
                tt("dve", Wp[:, :, c0:c1], Wp[:, :, c0:c1], bc(pp[:, Q_N1C:Q_N1C + 8], [128, 8, c1 - c0], 2), ALU.mult,
                   [b_Wg[g], b_pp], [b_Wg[g]])
            scale_w(0)
            scale_w(1)
            pending_w = [2, 3, 4, 5]
            for c in range(8):
                ts("dve", wr[:, c, :], wr[:, c, :], pp[:, Q_N2C + c:Q_N2C + c + 1], None, ALU.mult, None, [b_wr, b_pp], [b_wr])
            zt = hb[:]; b_zt = b_hb
            ZC = 1024
            mset("pool", zt, 0.0, [b_zt])
            sem_z = P.newsem()
            xz = xbuf_d.rearrange("(p r) d -> p (r d)", p=128)
            tot = (NSLOT // 128) * D
            zchunks = [(k0, min(tot, k0 + ZC)) for k0 in range(0, tot, ZC)]

            def zero_some(n):
                for _ in range(n):
                    if zchunks:
                        k0, k1 = zchunks.pop(0)
                        b_xbuf.last_w = dma("sp", xz[:, k0:k1], zt[:, 0:k1 - k0], sem_z, [b_zt], [Buf("ztmp")])

            mset("dve", cst[:, 0:1], math.pi / 2, [b_cst])
            mset("dve", cst[:, 1:2], EPS, [b_cst])
            dma("sp", m1[:], hlb_d, P.newsem(), [], [b_m1])
            hl = m1[:].rearrange("p (a c) -> p a c", a=2)
            tt("dve", lbt[:, 0, :], hl[:, 0, :], hl[:, 1, :], ALU.subtract, [b_m1], [b_lb])
            act(lbt[:, 0, :], lbt[:, 0, :], AF.Sigmoid, [b_lb], [b_lb])
            ts("dve", lbt[:, 1, :], lbt[:, 0, :], -1.0, 1.0, ALU.mult, ALU.add, [b_lb], [b_lb])
            act(esink[:], pp[:, Q_SINK:Q_SINK + 8], AF.Exp, [b_pp], [b_esink])
            b_tg = Buf("trigtmp")
            tA = xt[1][:, 0:2 * NA].rearrange("p (a n) -> p a n", a=2)
            tB = xt[2][:, 0:2 * NA].rearrange("p (a n) -> p a n", a=2)
            TR = [b_tg, b_xt[1], b_xt[2]]
            trg = {0: tA[:, 0, :], 1: tA[:, 1, :], 2: tB[:, 0, :], 3: tB[:, 1, :], 4: trig[:, 0, :], 5: trig[:, 1, :]}
            posf = trg[0][:, 0:NT + 1]
            cp("dve", posf, posi[:], [b_posi], TR)
            ang = trg[1].rearrange("p (i j) -> p i j", j=8)
            tt("dve", ang, bc(posf, [128, NT + 1, 8], 2), bc(invf, [128, NT + 1, 8], 1), ALU.mult, TR + [b_cf], TR)
            kf = trg[2]
            ts("dve", kf, trg[1], 1.0 / TWO_PI, None, ALU.mult, None, TR, TR)
            ki = trg[3].bitcast(I32)
            cp("dve", ki, kf, TR, TR)
            cp("dve", kf, ki, TR, TR)
            r_ = trg[1]
            stt("dve", r_, kf, -TWO_PI, r_, ALU.mult, ALU.add, TR, TR)
            s4 = trg[2]
            c4 = trg[3]
            act(s4, r_, AF.Sin, TR, TR, scale=0.25)
            act(c4, r_, AF.Sin, TR + [b_cst], TR, scale=0.25, bias=cst[:, 0:1])
            s2 = trg[0]
            c2 = trg[1]
            stt("dve", s2, s4, 2.0, c4, ALU.mult, ALU.mult, TR, TR)
            tt("dve", c2, s4, s4, ALU.mult, TR, TR)
            ts("dve", c2, c2, -2.0, 1.0, ALU.mult, ALU.add, TR, TR)
            stt("dve", trg[5], s2, 2.0, c2, ALU.mult, ALU.mult, TR, [b_trig])
            tt("dve", trg[4], s2, s2, ALU.mult, TR, [b_trig])
            ts("dve", trg[4], trg[4], -2.0, 1.0, ALU.mult, ALU.add, [b_trig], [b_trig])


            mmc = [0]

            def next_mm():
                k = mmc[0] % 2
                mmc[0] += 1
                return mmP[k], b_mm[k]

            def rstd_of(stt_, b_s, col, src_col, scale=1.0):
                act(stt_[:, col], stt_[:, src_col], AF.Ln, [b_s, b_cst], [b_s], bias=cst[:, 1:2], scale=scale)
                act(stt_[:, col], stt_[:, col], AF.Exp, [b_s], [b_s], scale=-0.5)

            def load_x(src, i, slot):
                dma("sp", xt[slot][:], src[i * 128:(i + 1) * 128, :], sem_x[slot], [], [b_xt[slot]])

            rstd1 = stF[:, 1:2]

            def front(slot):
                mset("dve", stF[:, 0:1], 0.0, [b_stF])
                act(junkF[:], xt[slot][:], AF.Square, [b_xt[slot]], [b_junkF, b_stF], scale=1.0 / 32, accum_out=stF[:, 0:1])
                rstd_of(stF, b_stF, slice(1, 2), slice(0, 1))
                cp("act", xb[:], xt[slot][:], [b_xt[slot]], [b_xb])
                for c in range(8):
                    tr(tp[:, c * 128:(c + 1) * 128], xb[:, c * 128:(c + 1) * 128], ident_b, [b_xb, b_cb], [b_tp])
                cp("dve", xT[:].rearrange("p c t -> p (c t)"), tp[:], [b_tp], [b_xT])

            def proj(c0, n):
                ps_, b_ps = next_mm()
                for c in range(8):
                    mm(ps_[:, 0:n], xT[:, c, :], Wp[:, c, c0:c0 + n], c == 0, c == 7, [b_xT, wbuf(c0)], [b_ps])
                return ps_, b_ps

            def qknorm_rope(src, b_src, nh, wtile, dst, b_dst, ti):
                n = nh * 64
                s3 = src.rearrange("p (h d) -> p h d", d=64)
                d3 = dst.rearrange("p (h d) -> p h d", d=64)
                w3 = wtile.rearrange("p (h d) -> p h d", d=64)
                tt("pool", dst, src, src, ALU.mult, [b_src], [b_dst])
                red("dve", stA[:, 8:8 + nh], d3, ALU.add, [b_dst], [b_stA])
                rstd_of(stA, b_stA, slice(16, 16 + nh), slice(8, 8 + nh), scale=1.0 / 64)
                tt("dve", s3, s3, bc(stA[:, 16:16 + nh], [128, nh, 64], 2), ALU.mult, [b_src, b_stA], [b_src])
                tt("dve", dst, src, wtile, ALU.mult, [b_src, b_pp], [b_dst])
                x1 = r16[:, 0, 0:nh, :]; x2 = r16[:, 1, 0:nh, :]
                tt("dve", x1, s3[:, :, 0:8], w3[:, :, 0:8], ALU.mult, [b_src, b_pp], [b_r16])
                tt("dve", x2, s3[:, :, 8:16], w3[:, :, 8:16], ALU.mult, [b_src, b_pp], [b_r16])
                cs = bc(cos_t[:, ti, :], [128, nh, 8], 1)
                sn = bc(sin_t[:, ti, :], [128, nh, 8], 1)
                a_ = r16[:, 2, 0:nh, :]; b_ = r16[:, 3, 0:nh, :]; c_ = r16[:, 4, 0:nh, :]; d_ = r16[:, 5, 0:nh, :]
                tt("dve", a_, x1, cs, ALU.mult, [b_r16, b_trig], [b_r16])
                tt("dve", b_, x2, sn, ALU.mult, [b_r16, b_trig], [b_r16])
                tt("dve", c_, x2, cs, ALU.mult, [b_r16, b_trig], [b_r16])
                tt("dve", d_, x1, sn, ALU.mult, [b_r16, b_trig], [b_r16])
                tt("dve", d3[:, :, 0:8], a_, b_, ALU.subtract, [b_r16], [b_dst])
                tt("dve", d3[:, :, 8:16], c_, d_, ALU.add, [b_r16], [b_dst])

            def kv_proj(slot):
                ps_, b_ps = proj(C_KV, 256)
                act(kv[:], ps_[:, 0:128], AF.Copy, [b_ps, b_stF], [b_kv], scale=rstd1)
                act(vx[slot][:, :, 0:64], ps_[:, 128:256].rearrange("p (m d) -> p m d", d=64), AF.Copy, [b_ps, b_stF], [b_vx[slot]], scale=rstd1)

            def k_finish(ti, slot):
                qknorm_rope(kv[:], b_kv, 2, kw, kr[:], b_kr, ti)
                tr(tp[:, 0:128], kr[:], ident_b, [b_kr, b_cb], [b_tp])
                cp("dve", kTs[slot][:], tp[:, 0:128], [b_tp], [b_kT[slot]])

            HS = [(hA, b_hA, hk, b_hk, eb, b_eb, iv, b_iv, kt, b_kt),
                  (qa, b_qa, qh, b_qh, gs, b_gs, qr, b_qr, attn, b_attn)]

            def hgrn_proj(hs=0):
                hA, b_hA, hk, b_hk, eb, b_eb, iv, b_iv, kt, b_kt = HS[hs]
                ps_, b_ps = proj(C_FH, 512)
                act(hA[:], ps_[:], AF.Sigmoid, [b_ps, b_stF], [b_hA], scale=rstd1)
                ps2, b_ps2 = proj(C_IH, 512)
                act(iv[:], ps2[:], AF.Copy, [b_ps2, b_stF], [b_iv], scale=rstd1)

            def hgrn_chain(hs=0):
                hA, b_hA, hk, b_hk, eb, b_eb, iv, b_iv, kt, b_kt = HS[hs]
                tt("pool", hA[:], hA[:], lbt[:, 1, :], ALU.mult, [b_hA, b_lb], [b_hA])
                tt("pool", hA[:], hA[:], lbt[:, 0, :], ALU.add, [b_hA, b_lb], [b_hA])
                yield
                ts("dve", hk[:], hA[:], -1.0, 1.0, ALU.mult, ALU.add, [b_hA], [b_hk])
                act(hA[:], hA[:], AF.Ln, [b_hA], [b_hA])
                yield
                mm(pv[1][:], tri_f, hA[:], True, True, [b_cf, b_hA], [b_pv[1]])
                act(eb[:], pv[1][:], AF.Exp, [b_pv[1]], [b_eb])
                act(hA[:], pv[1][:], AF.Exp, [b_pv[1]], [b_hA], scale=-1.0)
                yield
                tt("dve", kt[:], hk[:], hA[:], ALU.mult, [b_hk, b_hA], [b_kt])
                for hh in range(4):
                    mm(misc[:, hh * 2:hh * 2 + 2], eb[:, hh * 128:(hh + 1) * 128], sel2, True, True, [b_eb, b_cf], [b_misc])
                cp("dve", Dsb[:].rearrange("p h c -> p (h c)"), misc[:, 0:8], [b_misc], [b_Dsb])
                yield

            def state_update(c, dst, hs=0):
                hA, b_hA, hk, b_hk, eb, b_eb, iv, b_iv, kt, b_kt = HS[hs]
                for hh in range(4):
                    mm(misc[:, hh * 128:(hh + 1) * 128], kt[c * 64:(c + 1) * 64, hh * 128:(hh + 1) * 128],
                       iv[c * 64:(c + 1) * 64, hh * 128:(hh + 1) * 128], True, True, [b_kt, b_iv], [b_misc])
                Sf = S[:].rearrange("p h d -> p (h d)")
                tt("dve", Sf, Sf, misc[:], ALU.add, [b_S, b_misc], [b_S])
                tt("dve", S[:], S[:], bc(Dsb[:, :, c], [128, 4, 128], 2), ALU.mult, [b_S, b_Dsb], [b_S])
                cp("act", Sb[dst][:], S[:], [b_S], [b_Sb[dst]])

            flags = set()

            def run_threads(threads):
                active = [[g, 0.0, None] for g in threads]
                while active:
                    cand = [a for a in active if a[2] is None or a[2] in flags]
                    assert cand, "scheduler deadlock"
                    a = cand[0]
                    active.remove(a)
                    active.append(a)
                    a[2] = None
                    P.step_max = 0.0
                    try:
                        r = next(a[0])
                    except StopIteration:
                        active.remove(a)
                        continue
                    if P.step_max > 0.0:
                        a[1] = P.step_max
                    if r is None:
                        continue
                    if isinstance(r, tuple):
                        if r[0] == "need":
                            a[2] = r[1]
                        else:
                            flags.add(r[1])
                    else:
                        active.append([r, a[1], None])

            load_x(x_pre if NPRE > 0 else x_own, 0, 0)
            def thr_PXF(j):
                front(j % 3)
                yield
                hgrn_proj(j % 2)
                yield
                if j == NPRE - 1:
                    kv_proj(1)
                    yield
                    k_finish(NT, 1)

            def thr_PH(j):
                for _ in hgrn_chain(j % 2):
                    yield
                state_update(0, 1, j % 2)
                yield
                state_update(1, 0, j % 2)

            for j in range(NPRE + 1):
                if j < NPRE:
                    if j + 1 < NPRE:
                        load_x(x_pre, j + 1, (j + 1) % 3)
                    else:
                        load_x(x_own, 0, (j + 1) % 3)
                thr = []
                if j > 0:
                    thr.append(thr_PH(j - 1))
                if j < NPRE:
                    thr.append(thr_PXF(j))
                run_threads(thr)
                if pending_w and j >= 3 and j % 2 == 1:
                    scale_w(pending_w.pop(0))
                if j >= 1:
                    zero_some(4)
            while pending_w:
                scale_w(pending_w.pop(0))
            zero_some(len(zchunks))

            def thr_A(i):
                ks = i % 2
                k_finish(i, ks)
                yield
                qknorm_rope(qa[:], b_qa, 8, qw, qr[:], b_qr, i)
                yield
                for j in range(4):
                    tr(tp[:, j * 128:(j + 1) * 128], qr[:, j * 128:(j + 1) * 128], ident_b, [b_qr, b_cb], [b_tp])
                cp("dve", qT[:].rearrange("p j t -> p (j t)"), tp[:, 0:512], [b_tp], [b_qT])
                yield
                for m in range(2):
                    for kb in range(2):
                        ksl = (1 - ks) if kb == 0 else ks
                        msk = (mask_halo if i == 0 else mask_prev) if kb == 0 else mask_cur
                        mm(sc[0][:], ident_b, msk, True, False, [b_cb], [b_sc[0]])
                        mm(sc[0][:], kTs[ksl][m * 64:(m + 1) * 64, :], qT[m * 64:(m + 1) * 64, :, :].rearrange("p j t -> p (j t)"),
                           False, True, [b_kT[ksl], b_qT], [b_sc[0]])
                        act(pTb[kb][:], sc[0][:], AF.Exp, [b_sc[0]], [b_pTb[kb]], scale=0.125)
                        yield
                    for j in range(4):
                        for kb in range(2):
                            ksl = (1 - ks) if kb == 0 else ks
                            mm(pv[0][:, j * 80:(j + 1) * 80], pTb[kb][:, j * 128:(j + 1) * 128], vx[ksl][:, m, :],
                               kb == 0, kb == 1, [b_pTb[kb], b_vx[ksl]], [b_pv[0]])
                    pv3 = pv[0][:, 0:320].rearrange("p (j d) -> p j d", d=80)
                    den = stA[:, 24 + m * 4:28 + m * 4]
                    tt("dve", den, pv3[:, :, 64], esink[:, m * 4:(m + 1) * 4], ALU.add, [b_pv[0], b_esink], [b_stA])
                    P.op("dve", lambda e, den=den: e.reciprocal(out=den, in_=den), [b_stA], [b_stA])
                    tt("dve", attn[:, m * 256:(m + 1) * 256].rearrange("p (j d) -> p j d", d=64), pv3[:, :, 0:64],
                       bc(den, [128, 4, 64], 2), ALU.mult, [b_pv[0], b_stA], [b_attn])
                    yield
                if i > 0:
                    yield ("need", ("aT", i - 1))
                for j in range(4):
                    tr(tp[:, j * 128:(j + 1) * 128], attn[:, j * 128:(j + 1) * 128], ident_b, [b_attn, b_cb], [b_tp])
                cp("dve", aT[:].rearrange("p j t -> p (j t)"), tp[:, 0:512], [b_tp], [b_aT])

            def thr_H(i):
                for _ in hgrn_chain():
                    yield
                tt("dve", qt[:], qh[:], eb[:], ALU.mult, [b_qh, b_eb], [b_qt])
                tt("pool", gs[:], gs[:], hw, ALU.mult, [b_gs, b_pp], [b_gs])
                yield
                for hh in range(4):
                    tr(tpB[:, hh * 128:(hh + 1) * 128], qt[:, hh * 128:(hh + 1) * 128], ident_b, [b_qt, b_cb], [b_tpB])
                    tr(tpB[:, 512 + hh * 128:512 + (hh + 1) * 128], kt[:, hh * 128:(hh + 1) * 128], ident_b, [b_kt, b_cb], [b_tpB])
                tq = tpB[:, 0:512].rearrange("p (h t) -> p h t", t=128)
                cp("dve", qlh[:, :, 0, 0:64], tq[:, :, 0:64], [b_tpB], [b_qlh])
                cp("dve", qlh[:, :, 1, 64:128], tq[:, :, 64:128], [b_tpB], [b_qlh])
                cp("act", ktT[:].rearrange("p h t -> p (h t)"), tpB[:, 512:1024], [b_tpB], [b_ktT])
                yield
                state_update(0, 1)
                yield
                for hh in range(4):
                    a_ = hh % 2
                    sca = misc[:, 128 + a_ * 128:256 + a_ * 128]
                    mm(sca, ktT[:, hh, :], qlh[:, hh, 0, :], True, False, [b_ktT, b_qlh], [b_misc])
                    mm(sca, ktT[:, hh, :], qlh[:, hh, 1, :], False, True, [b_ktT, b_qlh], [b_misc])
                    tt("dve", Am[a_][:], sca, tri_f, ALU.mult, [b_misc, b_cf], [b_Am[a_]])
                    o_ = pv[1][:, hh * 128:(hh + 1) * 128]
                    mm(o_, Am[a_][:], iv[:, hh * 128:(hh + 1) * 128], True, False, [b_Am[a_], b_iv], [b_pv[1]])
                    mm(o_, qlh[:, hh, 0, :], Sb[0][:, hh, :], False, False, [b_qlh, b_Sb[0]], [b_pv[1]])
                    mm(o_, qlh[:, hh, 1, :], Sb[1][:, hh, :], False, True, [b_qlh, b_Sb[1]], [b_pv[1]])
                    yield
                state_update(1, 0)
                yield
                o3 = pv[1][:].rearrange("p (h d) -> p h d", d=128)
                act(hk[:], pv[1][:], AF.Square, [b_pv[1]], [b_hk])
                red("dve", stH[:, 0:4], hk[:].rearrange("p (h d) -> p h d", d=128), ALU.add, [b_hk], [b_stH])
                rstd_of(stH, b_stH, slice(4, 8), slice(0, 4), scale=1.0 / 128)
                tt("dve", qh[:].rearrange("p (h d) -> p h d", d=128), o3, bc(stH[:, 4:8], [128, 4, 128], 2), ALU.mult,
                   [b_pv[1], b_stH], [b_qh])
                yield
                tt("dve", hg[:], qh[:], gs[:], ALU.mult, [b_qh, b_gs], [b_hg])
                if i > 0:
                    yield ("need", ("hgT", i - 1))
                for j in range(4):
                    tr(tpB[:, j * 128:(j + 1) * 128], hg[:, j * 128:(j + 1) * 128], ident_b, [b_hg, b_cb], [b_tpB])
                cp("dve", hgT[:].rearrange("p j t -> p (j t)"), tpB[:, 0:512], [b_tpB], [b_hgT])

            def thr_XF(i, slot):
                front(slot)
                yield
                ps_, b_ps = proj(C_Q, 512)
                act(qa[:], ps_[:], AF.Copy, [b_ps, b_stF], [b_qa], scale=rstd1)
                kv_proj(i % 2)
                yield thr_A(i)
                hgrn_proj()
                yield
                ps_, b_ps = proj(C_QH, 512)
                act(qh[:], ps_[:], AF.Silu, [b_ps, b_stF], [b_qh], scale=rstd1)
                ps_, b_ps = proj(C_GH, 512)
                act(gs[:], ps_[:], AF.Silu, [b_ps, b_stF], [b_gs], scale=rstd1)
                yield thr_H(i)
                if i > 0:
                    yield ("need", ("hgT", i - 1))
                for half in range(2):
                    ps_, b_ps = proj(C_ZA + half * 512, 512)
                    act(sza[:, half * 512:(half + 1) * 512], ps_[:], AF.Sigmoid, [b_ps, b_stF], [b_sza], scale=rstd1)
                    yield
                for half in range(2):
                    ps_, b_ps = proj(C_ZB + half * 512, 512)
                    act(szb[:, half * 512:(half + 1) * 512], ps_[:], AF.Sigmoid, [b_ps, b_stF], [b_szb], scale=rstd1)
                    yield

            def thr_Y(i, slot):
                for half in range(2):
                    hs = slice(half * 512, (half + 1) * 512)
                    for c in range(4):
                        mm(mmP[half][:], aT[:, c, :], wba[:, c, hs], c == 0, c == 3, [b_aT, b_wba], [b_mm[half]])
                    tt("dve", m1[:, hs], mmP[half][:], sza[:, hs], ALU.mult, [b_mm[half], b_sza], [b_m1])
                    yield
                yield ("set", ("aT", i))
                for half in range(2):
                    hs = slice(half * 512, (half + 1) * 512)
                    for c in range(4):
                        mm(mmP[half][:], hgT[:, c, :], wbh[:, c, hs], c == 0, c == 3, [b_hgT, b_wbh], [b_mm[half]])
                    tt("dve", junkY[:, hs], mmP[half][:], szb[:, hs], ALU.mult, [b_mm[half], b_szb], [b_junkY])
                    tt("pool", mixed[:, hs], junkY[:, hs], m1[:, hs], ALU.add, [b_junkY, b_m1], [b_mixed])
                    yield
                yield ("set", ("hgT", i))
                for c in range(8):
                    tr(tpB[:, c * 128:(c + 1) * 128], mixed[:, c * 128:(c + 1) * 128], ident_b, [b_mixed, b_cb], [b_tpB])
                cp("dve", mT[:].rearrange("p c t -> p (c t)"), tpB[:], [b_tpB], [b_mT])
                yield
                for half in range(2):
                    hs = slice(half * 512, (half + 1) * 512)
                    for c in range(8):
                        mm(mmP[half][:], mT[:, c, :], wout[:, c, hs], c == 0, c == 7, [b_mT, b_wout], [b_mm[half]])
                    tt("dve", xt[slot][:, hs], xt[slot][:, hs], mmP[half][:], ALU.add, [b_xt[slot], b_mm[half]], [b_xt[slot]])
                    yield
                dma("sp", out_d[i * 128:(i + 1) * 128, :], xt[slot][:], sem_x1, [b_xt[slot]], [b_out])
                mset("dve", stY[:, 0:1], 0.0, [b_stY])
                act(junkY[:], xt[slot][:], AF.Square, [b_xt[slot]], [b_junkY, b_stY], scale=1.0 / 32, accum_out=stY[:, 0:1])
                rstd_of(stY, b_stY, slice(1, 2), slice(0, 1))
                rstd2 = stY[:, 1:2]
                stt("dve", hb[:], xt[slot][:], rstd2, n2w, ALU.mult, ALU.mult, [b_xt[slot], b_stY, b_pp], [b_hb])
                yield
                x1T = m1[:].rearrange("p (c t) -> p c t", t=128)
                for g4 in range(2):
                    for c in range(4):
                        tr(mmP[g4][:, c * 128:(c + 1) * 128], xt[slot][:, (g4 * 4 + c) * 128:(g4 * 4 + c + 1) * 128], ident_f,
                           [b_xt[slot], b_cf], [b_mm[g4]])
                    cp("act" if g4 == 0 else "dve", m1[:, g4 * 512:(g4 + 1) * 512], mmP[g4][:], [b_mm[g4]], [b_m1])
                    yield
                for c in range(8):
                    mm(mmP[0][:, 0:36], x1T[:, c, :], wr[:, c, :], c == 0, c == 7, [b_m1, b_wr], [b_mm[0]])
                L = rt[:, 0:36]
                stt("dve", L, mmP[0][:, 0:36], rstd2, brt, ALU.mult, ALU.add, [b_mm[0], b_stY, b_pp], [b_rt])
                yield
                R = [b_rt]
                gmax = rt[:, 36:37]; ngmax = rt[:, 37:38]; gsum = rt[:, 38:39]; pg = rt[:, 39:40]
                red("dve", gmax, L[:, 0:4], ALU.max, R, R)
                ts("dve", ngmax, gmax, -1.0, None, ALU.mult, None, R, R)
                mset("dve", gsum, 0.0, R)
                act(rt[:, 40:44], L[:, 0:4], AF.Exp, R, R, bias=ngmax, accum_out=gsum)
                P.op("dve", lambda e, pg=pg, gsum=gsum: e.reciprocal(out=pg, in_=gsum), R, R)
                gm = rt[:, 44:48]
                ts("dve", gm, L[:, 0:4], gmax, None, ALU.is_ge, None, R, R)
                ts("dve", gm, gm, 1e30, -1e30, ALU.mult, ALU.add, R, R)
                yield
                lem = rt[:, 48:80]
                tt("dve", lem.rearrange("p (g j) -> p g j", j=8), L[:, 4:36].rearrange("p (g j) -> p g j", j=8),
                   bc(gm, [128, 4, 8], 2), ALU.add, R, R)
                top8 = rt[:, 80:88]
                P.op("dve", lambda e, top8=top8, lem=lem: e.max(out=top8, in_=lem), R, R)
                sel = rt[:, 88:120]; is1 = rt[:, 120:152]; isB = rt[:, 152:184]
                ts("dve", sel, lem, top8[:, 1:2], None, ALU.is_ge, None, R, R)
                ts("dve", is1, lem, top8[:, 0:1], None, ALU.is_ge, None, R, R)
                tt("dve", isB, sel, is1, ALU.subtract, R, R)
                yield
                dd = rt[:, 184:185]; s1_ = rt[:, 185:186]
                tt("dve", dd, top8[:, 0:1], top8[:, 1:2], ALU.subtract, R, R)
                act(s1_, dd, AF.Sigmoid, R, R)
                tt("dve", gate[:, 0, i:i + 1], pg, s1_, ALU.mult, R, [b_gate])
                tt("dve", gate[:, 1, i:i + 1], pg, gate[:, 0, i:i + 1], ALU.subtract, R + [b_gate], [b_gate])
                cp("dve", selb[:], sel, R, [b_selb])
                yield
                mm(mmP[1][:, 0:32], stri_b, selb[:], True, False, [b_cb, b_selb], [b_mm[1]])
                mm(mmP[1][:, 0:32], ones_b, cumsb[:], False, True, [b_cb, b_cumsb], [b_mm[1]])
                tt("dve", cums[:], cums[:], sel, ALU.add, R + [b_cums], [b_cums])
                cp("dve", cumsb[:], cums[:], [b_cums], [b_cumsb])
                posc = rt[:, 192:224]
                ts("dve", posc, mmP[1][:, 0:32], float(CAP - 1), None, ALU.min, None, [b_mm[1]], R)
                yield
                tt("dve", posc, posc, iotae, ALU.add, R + [b_cf], R)
                tmpm = rt[:, 224:256]
                dA = rt[:, 186:187]; dB = rt[:, 187:188]
                tt("dve", tmpm, posc, is1, ALU.mult, R, R)
                red("dve", dA, tmpm, ALU.add, R, R)
                tt("dve", tmpm, posc, isB, ALU.mult, R, R)
                red("dve", dB, tmpm, ALU.add, R, R)
                cp("dve", dest[:, 0, i:i + 1], dA, R, [b_dest])
                cp("dve", dest[:, 1, i:i + 1], dB, R, [b_dest])
                yield
                for k in range(2):
                    off = dest[:, k, i:i + 1]
                    P.dma("pool", lambda e, off=off: e.indirect_dma_start(
                        out=xbuf_d, out_offset=bass.IndirectOffsetOnAxis(ap=off, axis=0), in_=hb[:, :], in_offset=None),
                        sem_sc, [b_hb, b_dest, b_xbuf], [Buf("scat")])

            sem_cv = P.newsem()
            cv_next = [0]

            def convert_experts(upto):
                while cv_next[0] < min(upto, NEX):
                    e = cv_next[0]
                    cv_next[0] += 1
                    for src, dst in ((wg_d, wgb_d), (wu_d, wub_d), (wd_d, wdb_d)):
                        s2_ = src[e].rearrange("a b -> (a b)").rearrange("(p f) -> p f", p=128)
                        d2_ = dst[e].rearrange("a b -> (a b)").rearrange("(p f) -> p f", p=128)
                        dma("pool", d2_, s2_, sem_cv, [], [Buf("cv")])

            for i in range(NT):
                gi = NPRE + i
                if i + 1 < NT:
                    load_x(x_own, i + 1, (gi + 1) % 3)
                convert_experts(((i + 1) * 32 + NT - 1) // NT)
                thr = []
                if i > 0:
                    thr.append(thr_Y(i - 1, (gi - 1) % 3))
                thr.append(thr_XF(i, gi % 3))
                run_threads(thr)
            run_threads([thr_Y(NT - 1, (NPRE + NT - 1) % 3)])
            P.barrier()
            P.emit_block()
        if mode in (1, 2, 3, 4):
            return nc

        with ExitStack() as s2:
            NWB = 2
            wg = [SB(s2, "wg%d" % k, [128, 8, 512], BF16) for k in range(NWB)]
            wu = [SB(s2, "wu%d" % k, [128, 8, 512], BF16) for k in range(NWB)]
            wd = [SB(s2, "wd%d" % k, [128, 4, D], BF16) for k in range(NWB)]
            b_w = [Buf("w%d" % k) for k in range(NWB)]
            sem_e = [P.newsem() for _ in range(NWB)]
            xg = [SB(s2, "xg%d" % k, [128, NB, D], BF16) for k in range(2)]; b_xg = [Buf("xg0"), Buf("xg1")]
            sem_xg = [P.newsem(), P.newsem()]
            xgT = SB(s2, "xgT", [128, 8, CAP], BF16); b_xgT = Buf("xgT")
            sg = [SB(s2, "sg%d" % k, [128, CAP], F32) for k in range(2)]; b_sg = [Buf("sg0"), Buf("sg1")]
            hT = SB(s2, "hT", [128, 4, CAP], BF16); b_hT = Buf("hT")
            ysb = [SB(s2, "ysb%d" % k, [128, D], F32) for k in range(2)]; b_ysb = [Buf("ysb0"), Buf("ysb1")]
            sem_y = [P.newsem(), P.newsem()]
            NP3 = 6
            x1t = [SB(s2, "x1t%d" % k, [128, D], F32) for k in range(NP3)]; b_x1t = [Buf("x1t%d" % k) for k in range(NP3)]
            yA = [SB(s2, "yA%d" % k, [128, D], F32) for k in range(NP3)]; b_yA = [Buf("yA%d" % k) for k in range(NP3)]
            yB = [SB(s2, "yB%d" % k, [128, D], F32) for k in range(NP3)]; b_yB = [Buf("yB%d" % k) for k in range(NP3)]
            sem_l = [P.newsem() for _ in range(NP3)]
            sem_ga = [P.newsem() for _ in range(NP3)]
            sem_gb = [P.newsem() for _ in range(NP3)]
            sem_o = [P.newsem() for _ in range(NP3)]
            tp2s = [PS(s2, "tp2%d" % k, [128, 1024], BF16) for k in range(2)]; b_tp2s = [Buf("tp20", True), Buf("tp21", True)]
            tcount = [0]
            pg_ = [PS(s2, "pg%d" % k, [128, 512], F32) for k in range(2)]; b_pg = [Buf("pg0", True), Buf("pg1", True)]
            pu_ = [PS(s2, "pu%d" % k, [128, 512], F32) for k in range(2)]; b_pu = [Buf("pu0", True), Buf("pu1", True)]
            py_ = [PS(s2, "py%d" % k, [128, 512], F32) for k in range(2)]; b_py = [Buf("py0", True), Buf("py1", True)]

            def load_w(e):
                k = e % NWB
                hws = []
                dma("pool", wg[k][:], wgb_d[e].rearrange("(c p) n -> p c n", p=128), sem_e[k], [], [b_w[k]])
                P.dma("pool", lambda en, k=k, e=e: en.dma_start(out=wu[k][:], in_=wub_d[e].rearrange("(c p) n -> p c n", p=128)), sem_e[k], [], [Buf("t")])
                h = P.dma("pool", lambda en, k=k, e=e: en.dma_start(out=wd[k][:], in_=wdb_d[e].rearrange("(c p) n -> p c n", p=128)), sem_e[k], [], [Buf("t")])
                b_w[k].last_w = h

            def load_xg(e):
                k = e % 2
                dma("sp", xg[k][:], xbuf_d[e * CAP:(e + 1) * CAP, :].rearrange("(b p) d -> p b d", p=128), sem_xg[k], [b_xbuf], [b_xg[k]])

            load_w(0)
            load_xg(0)
            ycount = 0
            for e in range(32):
                k = e % NWB
                kx = e % 2
                if e + 1 < 32:
                    load_w(e + 1)
                    load_xg(e + 1)
                for sb_ in range(NB):
                    tp2 = tp2s[tcount[0] % 2]; b_tp2 = b_tp2s[tcount[0] % 2]
                    tcount[0] += 1
                    for c in range(8):
                        tr(tp2[:, c * 128:(c + 1) * 128], xg[kx][:, sb_, c * 128:(c + 1) * 128], ident_b, [b_xg[kx], b_cb], [b_tp2])
                    cp("dve" if sb_ % 2 == 0 else "act", xgT[:, :, sb_ * 128:(sb_ + 1) * 128], tp2[:].rearrange("p (c t) -> p c t", t=128), [b_tp2], [b_xgT])
                for fc in range(4):
                    a_ = fc % 2
                    for c in range(8):
                        mm(pg_[a_][:, 0:CAP], wg[k][:, c, fc * 128:(fc + 1) * 128], xgT[:, c, :], c == 0, c == 7, [b_w[k], b_xgT], [b_pg[a_]])
                    for c in range(8):
                        mm(pu_[a_][:, 0:CAP], wu[k][:, c, fc * 128:(fc + 1) * 128], xgT[:, c, :], c == 0, c == 7, [b_w[k], b_xgT], [b_pu[a_]])
                    act(sg[a_][:], pg_[a_][:, 0:CAP], AF.Silu, [b_pg[a_]], [b_sg[a_]])
                    tt("dve", hT[:, fc, :], sg[a_][:], pu_[a_][:, 0:CAP], ALU.mult, [b_sg[a_], b_pu[a_]], [b_hT])
                for sb_ in range(NB):
                    ky = ycount % 2
                    ycount += 1
                    for nh in range(2):
                        for fc in range(4):
                            mm(py_[nh][:], hT[:, fc, sb_ * 128:(sb_ + 1) * 128], wd[k][:, fc, nh * 512:(nh + 1) * 512], fc == 0, fc == 3,
                               [b_hT, b_w[k]], [b_py[nh]])
                        cp("act" if nh == 0 else "dve", ysb[ky][:, nh * 512:(nh + 1) * 512], py_[nh][:], [b_py[nh]], [b_ysb[ky]])
                    r0 = e * CAP + sb_ * 128
                    dma("sp", ybuf_d[r0:r0 + 128, :], ysb[ky][:], sem_y[ky], [b_ysb[ky]], [Buf("yst%d" % ky)])
            P.barrier()

            def p3_fetch(i):
                k = i % NP3
                dma("sp", x1t[k][:], out_d[i * 128:(i + 1) * 128, :], sem_l[k], [b_out], [b_x1t[k]])
                offA = dest[:, 0, i:i + 1]
                offB = dest[:, 1, i:i + 1]
                P.dma("pool", lambda en, off=offA, dst=yA[k]: en.indirect_dma_start(
                    out=dst[:, :], out_offset=None, in_=ybuf_d, in_offset=bass.IndirectOffsetOnAxis(ap=off, axis=0)),
                    sem_ga[k], [b_dest], [b_yA[k]])
                P.dma("pool", lambda en, off=offB, dst=yB[k]: en.indirect_dma_start(
                    out=dst[:, :], out_offset=None, in_=ybuf_d, in_offset=bass.IndirectOffsetOnAxis(ap=off, axis=0)),
                    sem_gb[k], [b_dest], [b_yB[k]])

            for i in range(min(NP3 - 1, NT)):
                p3_fetch(i)
            for i in range(NT):
                k = i % NP3
                if i + NP3 - 1 < NT:
                    p3_fetch(i + NP3 - 1)
                stt("dve", x1t[k][:], yA[k][:], gate[:, 0, i:i + 1], x1t[k][:], ALU.mult, ALU.add, [b_yA[k], b_gate, b_x1t[k]], [b_x1t[k]])
                stt("dve", x1t[k][:], yB[k][:], gate[:, 1, i:i + 1], x1t[k][:], ALU.mult, ALU.add, [b_yB[k], b_gate, b_x1t[k]], [b_x1t[k]])
                dma("act", out_d[i * 128:(i + 1) * 128, :], x1t[k][:], sem_o[k], [b_x1t[k]], [Buf("ost%d" % k)])
            P.barrier()
            P.emit_block()
    return nc


def _consts(CAP):
    cf = np.zeros((128, K_END), np.float32)
    cf[:, K_ID:K_ID + 128] = np.eye(128, dtype=np.float32)
    s = np.arange(128)[:, None]
    t = np.arange(128)[None, :]
    cf[:, K_TRI:K_TRI + 128] = ((s <= t) & ((s // 64) == (t // 64))).astype(np.float32)
    cf[63, K_SEL2] = 1.0
    cf[127, K_SEL2 + 1] = 1.0
    invf = (500000.0 ** (-np.arange(0, 16, 2, dtype=np.float32) / 16)).astype(np.float32)
    cf[:, K_INVF:K_INVF + 8] = invf[None, :]
    cf[:, K_IOTA:K_IOTA + 32] = (np.arange(32, dtype=np.float32) * CAP)[None, :]
    cb = np.zeros((128, B_END), np.float32)
    cb[:, B_ID:B_ID + 128] = np.eye(128, dtype=np.float32)
    cb[:, B_STRI:B_STRI + 128] = (s < t).astype(np.float32)
    cb[:, B_ONES:B_ONES + 128] = 1.0
    mcur = np.where(s <= t, 0.0, NEG).astype(np.float32)
    mprev = np.where(s > t, 0.0, NEG).astype(np.float32)
    cb[:, B_MCUR:B_MCUR + 512] = np.tile(mcur, (1, 4))
    cb[:, B_MPREV:B_MPREV + 512] = np.tile(mprev, (1, 4))
    return cf, cb, mprev


def make_in_maps(inp, NT, CAP, n_cores=8):
    x = np.asarray(inp["x"], np.float32)
    B, S, _ = x.shape
    T = NT * 128
    assert S == 2 * T and B * 2 == n_cores
    pos = np.asarray(inp["positions"]).astype(np.int32)
    cf, cb0, mprev = _consts(CAP)
    w_in = np.asarray(inp["w_in"], np.float32)[0]
    qperm = np.concatenate([np.arange(64) + (g * 4 + j) * 64 for j in range(4) for g in range(2)])
    w_in_p = np.ascontiguousarray(np.concatenate([w_in[:, :512][:, qperm], w_in[:, 512:]], axis=1))
    f = lambda k: np.ascontiguousarray(np.asarray(inp[k], np.float32)[0])
    pp = np.zeros((128, Q_END), np.float32)
    pp[:, Q_N1C:Q_N1C + 8] = f("norm1_w").reshape(8, 128).T
    pp[:, Q_N2C:Q_N2C + 8] = f("norm2_w").reshape(8, 128).T
    pp[:, Q_N2:Q_N2 + 1024] = f("norm2_w")[None, :]
    pp[:, Q_QW:Q_QW + 512] = np.tile(f("q_norm_w"), 8)[None, :]
    pp[:, Q_KW:Q_KW + 128] = np.tile(f("k_norm_w"), 2)[None, :]
    pp[:, Q_HW:Q_HW + 512] = np.tile(f("hgrn_norm_w"), 4)[None, :]
    hlb = np.asarray(inp["hgrn_lower_bounds"], np.float32)
    hlbt = np.ascontiguousarray(np.tile(hlb.reshape(1, 1024), (128, 1)))
    pp[:, Q_SINK:Q_SINK + 8] = f("attn_sinks")[None, :]
    pp[:, Q_BRT:Q_BRT + 4] = f("b_router_group")[None, :]
    pp[:, Q_BRT + 4:Q_BRT + 36] = f("b_router_expert")[None, :]
    w_r = np.ascontiguousarray(np.concatenate([f("w_router_group"), f("w_router_expert")], axis=1))
    shared = {
        "cf32": cf, "pp32": pp, "hlb": hlbt, "w_in": w_in_p, "w_ba": f("w_branch_attn"), "w_bh": f("w_branch_hgrn"),
        "w_out": f("w_out"), "w_r": w_r, "w_g": f("w_gate_experts"), "w_u": f("w_up_experts"), "w_d": f("w_down_experts"),
    }
    maps = []
    for c in range(n_cores):
        b, half = c // 2, c % 2
        cb = cb0.copy()
        if half == 1:
            cb[:, B_MHALO:B_MHALO + 512] = np.tile(mprev, (1, 4))
            x_pre = np.ascontiguousarray(x[b, 0:T])
            halo_pos = pos[b, T - 128:T]
        else:
            cb[:, B_MHALO:B_MHALO + 512] = NEG
            x_pre = np.zeros((T, D), np.float32)
            halo_pos = np.zeros(128, np.int32)
        own = slice(half * T, (half + 1) * T)
        pt = np.concatenate([pos[b, own].reshape(NT, 128).T, halo_pos[:, None]], axis=1).astype(np.int32)
        m = dict(shared)
        m.update({"x_own": np.ascontiguousarray(x[b, own]), "x_pre": x_pre, "pos": np.ascontiguousarray(pt), "cb16": cb})
        maps.append(m)
    return maps


_NC_CACHE = {}


def run(inp, NT, CAP, mode=0, raw=False):
    key = (NT, CAP, mode)
    if key not in _NC_CACHE:
        _NC_CACHE[key] = build(NT, CAP, mode=mode)
    nc = _NC_CACHE[key]
    maps = make_in_maps(inp, NT, CAP)
    if mode != 0:
        for m in maps:
            for k in ("w_g", "w_u", "w_d"):
                m[k] = np.ascontiguousarray(m[k][:1])
    res = run_bass_kernel_spmd(nc, maps, core_ids=list(range(8)))
    if raw:
        return res.results
    x = np.asarray(inp["x"])
    B, S, _ = x.shape
    T = NT * 128
    out = np.empty((B, S, D), np.float32)
    for c in range(8):
        b, half = c // 2, c % 2
        out[b, half * T:(half + 1) * T] = res.results[c]["out"]
    return out


def kernel(**inputs):
    return run(inputs, 32, 384)
```

```python
import math
import os
from contextlib import ExitStack
import numpy as np
import concourse.bass as bass
import concourse.mybir as mybir
from concourse.bass_utils import run_bass_kernel_spmd

F32 = mybir.dt.float32
BF16 = mybir.dt.bfloat16
I32 = mybir.dt.int32
ALU = mybir.AluOpType
AF = mybir.ActivationFunctionType
AX = mybir.AxisListType

D = 1024
NEG = -30000.0
EPS = 1e-6
TWO_PI = 2.0 * math.pi


class Buf:
    __slots__ = ("name", "last_w", "readers", "excl")

    def __init__(self, name, excl=False):
        self.name = name
        self.last_w = None
        self.readers = []
        self.excl = excl


class DmaSem:
    def __init__(self, key):
        self.key = key
        self.count = 0


class Prog:
    ENGS = ("pe", "act", "dve", "pool", "sp")
    EMAP = {"pe": "tensor", "act": "scalar", "dve": "vector", "pool": "gpsimd", "sp": "sync"}

    def __init__(self, nc, stack, n_dma_sems=64):
        self.nc = nc
        self.ops = {e: [] for e in self.ENGS}
        self.n = {e: 0 for e in self.ENGS}
        self.waited = {e: {} for e in self.ENGS}
        self.dma_sems = [DmaSem("d%d" % i) for i in range(n_dma_sems)]
        self.sems = {}
        for e in self.ENGS:
            self.sems[e] = stack.enter_context(nc.semaphore("s_" + e))
        for d in self.dma_sems:
            self.sems[d.key] = stack.enter_context(nc.semaphore("s_" + d.key))
        self._next = 0
        self.tfree = {e: 0.0 for e in self.ENGS}
        self.hfin = {}
        self.step_max = 0.0
        self.act_grp = None

    def _time(self, eng, deps, h, cost):
        t = self.tfree[eng]
        for d in deps:
            f = self.hfin.get(d)
            if f is not None:
                f = f + (0.05 if d[0] == eng else 0.2)
                if f > t:
                    t = f
        fin = t + cost
        if h[0] in self.ENGS:
            self.tfree[eng] = fin
        else:
            self.tfree[eng] = t + 0.1
        self.hfin[h] = fin
        if fin > self.step_max:
            self.step_max = fin

    def newsem(self):
        s = self.dma_sems[self._next]
        self._next += 1
        return s

    def _deps(self, eng, reads, writes):
        deps = set()
        for b in reads:
            if b.last_w is not None:
                deps.add(b.last_w)
        for b in writes:
            if b.last_w is not None:
                deps.add(b.last_w)
            deps.update(b.readers)
        w = self.waited[eng]
        best = {}
        for (sk, v) in deps:
            if eng == "pe" and sk == "pe":
                continue
            if w.get(sk, 0) < v and best.get(sk, 0) < v:
                best[sk] = v
        for sk, v in best.items():
            self.ops[eng].append(("wait", sk, v))
            w[sk] = v
        return deps

    def _mark(self, h, reads, writes):
        for b in reads:
            b.readers.append(h)
            if len(b.readers) > 64:
                b.readers = b.readers[-48:]
        for b in writes:
            b.last_w = h
            b.readers = []

    def op(self, eng, fn, reads=(), writes=(), cost=0.3):
        ex = [b for b in reads if b.excl and b not in writes]
        if ex:
            writes = list(writes) + ex
        deps = self._deps(eng, reads, writes)
        self.n[eng] += 1
        h = (eng, self.n[eng])
        self.ops[eng].append(("op", fn))
        self._time(eng, deps, h, cost)
        self._mark(h, reads, writes)
        return h

    def dma(self, eng, fn, sem, reads=(), writes=(), cost=3.0):
        deps = self._deps(eng, reads, writes)
        sem.count += 16
        h = (sem.key, sem.count)
        self.ops[eng].append(("dma", fn, sem.key))
        self._time(eng, deps, h, cost)
        self._mark(h, reads, writes)
        return h

    def wait_all(self, eng, bufs):
        self._deps(eng, bufs, ())

    def barrier(self):
        for e in self.ENGS:
            w = self.waited[e]
            for f in self.ENGS:
                if f != e and self.n[f] > w.get(f, 0):
                    self.ops[e].append(("wait", f, self.n[f]))
                    w[f] = self.n[f]
            for d in self.dma_sems:
                if d.count > w.get(d.key, 0):
                    self.ops[e].append(("wait", d.key, d.count))
                    w[d.key] = d.count

    def emit_block(self):
        sems = self.sems
        with self.nc.Block() as block:
            def mk(e):
                items = self.ops[e]

                def body(engobj):
                    for item in items:
                        if item[0] == "wait":
                            engobj.wait_ge(sems[item[1]], item[2])
                        elif item[0] == "op":
                            item[1](engobj).then_inc(sems[e], 1)
                        else:
                            item[1](engobj).then_inc(sems[item[2]], 16)
                return body
            for e in self.ENGS:
                if self.ops[e]:
                    getattr(block, self.EMAP[e])(mk(e))
        self.ops = {e: [] for e in self.ENGS}


C_Q, C_KV, C_QH, C_FH, C_IH, C_GH, C_ZA, C_ZB = 0, 512, 768, 1280, 1792, 2304, 2816, 3840
NIN = 4864
K_ID, K_TRI, K_SEL2, K_INVF, K_IOTA, K_END = 0, 128, 256, 258, 266, 298
B_ID, B_STRI, B_ONES, B_MCUR, B_MPREV, B_MHALO, B_END = 0, 128, 256, 384, 896, 1408, 1920
Q_N2, Q_QW, Q_KW, Q_HW, Q_SINK, Q_BRT, Q_N1C, Q_N2C, Q_END = 0, 1024, 1536, 1664, 2176, 2184, 2220, 2228, 2236


def build(NT=32, CAP=384, NPRE=None, mode=0):
    if NPRE is None:
        NPRE = NT
    T = NT * 128
    NSLOT = 32 * CAP
    NB = CAP // 128
    nc = bass.Bass("TRN2", target_bir_lowering=False)

    def din(name, shape, dt=F32):
        return nc.dram_tensor(name, shape, dt, kind="ExternalInput").ap()

    x_own = din("x_own", [T, D])
    x_pre = din("x_pre", [NPRE * 128, D])
    pos_d = din("pos", [128, NT + 1], I32)
    cf_d = din("cf32", [128, K_END])
    cb_d = din("cb16", [128, B_END])
    pp_d = din("pp32", [128, Q_END])
    hlb_d = din("hlb", [128, 1024])
    win_d = din("w_in", [D, NIN])
    wba_d = din("w_ba", [512, D])
    wbh_d = din("w_bh", [512, D])
    wout_d = din("w_out", [D, D])
    wr_d = din("w_r", [D, 36])
    NEX = 32 if mode == 0 else 1
    wg_d = din("w_g", [NEX, D, 512])
    wu_d = din("w_u", [NEX, D, 512])
    wd_d = din("w_d", [NEX, 512, D])
    out_d = nc.dram_tensor("out", [T, D], F32, kind="ExternalOutput").ap()
    xbuf_d = nc.dram_tensor("xbuf", [NSLOT, D], BF16, kind="Internal").ap()
    ybuf_d = nc.dram_tensor("ybuf", [NSLOT, D], F32, kind="Internal").ap()
    wgb_d = nc.dram_tensor("wgb", [NEX, D, 512], BF16, kind="Internal").ap()
    wub_d = nc.dram_tensor("wub", [NEX, D, 512], BF16, kind="Internal").ap()
    wdb_d = nc.dram_tensor("wdb", [NEX, 512, D], BF16, kind="Internal").ap()

    with ExitStack() as top:
        P = Prog(nc, top)

        def SB(st, name, shape, dt):
            return st.enter_context(nc.sbuf_tensor(name, shape, dt))

        def PS(st, name, shape, dt):
            return st.enter_context(nc.psum_tensor(name, shape, dt))

        def nfree(ap):
            n = 1
            for d in ap.shape[1:]:
                n *= d
            return n

        def ecost(eng, ap):
            n = nfree(ap)
            if eng == "pool":
                return 0.12 + n * 0.0022
            if eng == "act":
                return 0.2 + n * 0.00095
            return 0.07 + n * 0.00123

        AGRP = {AF.Exp: 1, AF.Ln: 1, AF.Sigmoid: 2, AF.Silu: 3, AF.Sin: 4}

        def mm(out, lhsT, rhs, start, stop, r, w):
            c = max(0.064, 0.00042 * nfree(rhs))
            if lhsT.dtype == F32:
                c *= 4
            return P.op("pe", lambda e: e.matmul(out=out, lhsT=lhsT, rhs=rhs, start=start, stop=stop), r, w, cost=c)

        def tr(out, in_, ident, r, w):
            c = 0.11 * (4 if in_.dtype == F32 else 1)
            return P.op("pe", lambda e: e.transpose(out=out, in_=in_, identity=ident), r, w, cost=c)

        def act(out, in_, func, r, w, **kw):
            c = ecost("act", in_)
            g = AGRP.get(func)
            if g is not None and g != P.act_grp:
                c += 1.3
                P.act_grp = g
            return P.op("act", lambda e: e.activation(out=out, in_=in_, func=func, **kw), r, w, cost=c)

        def tt(eng, out, in0, in1, op, r, w):
            return P.op(eng, lambda e: e.tensor_tensor(out=out, in0=in0, in1=in1, op=op), r, w, cost=ecost(eng, out))

        def ts(eng, out, in0, s1, s2, op0, op1, r, w):
            if op1 is None:
                return P.op(eng, lambda e: e.tensor_scalar(out=out, in0=in0, scalar1=s1, scalar2=None, op0=op0), r, w, cost=ecost(eng, out))
            return P.op(eng, lambda e: e.tensor_scalar(out=out, in0=in0, scalar1=s1, scalar2=s2, op0=op0, op1=op1), r, w, cost=ecost(eng, out))

        def stt(eng, out, in0, scalar, in1, op0, op1, r, w):
            return P.op(eng, lambda e: e.scalar_tensor_tensor(out=out, in0=in0, scalar=scalar, in1=in1, op0=op0, op1=op1), r, w, cost=ecost(eng, out))

        def cp(eng, out, in_, r, w):
            if eng == "act":
                return P.op("act", lambda e: e.copy(out=out, in_=in_), r, w, cost=ecost(eng, out))
            return P.op(eng, lambda e: e.tensor_copy(out=out, in_=in_), r, w, cost=ecost(eng, out))

        def red(eng, out, in_, op, r, w):
            return P.op(eng, lambda e: e.tensor_reduce(out=out, in_=in_, axis=AX.X, op=op), r, w, cost=ecost(eng, in_))

        def mset(eng, ap, val, w):
            return P.op(eng, lambda e: e.memset(ap, val), (), w, cost=ecost(eng, ap))

        def dma(eng, out, in_, sem, r, w):
            return P.dma(eng, lambda e: e.dma_start(out=out, in_=in_), sem, r, w)

        def bc(ap, shape, axis):
            return ap.unsqueeze(axis).to_broadcast(shape)

        idb = SB(top, "idb", [128, B_END], BF16); b_cb = Buf("cb")
        cf = SB(top, "cf", [128, K_END], F32); b_cf = Buf("cf")
        dest = SB(top, "dest", [128, 2, NT], I32); b_dest = Buf("dest")
        gate = SB(top, "gate", [128, 2, NT], F32); b_gate = Buf("gate")
        b_xbuf = Buf("xbuf"); b_ybuf = Buf("ybuf"); b_out = Buf("out")
        ident_f = cf[:, K_ID:K_ID + 128]
        tri_f = cf[:, K_TRI:K_TRI + 128]
        sel2 = cf[:, K_SEL2:K_SEL2 + 2]
        invf = cf[:, K_INVF:K_INVF + 8]
        iotae = cf[:, K_IOTA:K_IOTA + 32]
        ident_b = idb[:, B_ID:B_ID + 128]
        stri_b = idb[:, B_STRI:B_STRI + 128]
        ones_b = idb[:, B_ONES:B_ONES + 128]
        mask_cur = idb[:, B_MCUR:B_MCUR + 512]
        mask_prev = idb[:, B_MPREV:B_MPREV + 512]
        mask_halo = idb[:, B_MHALO:B_MHALO + 512]

        sem_c = [P.newsem() for _ in range(4)]
        dma("sp", cf[:], cf_d, sem_c[0], [], [b_cf])
        dma("pool", idb[:], cb_d, sem_c[1], [], [b_cb])

        with ExitStack() as s1:
            Wp = SB(s1, "Wp", [128, 8, NIN], BF16); b_Wp = Buf("Wp")
            wba = SB(s1, "wba", [128, 4, D], BF16); b_wba = Buf("wba")
            wbh = SB(s1, "wbh", [128, 4, D], BF16); b_wbh = Buf("wbh")
            wout = SB(s1, "wout", [128, 8, D], BF16); b_wout = Buf("wout")
            wr = SB(s1, "wr", [128, 8, 36], F32); b_wr = Buf("wr")
            pp = SB(s1, "pp", [128, Q_END], F32); b_pp = Buf("pp")
            lbt = SB(s1, "lbt", [128, 2, 512], F32); b_lb = Buf("lb")
            esink = SB(s1, "esink", [128, 8], F32); b_esink = Buf("esink")
            posi = SB(s1, "posi", [128, NT + 1], I32); b_posi = Buf("posi")
            NA = (NT + 1) * 8
            trig = SB(s1, "trig", [128, 2, NA], F32); b_trig = Buf("trig")
            cst = SB(s1, "cst", [128, 4], F32); b_cst = Buf("cst")
            cos_t = trig[:, 0, :].rearrange("p (i j) -> p i j", j=8)
            sin_t = trig[:, 1, :].rearrange("p (i j) -> p i j", j=8)

            n2w = pp[:, Q_N2:Q_N2 + 1024]
            qw = pp[:, Q_QW:Q_QW + 512]
            kw = pp[:, Q_KW:Q_KW + 128]
            hw = pp[:, Q_HW:Q_HW + 512]
            brt = pp[:, Q_BRT:Q_BRT + 36]

            dma("sp", pp[:], pp_d, sem_c[2], [], [b_pp])
            dma("sp", posi[:], pos_d, sem_c[3], [], [b_posi])
            WG = [(C_FH, C_GH), (C_KV, C_QH), (C_Q, C_KV), (C_QH, C_FH), (C_GH, C_ZA), (C_ZA, NIN)]
            b_Wg = [Buf("Wg%d" % g) for g in range(len(WG))]
            win3 = win_d.rearrange("(c p) n -> p c n", p=128)
            for g, (c0, c1) in enumerate(WG):
                dma("pool", Wp[:, :, c0:c1], win3[:, :, c0:c1], P.newsem(), [], [b_Wg[g]])

            def wbuf(c0):
                for g, (a0, a1) in enumerate(WG):
                    if a0 <= c0 < a1:
                        return b_Wg[g]
                raise ValueError(c0)
            sem_w = P.newsem()
            dma("pool", wba[:], wba_d.rearrange("(c p) n -> p c n", p=128), sem_w, [], [Buf("wtmp")])
            dma("pool", wbh[:], wbh_d.rearrange("(c p) n -> p c n", p=128), sem_w, [], [Buf("wtmp")])
            hW = dma("pool", wout[:], wout_d.rearrange("(c p) n -> p c n", p=128), sem_w, [], [Buf("wtmp")])
            for b in (b_wba, b_wbh, b_wout):
                b.last_w = hW
            sem_wr = P.newsem()
            dma("sp", wr[:], wr_d.rearrange("(c p) n -> p c n", p=128), sem_wr, [], [b_wr])

            xt = [SB(s1, "xt%d" % k, [128, D], F32) for k in range(3)]; b_xt = [Buf("xt%d" % k) for k in range(3)]
            sem_x = [P.newsem() for _ in range(3)]
            junkF = SB(s1, "junkF", [128, D], BF16); b_junkF = Buf("junkF")
            junkY = SB(s1, "junkY", [128, D], BF16); b_junkY = Buf("junkY")
            stF = SB(s1, "stF", [128, 8], F32); b_stF = Buf("stF")
            stA = SB(s1, "stA", [128, 64], F32); b_stA = Buf("stA")
            stH = SB(s1, "stH", [128, 16], F32); b_stH = Buf("stH")
            stY = SB(s1, "stY", [128, 8], F32); b_stY = Buf("stY")
            xb = SB(s1, "xb", [128, D], BF16); b_xb = Buf("xb")
            xT = SB(s1, "xT", [128, 8, 128], BF16); b_xT = Buf("xT")
            qa = SB(s1, "qa", [128, 512], F32); b_qa = Buf("qa")
            kv = SB(s1, "kvf", [128, 128], F32); b_kv = Buf("kv")
            r16 = SB(s1, "r16", [128, 6, 8, 8], F32); b_r16 = Buf("r16")
            qr = SB(s1, "qr", [128, 512], BF16); b_qr = Buf("qr")
            kr = SB(s1, "kr", [128, 128], BF16); b_kr = Buf("kr")
            qT = SB(s1, "qT", [128, 4, 128], BF16); b_qT = Buf("qT")
            kTs = [SB(s1, "kT%d" % k, [128, 128], BF16) for k in range(2)]; b_kT = [Buf("kT0"), Buf("kT1")]
            vx = [SB(s1, "vx%d" % k, [128, 2, 80], BF16) for k in range(2)]; b_vx = [Buf("vx0"), Buf("vx1")]
            pTb = [SB(s1, "pT%d" % k, [128, 512], BF16) for k in range(2)]; b_pTb = [Buf("pT0"), Buf("pT1")]
            attn = SB(s1, "attn", [128, 512], BF16); b_attn = Buf("attn")
            aT = SB(s1, "aT", [128, 4, 128], BF16); b_aT = Buf("aT")
            qh = SB(s1, "qh", [128, 512], F32); b_qh = Buf("qh")
            hA = SB(s1, "hA", [128, 512], F32); b_hA = Buf("hA")
            hk = SB(s1, "hk", [128, 512], F32); b_hk = Buf("hk")
            eb = SB(s1, "eb", [128, 512], F32); b_eb = Buf("eb")
            iv = SB(s1, "iv", [128, 512], BF16); b_iv = Buf("iv")
            gs = SB(s1, "gs", [128, 512], F32); b_gs = Buf("gs")
            qt = SB(s1, "qt", [128, 512], BF16); b_qt = Buf("qt")
            kt = SB(s1, "kt", [128, 512], BF16); b_kt = Buf("kt")
            qlh = SB(s1, "qlh", [128, 4, 2, 128], BF16); b_qlh = Buf("qlh")
            ktT = SB(s1, "ktT", [128, 4, 128], BF16); b_ktT = Buf("ktT")
            Am = [SB(s1, "Am%d" % k, [128, 128], BF16) for k in range(2)]; b_Am = [Buf("Am0"), Buf("Am1")]
            S = SB(s1, "S", [128, 4, 128], F32); b_S = Buf("S")
            Sb = [SB(s1, "Sb%d" % k, [128, 4, 128], BF16) for k in range(2)]; b_Sb = [Buf("Sb0"), Buf("Sb1")]
            Dsb = SB(s1, "Dsb", [128, 4, 2], F32); b_Dsb = Buf("Dsb")
            hg = SB(s1, "hg", [128, 512], BF16); b_hg = Buf("hg")
            hgT = SB(s1, "hgT", [128, 4, 128], BF16); b_hgT = Buf("hgT")
            sza = SB(s1, "sza", [128, D], BF16); b_sza = Buf("sza")
            szb = SB(s1, "szb", [128, D], BF16); b_szb = Buf("szb")
            m1 = SB(s1, "m1", [128, D], F32); b_m1 = Buf("m1")
            mixed = SB(s1, "mixed", [128, D], BF16); b_mixed = Buf("mixed")
            mT = SB(s1, "mT", [128, 8, 128], BF16); b_mT = Buf("mT")
            hb = SB(s1, "hb", [128, D], BF16); b_hb = Buf("hb")
            rt = SB(s1, "rt", [128, 256], F32); b_rt = Buf("rt")
            cums = SB(s1, "cums", [128, 32], F32); b_cums = Buf("cums")
            cumsb = SB(s1, "cumsb", [128, 32], BF16); b_cumsb = Buf("cumsb")
            selb = SB(s1, "selb", [128, 32], BF16); b_selb = Buf("selb")
            sem_x1 = P.newsem()
            sem_sc = P.newsem()

            tp = PS(s1, "tp", [128, 1024], BF16); b_tp = Buf("tp", True)
            mmA = PS(s1, "mmA", [128, 512], F32); mmB = PS(s1, "mmB", [128, 512], F32)
            b_mm = [Buf("mmA", True), Buf("mmB", True)]; mmP = [mmA, mmB]
            sc = [PS(s1, "sc0", [128, 512], F32)]; b_sc = [Buf("sc0", True)]
            tpB = PS(s1, "tpB", [128, 1024], BF16); b_tpB = Buf("tpB", True)
            pv = [PS(s1, "pv0", [128, 512], F32), PS(s1, "pv1", [128, 512], F32)]; b_pv = [Buf("pv0", True), Buf("pv1", True)]
            misc = PS(s1, "misc", [128, 512], F32); b_misc = Buf("misc", True)

            mset("dve", qlh[:], 0.0, [b_qlh])
            mset("dve", S[:], 0.0, [b_S])
            mset("dve", Sb[0][:], 0.0, [b_Sb[0]])
            mset("dve", cums[:], 0.0, [b_cums])
            mset("dve", cumsb[:], 0.0, [b_cumsb])
            for k in range(2):
                mset("dve", vx[k][:], 0.0, [b_vx[k]])
                mset("dve", vx[k][:, :, 64:65], 1.0, [b_vx[k]])
            def scale_w(g):
                c0, c1 = WG[g]
                tt("dve", Wp[:, :, c0:c1], Wp[:, :, c0:c1], bc(pp[:, Q_N1C:Q_N1C + 8], [128, 8, c1 - c0], 2), ALU.mult,
                   [b_Wg[g], b_pp], [b_Wg[g]])
            scale_w(0)
            scale_w(1)
            pending_w = [2, 3, 4, 5]
            for c in range(8):
                ts("dve", wr[:, c, :], wr[:, c, :], pp[:, Q_N2C + c:Q_N2C + c + 1], None, ALU.mult, None, [b_wr, b_pp], [b_wr])
            zt = hb[:]; b_zt = b_hb
            ZC = 1024
            mset("pool", zt, 0.0, [b_zt])
            sem_z = P.newsem()
            xz = xbuf_d.rearrange("(p r) d -> p (r d)", p=128)
            tot = (NSLOT // 128) * D
            zchunks = [(k0, min(tot, k0 + ZC)) for k0 in range(0, tot, ZC)]

            def zero_some(n):
                for _ in range(n):
                    if zchunks:
                        k0, k1 = zchunks.pop(0)
                        b_xbuf.last_w = dma("sp", xz[:, k0:k1], zt[:, 0:k1 - k0], sem_z, [b_zt], [Buf("ztmp")])

            mset("dve", cst[:, 0:1], math.pi / 2, [b_cst])
            mset("dve", cst[:, 1:2], EPS, [b_cst])
            dma("sp", m1[:], hlb_d, P.newsem(), [], [b_m1])
            hl = m1[:].rearrange("p (a c) -> p a c", a=2)
            tt("dve", lbt[:, 0, :], hl[:, 0, :], hl[:, 1, :], ALU.subtract, [b_m1], [b_lb])
            act(lbt[:, 0, :], lbt[:, 0, :], AF.Sigmoid, [b_lb], [b_lb])
            ts("dve", lbt[:, 1, :], lbt[:, 0, :], -1.0, 1.0, ALU.mult, ALU.add, [b_lb], [b_lb])
            act(esink[:], pp[:, Q_SINK:Q_SINK + 8], AF.Exp, [b_pp], [b_esink])
            b_tg = Buf("trigtmp")
            tA = xt[1][:, 0:2 * NA].rearrange("p (a n) -> p a n", a=2)
            tB = xt[2][:, 0:2 * NA].rearrange("p (a n) -> p a n", a=2)
            TR = [b_tg, b_xt[1], b_xt[2]]
            trg = {0: tA[:, 0, :], 1: tA[:, 1, :], 2: tB[:, 0, :], 3: tB[:, 1, :], 4: trig[:, 0, :], 5: trig[:, 1, :]}
            posf = trg[0][:, 0:NT + 1]
            cp("dve", posf, posi[:], [b_posi], TR)
            ang = trg[1].rearrange("p (i j) -> p i j", j=8)
            tt("dve", ang, bc(posf, [128, NT + 1, 8], 2), bc(invf, [128, NT + 1, 8], 1), ALU.mult, TR + [b_cf], TR)
            kf = trg[2]
            ts("dve", kf, trg[1], 1.0 / TWO_PI, None, ALU.mult, None, TR, TR)
            ki = trg[3].bitcast(I32)
            cp("dve", ki, kf, TR, TR)
            cp("dve", kf, ki, TR, TR)
            r_ = trg[1]
            stt("dve", r_, kf, -TWO_PI, r_, ALU.mult, ALU.add, TR, TR)
            s4 = trg[2]
            c4 = trg[3]
            act(s4, r_, AF.Sin, TR, TR, scale=0.25)
            act(c4, r_, AF.Sin, TR + [b_cst], TR, scale=0.25, bias=cst[:, 0:1])
            s2 = trg[0]
            c2 = trg[1]
            stt("dve", s2, s4, 2.0, c4, ALU.mult, ALU.mult, TR, TR)
            tt("dve", c2, s4, s4, ALU.mult, TR, TR)
            ts("dve", c2, c2, -2.0, 1.0, ALU.mult, ALU.add, TR, TR)
            stt("dve", trg[5], s2, 2.0, c2, ALU.mult, ALU.mult, TR, [b_trig])
            tt("dve", trg[4], s2, s2, ALU.mult, TR, [b_trig])
            ts("dve", trg[4], trg[4], -2.0, 1.0, ALU.mult, ALU.add, [b_trig], [b_trig])


            mmc = [0]

            def next_mm():
                k = mmc[0] % 2
                mmc[0] += 1
                return mmP[k], b_mm[k]

            def rstd_of(stt_, b_s, col, src_col, scale=1.0):
                act(stt_[:, col], stt_[:, src_col], AF.Ln, [b_s, b_cst], [b_s], bias=cst[:, 1:2], scale=scale)
                act(stt_[:, col], stt_[:, col], AF.Exp, [b_s], [b_s], scale=-0.5)

            def load_x(src, i, slot):
                dma("sp", xt[slot][:], src[i * 128:(i + 1) * 128, :], sem_x[slot], [], [b_xt[slot]])

            rstd1 = stF[:, 1:2]

            def front(slot):
                mset("dve", stF[:, 0:1], 0.0, [b_stF])
                act(junkF[:], xt[slot][:], AF.Square, [b_xt[slot]], [b_junkF, b_stF], scale=1.0 / 32, accum_out=stF[:, 0:1])
                rstd_of(stF, b_stF, slice(1, 2), slice(0, 1))
                cp("act", xb[:], xt[slot][:], [b_xt[slot]], [b_xb])
                for c in range(8):
                    tr(tp[:, c * 128:(c + 1) * 128], xb[:, c * 128:(c + 1) * 128], ident_b, [b_xb, b_cb], [b_tp])
                cp("dve", xT[:].rearrange("p c t -> p (c t)"), tp[:], [b_tp], [b_xT])

            def proj(c0, n):
                ps_, b_ps = next_mm()
                for c in range(8):
                    mm(ps_[:, 0:n], xT[:, c, :], Wp[:, c, c0:c0 + n], c == 0, c == 7, [b_xT, wbuf(c0)], [b_ps])
                return ps_, b_ps

            def qknorm_rope(src, b_src, nh, wtile, dst, b_dst, ti):
                n = nh * 64
                s3 = src.rearrange("p (h d) -> p h d", d=64)
                d3 = dst.rearrange("p (h d) -> p h d", d=64)
                w3 = wtile.rearrange("p (h d) -> p h d", d=64)
                tt("pool", dst, src, src, ALU.mult, [b_src], [b_dst])
                red("dve", stA[:, 8:8 + nh], d3, ALU.add, [b_dst], [b_stA])
                rstd_of(stA, b_stA, slice(16, 16 + nh), slice(8, 8 + nh), scale=1.0 / 64)
                tt("dve", s3, s3, bc(stA[:, 16:16 + nh], [128, nh, 64], 2), ALU.mult, [b_src, b_stA], [b_src])
                tt("dve", dst, src, wtile, ALU.mult, [b_src, b_pp], [b_dst])
                x1 = r16[:, 0, 0:nh, :]; x2 = r16[:, 1, 0:nh, :]
                tt("dve", x1, s3[:, :, 0:8], w3[:, :, 0:8], ALU.mult, [b_src, b_pp], [b_r16])
                tt("dve", x2, s3[:, :, 8:16], w3[:, :, 8:16], ALU.mult, [b_src, b_pp], [b_r16])
                cs = bc(cos_t[:, ti, :], [128, nh, 8], 1)
                sn = bc(sin_t[:, ti, :], [128, nh, 8], 1)
                a_ = r16[:, 2, 0:nh, :]; b_ = r16[:, 3, 0:nh, :]; c_ = r16[:, 4, 0:nh, :]; d_ = r16[:, 5, 0:nh, :]
                tt("dve", a_, x1, cs, ALU.mult, [b_r16, b_trig], [b_r16])
                tt("dve", b_, x2, sn, ALU.mult, [b_r16, b_trig], [b_r16])
                tt("dve", c_, x2, cs, ALU.mult, [b_r16, b_trig], [b_r16])
                tt("dve", d_, x1, sn, ALU.mult, [b_r16, b_trig], [b_r16])
                tt("dve", d3[:, :, 0:8], a_, b_, ALU.subtract, [b_r16], [b_dst])
                tt("dve", d3[:, :, 8:16], c_, d_, ALU.add, [b_r16], [b_dst])

            def kv_proj(slot):
                ps_, b_ps = proj(C_KV, 256)
                act(kv[:], ps_[:, 0:128], AF.Copy, [b_ps, b_stF], [b_kv], scale=rstd1)
                act(vx[slot][:, :, 0:64], ps_[:, 128:256].rearrange("p (m d) -> p m d", d=64), AF.Copy, [b_ps, b_stF], [b_vx[slot]], scale=rstd1)

            def k_finish(ti, slot):
                qknorm_rope(kv[:], b_kv, 2, kw, kr[:], b_kr, ti)
                tr(tp[:, 0:128], kr[:], ident_b, [b_kr, b_cb], [b_tp])
                cp("dve", kTs[slot][:], tp[:, 0:128], [b_tp], [b_kT[slot]])

            HS = [(hA, b_hA, hk, b_hk, eb, b_eb, iv, b_iv, kt, b_kt),
                  (qa, b_qa, qh, b_qh, gs, b_gs, qr, b_qr, attn, b_attn)]

            def hgrn_proj(hs=0):
                hA, b_hA, hk, b_hk, eb, b_eb, iv, b_iv, kt, b_kt = HS[hs]
                ps_, b_ps = proj(C_FH, 512)
                act(hA[:], ps_[:], AF.Sigmoid, [b_ps, b_stF], [b_hA], scale=rstd1)
                ps2, b_ps2 = proj(C_IH, 512)
                act(iv[:], ps2[:], AF.Copy, [b_ps2, b_stF], [b_iv], scale=rstd1)

            def hgrn_chain(hs=0):
                hA, b_hA, hk, b_hk, eb, b_eb, iv, b_iv, kt, b_kt = HS[hs]
                tt("pool", hA[:], hA[:], lbt[:, 1, :], ALU.mult, [b_hA, b_lb], [b_hA])
                tt("pool", hA[:], hA[:], lbt[:, 0, :], ALU.add, [b_hA, b_lb], [b_hA])
                yield
                ts("dve", hk[:], hA[:], -1.0, 1.0, ALU.mult, ALU.add, [b_hA], [b_hk])
                act(hA[:], hA[:], AF.Ln, [b_hA], [b_hA])
                yield
                mm(pv[1][:], tri_f, hA[:], True, True, [b_cf, b_hA], [b_pv[1]])
                act(eb[:], pv[1][:], AF.Exp, [b_pv[1]], [b_eb])
                act(hA[:], pv[1][:], AF.Exp, [b_pv[1]], [b_hA], scale=-1.0)
                yield
                tt("dve", kt[:], hk[:], hA[:], ALU.mult, [b_hk, b_hA], [b_kt])
                for hh in range(4):
                    mm(misc[:, hh * 2:hh * 2 + 2], eb[:, hh * 128:(hh + 1) * 128], sel2, True, True, [b_eb, b_cf], [b_misc])
                cp("dve", Dsb[:].rearrange("p h c -> p (h c)"), misc[:, 0:8], [b_misc], [b_Dsb])
                yield

            def state_update(c, dst, hs=0):
                hA, b_hA, hk, b_hk, eb, b_eb, iv, b_iv, kt, b_kt = HS[hs]
                for hh in range(4):
                    mm(misc[:, hh * 128:(hh + 1) * 128], kt[c * 64:(c + 1) * 64, hh * 128:(hh + 1) * 128],
                       iv[c * 64:(c + 1) * 64, hh * 128:(hh + 1) * 128], True, True, [b_kt, b_iv], [b_misc])
                Sf = S[:].rearrange("p h d -> p (h d)")
                tt("dve", Sf, Sf, misc[:], ALU.add, [b_S, b_misc], [b_S])
                tt("dve", S[:], S[:], bc(Dsb[:, :, c], [128, 4, 128], 2), ALU.mult, [b_S, b_Dsb], [b_S])
                cp("act", Sb[dst][:], S[:], [b_S], [b_Sb[dst]])

            flags = set()

            def run_threads(threads):
                active = [[g, 0.0, None] for g in threads]
                while active:
                    cand = [a for a in active if a[2] is None or a[2] in flags]
                    assert cand, "scheduler deadlock"
                    a = cand[0]
                    active.remove(a)
                    active.append(a)
                    a[2] = None
                    P.step_max = 0.0
                    try:
                        r = next(a[0])
                    except StopIteration:
                        active.remove(a)
                        continue
                    if P.step_max > 0.0:
                        a[1] = P.step_max
                    if r is None:
                        continue
                    if isinstance(r, tuple):
                        if r[0] == "need":
                            a[2] = r[1]
                        else:
                            flags.add(r[1])
                    else:
                        active.append([r, a[1], None])

            load_x(x_pre if NPRE > 0 else x_own, 0, 0)
            def thr_PXF(j):
                front(j % 3)
                yield
                hgrn_proj(j % 2)
                yield
                if j == NPRE - 1:
                    kv_proj(1)
                    yield
                    k_finish(NT, 1)

            def thr_PH(j):
                for _ in hgrn_chain(j % 2):
                    yield
                state_update(0, 1, j % 2)
                yield
                state_update(1, 0, j % 2)

            for j in range(NPRE + 1):
                if j < NPRE:
                    if j + 1 < NPRE:
                        load_x(x_pre, j + 1, (j + 1) % 3)
                    else:
                        load_x(x_own, 0, (j + 1) % 3)
                thr = []
                if j > 0:
                    thr.append(thr_PH(j - 1))
                if j < NPRE:
                    thr.append(thr_PXF(j))
                run_threads(thr)
                if pending_w and j >= 3 and j % 2 == 1:
                    scale_w(pending_w.pop(0))
                if j >= 1:
                    zero_some(4)
            while pending_w:
                scale_w(pending_w.pop(0))
            zero_some(len(zchunks))

            def thr_A(i):
                ks = i % 2
                k_finish(i, ks)
                yield
                qknorm_rope(qa[:], b_qa, 8, qw, qr[:], b_qr, i)
                yield
                for j in range(4):
                    tr(tp[:, j * 128:(j + 1) * 128], qr[:, j * 128:(j + 1) * 128], ident_b, [b_qr, b_cb], [b_tp])
                cp("dve", qT[:].rearrange("p j t -> p (j t)"), tp[:, 0:512], [b_tp], [b_qT])
                yield
                for m in range(2):
                    for kb in range(2):
                        ksl = (1 - ks) if kb == 0 else ks
                        msk = (mask_halo if i == 0 else mask_prev) if kb == 0 else mask_cur
                        mm(sc[0][:], ident_b, msk, True, False, [b_cb], [b_sc[0]])
                        mm(sc[0][:], kTs[ksl][m * 64:(m + 1) * 64, :], qT[m * 64:(m + 1) * 64, :, :].rearrange("p j t -> p (j t)"),
                           False, True, [b_kT[ksl], b_qT], [b_sc[0]])
                        act(pTb[kb][:], sc[0][:], AF.Exp, [b_sc[0]], [b_pTb[kb]], scale=0.125)
                        yield
                    for j in range(4):
                        for kb in range(2):
                            ksl = (1 - ks) if kb == 0 else ks
                            mm(pv[0][:, j * 80:(j + 1) * 80], pTb[kb][:, j * 128:(j + 1) * 128], vx[ksl][:, m, :],
                               kb == 0, kb == 1, [b_pTb[kb], b_vx[ksl]], [b_pv[0]])
                    pv3 = pv[0][:, 0:320].rearrange("p (j d) -> p j d", d=80)
                    den = stA[:, 24 + m * 4:28 + m * 4]
                    tt("dve", den, pv3[:, :, 64], esink[:, m * 4:(m + 1) * 4], ALU.add, [b_pv[0], b_esink], [b_stA])
                    P.op("dve", lambda e, den=den: e.reciprocal(out=den, in_=den), [b_stA], [b_stA])
                    tt("dve", attn[:, m * 256:(m + 1) * 256].rearrange("p (j d) -> p j d", d=64), pv3[:, :, 0:64],
                       bc(den, [128, 4, 64], 2), ALU.mult, [b_pv[0], b_stA], [b_attn])
                    yield
                if i > 0:
                    yield ("need", ("aT", i - 1))
                for j in range(4):
                    tr(tp[:, j * 128:(j + 1) * 128], attn[:, j * 128:(j + 1) * 128], ident_b, [b_attn, b_cb], [b_tp])
                cp("dve", aT[:].rearrange("p j t -> p (j t)"), tp[:, 0:512], [b_tp], [b_aT])

            def thr_H(i):
                for _ in hgrn_chain():
                    yield
                tt("dve", qt[:], qh[:], eb[:], ALU.mult, [b_qh, b_eb], [b_qt])
                tt("pool", gs[:], gs[:], hw, ALU.mult, [b_gs, b_pp], [b_gs])
                yield
                for hh in range(4):
                    tr(tpB[:, hh * 128:(hh + 1) * 128], qt[:, hh * 128:(hh + 1) * 128], ident_b, [b_qt, b_cb], [b_tpB])
                    tr(tpB[:, 512 + hh * 128:512 + (hh + 1) * 128], kt[:, hh * 128:(hh + 1) * 128], ident_b, [b_kt, b_cb], [b_tpB])
                tq = tpB[:, 0:512].rearrange("p (h t) -> p h t", t=128)
                cp("dve", qlh[:, :, 0, 0:64], tq[:, :, 0:64], [b_tpB], [b_qlh])
                cp("dve", qlh[:, :, 1, 64:128], tq[:, :, 64:128], [b_tpB], [b_qlh])
                cp("act", ktT[:].rearrange("p h t -> p (h t)"), tpB[:, 512:1024], [b_tpB], [b_ktT])
                yield
                state_update(0, 1)
                yield
                for hh in range(4):
                    a_ = hh % 2
                    sca = misc[:, 128 + a_ * 128:256 + a_ * 128]
                    mm(sca, ktT[:, hh, :], qlh[:, hh, 0, :], True, False, [b_ktT, b_qlh], [b_misc])
                    mm(sca, ktT[:, hh, :], qlh[:, hh, 1, :], False, True, [b_ktT, b_qlh], [b_misc])
                    tt("dve", Am[a_][:], sca, tri_f, ALU.mult, [b_misc, b_cf], [b_Am[a_]])
                    o_ = pv[1][:, hh * 128:(hh + 1) * 128]
                    mm(o_, Am[a_][:], iv[:, hh * 128:(hh + 1) * 128], True, False, [b_Am[a_], b_iv], [b_pv[1]])
                    mm(o_, qlh[:, hh, 0, :], Sb[0][:, hh, :], False, False, [b_qlh, b_Sb[0]], [b_pv[1]])
                    mm(o_, qlh[:, hh, 1, :], Sb[1][:, hh, :], False, True, [b_qlh, b_Sb[1]], [b_pv[1]])
                    yield
                state_update(1, 0)
                yield
                o3 = pv[1][:].rearrange("p (h d) -> p h d", d=128)
                act(hk[:], pv[1][:], AF.Square, [b_pv[1]], [b_hk])
                red("dve", stH[:, 0:4], hk[:].rearrange("p (h d) -> p h d", d=128), ALU.add, [b_hk], [b_stH])
                rstd_of(stH, b_stH, slice(4, 8), slice(0, 4), scale=1.0 / 128)
                tt("dve", qh[:].rearrange("p (h d) -> p h d", d=128), o3, bc(stH[:, 4:8], [128, 4, 128], 2), ALU.mult,
                   [b_pv[1], b_stH], [b_qh])
                yield
                tt("dve", hg[:], qh[:], gs[:], ALU.mult, [b_qh, b_gs], [b_hg])
                if i > 0:
                    yield ("need", ("hgT", i - 1))
                for j in range(4):
                    tr(tpB[:, j * 128:(j + 1) * 128], hg[:, j * 128:(j + 1) * 128], ident_b, [b_hg, b_cb], [b_tpB])
                cp("dve", hgT[:].rearrange("p j t -> p (j t)"), tpB[:, 0:512], [b_tpB], [b_hgT])

            def thr_XF(i, slot):
                front(slot)
                yield
                ps_, b_ps = proj(C_Q, 512)
                act(qa[:], ps_[:], AF.Copy, [b_ps, b_stF], [b_qa], scale=rstd1)
                kv_proj(i % 2)
                yield thr_A(i)
                hgrn_proj()
                yield
                ps_, b_ps = proj(C_QH, 512)
                act(qh[:], ps_[:], AF.Silu, [b_ps, b_stF], [b_qh], scale=rstd1)
                ps_, b_ps = proj(C_GH, 512)
                act(gs[:], ps_[:], AF.Silu, [b_ps, b_stF], [b_gs], scale=rstd1)
                yield thr_H(i)
                if i > 0:
                    yield ("need", ("hgT", i - 1))
                for half in range(2):
                    ps_, b_ps = proj(C_ZA + half * 512, 512)
                    act(sza[:, half * 512:(half + 1) * 512], ps_[:], AF.Sigmoid, [b_ps, b_stF], [b_sza], scale=rstd1)
                    yield
                for half in range(2):
                    ps_, b_ps = proj(C_ZB + half * 512, 512)
                    act(szb[:, half * 512:(half + 1) * 512], ps_[:], AF.Sigmoid, [b_ps, b_stF], [b_szb], scale=rstd1)
                    yield

            def thr_Y(i, slot):
                for half in range(2):
                    hs = slice(half * 512, (half + 1) * 512)
                    for c in range(4):
                        mm(mmP[half][:], aT[:, c, :], wba[:, c, hs], c == 0, c == 3, [b_aT, b_wba], [b_mm[half]])
                    tt("dve", m1[:, hs], mmP[half][:], sza[:, hs], ALU.mult, [b_mm[half], b_sza], [b_m1])
                    yield
                yield ("set", ("aT", i))
                for half in range(2):
                    hs = slice(half * 512, (half + 1) * 512)
                    for c in range(4):
                        mm(mmP[half][:], hgT[:, c, :], wbh[:, c, hs], c == 0, c == 3, [b_hgT, b_wbh], [b_mm[half]])
                    tt("dve", junkY[:, hs], mmP[half][:], szb[:, hs], ALU.mult, [b_mm[half], b_szb], [b_junkY])
                    tt("pool", mixed[:, hs], junkY[:, hs], m1[:, hs], ALU.add, [b_junkY, b_m1], [b_mixed])
                    yield
                yield ("set", ("hgT", i))
                for c in range(8):
                    tr(tpB[:, c * 128:(c + 1) * 128], mixed[:, c * 128:(c + 1) * 128], ident_b, [b_mixed, b_cb], [b_tpB])
                cp("dve", mT[:].rearrange("p c t -> p (c t)"), tpB[:], [b_tpB], [b_mT])
                yield
                for half in range(2):
                    hs = slice(half * 512, (half + 1) * 512)
                    for c in range(8):
                        mm(mmP[half][:], mT[:, c, :], wout[:, c, hs], c == 0, c == 7, [b_mT, b_wout], [b_mm[half]])
                    tt("dve", xt[slot][:, hs], xt[slot][:, hs], mmP[half][:], ALU.add, [b_xt[slot], b_mm[half]], [b_xt[slot]])
                    yield
                dma("sp", out_d[i * 128:(i + 1) * 128, :], xt[slot][:], sem_x1, [b_xt[slot]], [b_out])
                mset("dve", stY[:, 0:1], 0.0, [b_stY])
                act(junkY[:], xt[slot][:], AF.Square, [b_xt[slot]], [b_junkY, b_stY], scale=1.0 / 32, accum_out=stY[:, 0:1])
                rstd_of(stY, b_stY, slice(1, 2), slice(0, 1))
                rstd2 = stY[:, 1:2]
                stt("dve", hb[:], xt[slot][:], rstd2, n2w, ALU.mult, ALU.mult, [b_xt[slot], b_stY, b_pp], [b_hb])
                yield
                x1T = m1[:].rearrange("p (c t) -> p c t", t=128)
                for g4 in range(2):
                    for c in range(4):
                        tr(mmP[g4][:, c * 128:(c + 1) * 128], xt[slot][:, (g4 * 4 + c) * 128:(g4 * 4 + c + 1) * 128], ident_f,
                           [b_xt[slot], b_cf], [b_mm[g4]])
                    cp("act" if g4 == 0 else "dve", m1[:, g4 * 512:(g4 + 1) * 512], mmP[g4][:], [b_mm[g4]], [b_m1])
                    yield
                for c in range(8):
                    mm(mmP[0][:, 0:36], x1T[:, c, :], wr[:, c, :], c == 0, c == 7, [b_m1, b_wr], [b_mm[0]])
                L = rt[:, 0:36]
                stt("dve", L, mmP[0][:, 0:36], rstd2, brt, ALU.mult, ALU.add, [b_mm[0], b_stY, b_pp], [b_rt])
                yield
                R = [b_rt]
                gmax = rt[:, 36:37]; ngmax = rt[:, 37:38]; gsum = rt[:, 38:39]; pg = rt[:, 39:40]
                red("dve", gmax, L[:, 0:4], ALU.max, R, R)
                ts("dve", ngmax, gmax, -1.0, None, ALU.mult, None, R, R)
                mset("dve", gsum, 0.0, R)
                act(rt[:, 40:44], L[:, 0:4], AF.Exp, R, R, bias=ngmax, accum_out=gsum)
                P.op("dve", lambda e, pg=pg, gsum=gsum: e.reciprocal(out=pg, in_=gsum), R, R)
                gm = rt[:, 44:48]
                ts("dve", gm, L[:, 0:4], gmax, None, ALU.is_ge, None, R, R)
                ts("dve", gm, gm, 1e30, -1e30, ALU.mult, ALU.add, R, R)
                yield
                lem = rt[:, 48:80]
                tt("dve", lem.rearrange("p (g j) -> p g j", j=8), L[:, 4:36].rearrange("p (g j) -> p g j", j=8),
                   bc(gm, [128, 4, 8], 2), ALU.add, R, R)
                top8 = rt[:, 80:88]
                P.op("dve", lambda e, top8=top8, lem=lem: e.max(out=top8, in_=lem), R, R)
                sel = rt[:, 88:120]; is1 = rt[:, 120:152]; isB = rt[:, 152:184]
                ts("dve", sel, lem, top8[:, 1:2], None, ALU.is_ge, None, R, R)
                ts("dve", is1, lem, top8[:, 0:1], None, ALU.is_ge, None, R, R)
                tt("dve", isB, sel, is1, ALU.subtract, R, R)
                yield
                dd = rt[:, 184:185]; s1_ = rt[:, 185:186]
                tt("dve", dd, top8[:, 0:1], top8[:, 1:2], ALU.subtract, R, R)
                act(s1_, dd, AF.Sigmoid, R, R)
                tt("dve", gate[:, 0, i:i + 1], pg, s1_, ALU.mult, R, [b_gate])
                tt("dve", gate[:, 1, i:i + 1], pg, gate[:, 0, i:i + 1], ALU.subtract, R + [b_gate], [b_gate])
                cp("dve", selb[:], sel, R, [b_selb])
                yield
                mm(mmP[1][:, 0:32], stri_b, selb[:], True, False, [b_cb, b_selb], [b_mm[1]])
                mm(mmP[1][:, 0:32], ones_b, cumsb[:], False, True, [b_cb, b_cumsb], [b_mm[1]])
                tt("dve", cums[:], cums[:], sel, ALU.add, R + [b_cums], [b_cums])
                cp("dve", cumsb[:], cums[:], [b_cums], [b_cumsb])
                posc = rt[:, 192:224]
                ts("dve", posc, mmP[1][:, 0:32], float(CAP - 1), None, ALU.min, None, [b_mm[1]], R)
                yield
                tt("dve", posc, posc, iotae, ALU.add, R + [b_cf], R)
                tmpm = rt[:, 224:256]
                dA = rt[:, 186:187]; dB = rt[:, 187:188]
                tt("dve", tmpm, posc, is1, ALU.mult, R, R)
                red("dve", dA, tmpm, ALU.add, R, R)
                tt("dve", tmpm, posc, isB, ALU.mult, R, R)
                red("dve", dB, tmpm, ALU.add, R, R)
                cp("dve", dest[:, 0, i:i + 1], dA, R, [b_dest])
                cp("dve", dest[:, 1, i:i + 1], dB, R, [b_dest])
                yield
                for k in range(2):
                    off = dest[:, k, i:i + 1]
                    P.dma("pool", lambda e, off=off: e.indirect_dma_start(
                        out=xbuf_d, out_offset=bass.IndirectOffsetOnAxis(ap=off, axis=0), in_=hb[:, :], in_offset=None),
                        sem_sc, [b_hb, b_dest, b_xbuf], [Buf("scat")])

            sem_cv = P.newsem()
            cv_next = [0]

            def convert_experts(upto):
                while cv_next[0] < min(upto, NEX):
                    e = cv_next[0]
                    cv_next[0] += 1
                    for src, dst in ((wg_d, wgb_d), (wu_d, wub_d), (wd_d, wdb_d)):
                        s2_ = src[e].rearrange("a b -> (a b)").rearrange("(p f) -> p f", p=128)
                        d2_ = dst[e].rearrange("a b -> (a b)").rearrange("(p f) -> p f", p=128)
                        dma("pool", d2_, s2_, sem_cv, [], [Buf("cv")])

            for i in range(NT):
                gi = NPRE + i
                if i + 1 < NT:
                    load_x(x_own, i + 1, (gi + 1) % 3)
                convert_experts(((i + 1) * 32 + NT - 1) // NT)
                thr = []
                if i > 0:
                    thr.append(thr_Y(i - 1, (gi - 1) % 3))
                thr.append(thr_XF(i, gi % 3))
                run_threads(thr)
            run_threads([thr_Y(NT - 1, (NPRE + NT - 1) % 3)])
            P.barrier()
            P.emit_block()
        if mode in (1, 2, 3, 4):
            return nc

        with ExitStack() as s2:
            NWB = 2
            wg = [SB(s2, "wg%d" % k, [128, 8, 512], BF16) for k in range(NWB)]
            wu = [SB(s2, "wu%d" % k, [128, 8, 512], BF16) for k in range(NWB)]
            wd = [SB(s2, "wd%d" % k, [128, 4, D], BF16) for k in range(NWB)]
            b_w = [Buf("w%d" % k) for k in range(NWB)]
            sem_e = [P.newsem() for _ in range(NWB)]
            xg = [SB(s2, "xg%d" % k, [128, NB, D], BF16) for k in range(2)]; b_xg = [Buf("xg0"), Buf("xg1")]
            sem_xg = [P.newsem(), P.newsem()]
            xgT = SB(s2, "xgT", [128, 8, CAP], BF16); b_xgT = Buf("xgT")
            sg = [SB(s2, "sg%d" % k, [128, CAP], F32) for k in range(2)]; b_sg = [Buf("sg0"), Buf("sg1")]
            hTs = [SB(s2, "hT%d" % k, [128, 4, CAP], BF16) for k in range(2)]; b_hTs = [Buf("hT0"), Buf("hT1")]
            NYB = 4
            ysb = [SB(s2, "ysb%d" % k, [128, D], F32) for k in range(NYB)]; b_ysb = [Buf("ysb%d" % k) for k in range(NYB)]
            sem_y = [P.newsem() for _ in range(NYB)]
            NP3 = 6
            x1t = [SB(s2, "x1t%d" % k, [128, D], F32) for k in range(NP3)]; b_x1t = [Buf("x1t%d" % k) for k in range(NP3)]
            yA = [SB(s2, "yA%d" % k, [128, D], F32) for k in range(NP3)]; b_yA = [Buf("yA%d" % k) for k in range(NP3)]
            yB = [SB(s2, "yB%d" % k, [128, D], F32) for k in range(NP3)]; b_yB = [Buf("yB%d" % k) for k in range(NP3)]
            sem_l = [P.newsem() for _ in range(NP3)]
            sem_ga = [P.newsem() for _ in range(NP3)]
            sem_gb = [P.newsem() for _ in range(NP3)]
            sem_o = [P.newsem() for _ in range(NP3)]
            tp2s = [PS(s2, "tp2%d" % k, [128, 1024], BF16) for k in range(2)]; b_tp2s = [Buf("tp20", True), Buf("tp21", True)]
            tcount = [0]
            pg_ = [PS(s2, "pg%d" % k, [128, 512], F32) for k in range(2)]; b_pg = [Buf("pg0", True), Buf("pg1", True)]
            pu_ = [PS(s2, "pu%d" % k, [128, 512], F32) for k in range(2)]; b_pu = [Buf("pu0", True), Buf("pu1", True)]
            py_ = [PS(s2, "py%d" % k, [128, 512], F32) for k in range(2)]; b_py = [Buf("py0", True), Buf("py1", True)]

            def load_w(e):
                k = e % NWB
                hws = []
                dma("pool", wg[k][:], wgb_d[e].rearrange("(c p) n -> p c n", p=128), sem_e[k], [], [b_w[k]])
                P.dma("pool", lambda en, k=k, e=e: en.dma_start(out=wu[k][:], in_=wub_d[e].rearrange("(c p) n -> p c n", p=128)), sem_e[k], [], [Buf("t")])
                h = P.dma("pool", lambda en, k=k, e=e: en.dma_start(out=wd[k][:], in_=wdb_d[e].rearrange("(c p) n -> p c n", p=128)), sem_e[k], [], [Buf("t")])
                b_w[k].last_w = h

            def load_xg(e):
                k = e % 2
                dma("sp", xg[k][:], xbuf_d[e * CAP:(e + 1) * CAP, :].rearrange("(b p) d -> p b d", p=128), sem_xg[k], [b_xbuf], [b_xg[k]])

            load_w(0)
            load_xg(0)
            ycount = 0
            for e in range(32):
                k = e % NWB
                kx = e % 2
                hT = hTs[e % 2]; b_hT = b_hTs[e % 2]
                if e + 1 < 32:
                    load_w(e + 1)
                    load_xg(e + 1)
                for sb_ in range(NB):
                    tp2 = tp2s[tcount[0] % 2]; b_tp2 = b_tp2s[tcount[0] % 2]
                    tcount[0] += 1
                    for c in range(8):
                        tr(tp2[:, c * 128:(c + 1) * 128], xg[kx][:, sb_, c * 128:(c + 1) * 128], ident_b, [b_xg[kx], b_cb], [b_tp2])
                    cp("dve" if sb_ % 2 == 0 else "act", xgT[:, :, sb_ * 128:(sb_ + 1) * 128], tp2[:].rearrange("p (c t) -> p c t", t=128), [b_tp2], [b_xgT])
                for fc in range(4):
                    a_ = fc % 2
                    for c in range(8):
                        mm(pg_[a_][:, 0:CAP], wg[k][:, c, fc * 128:(fc + 1) * 128], xgT[:, c, :], c == 0, c == 7, [b_w[k], b_xgT], [b_pg[a_]])
                    for c in range(8):
                        mm(pu_[a_][:, 0:CAP], wu[k][:, c, fc * 128:(fc + 1) * 128], xgT[:, c, :], c == 0, c == 7, [b_w[k], b_xgT], [b_pu[a_]])
                    act(sg[a_][:], pg_[a_][:, 0:CAP], AF.Silu, [b_pg[a_]], [b_sg[a_]])
                    tt("dve", hT[:, fc, :], sg[a_][:], pu_[a_][:, 0:CAP], ALU.mult, [b_sg[a_], b_pu[a_]], [b_hT])
                for sb_ in range(NB):
                    ky = ycount % NYB
                    ycount += 1
                    for nh in range(2):
                        for fc in range(4):
                            mm(py_[nh][:], hT[:, fc, sb_ * 128:(sb_ + 1) * 128], wd[k][:, fc, nh * 512:(nh + 1) * 512], fc == 0, fc == 3,
                               [b_hT, b_w[k]], [b_py[nh]])
                        cp("act" if nh == 0 else "dve", ysb[ky][:, nh * 512:(nh + 1) * 512], py_[nh][:], [b_py[nh]], [b_ysb[ky]])
                    r0 = e * CAP + sb_ * 128
                    dma("sp", ybuf_d[r0:r0 + 128, :], ysb[ky][:], sem_y[ky], [b_ysb[ky]], [Buf("yst%d" % ky)])
            P.barrier()

            def p3_fetch(i):
                k = i % NP3
                dma("sp", x1t[k][:], out_d[i * 128:(i + 1) * 128, :], sem_l[k], [b_out], [b_x1t[k]])
                offA = dest[:, 0, i:i + 1]
                offB = dest[:, 1, i:i + 1]
                P.dma("pool", lambda en, off=offA, dst=yA[k]: en.indirect_dma_start(
                    out=dst[:, :], out_offset=None, in_=ybuf_d, in_offset=bass.IndirectOffsetOnAxis(ap=off, axis=0)),
                    sem_ga[k], [b_dest], [b_yA[k]])
                P.dma("pool", lambda en, off=offB, dst=yB[k]: en.indirect_dma_start(
                    out=dst[:, :], out_offset=None, in_=ybuf_d, in_offset=bass.IndirectOffsetOnAxis(ap=off, axis=0)),
                    sem_gb[k], [b_dest], [b_yB[k]])

            for i in range(min(NP3 - 1, NT)):
                p3_fetch(i)
            for i in range(NT):
                k = i % NP3
                if i + NP3 - 1 < NT:
                    p3_fetch(i + NP3 - 1)
                stt("dve", x1t[k][:], yA[k][:], gate[:, 0, i:i + 1], x1t[k][:], ALU.mult, ALU.add, [b_yA[k], b_gate, b_x1t[k]], [b_x1t[k]])
                stt("dve", x1t[k][:], yB[k][:], gate[:, 1, i:i + 1], x1t[k][:], ALU.mult, ALU.add, [b_yB[k], b_gate, b_x1t[k]], [b_x1t[k]])
                dma("act", out_d[i * 128:(i + 1) * 128, :], x1t[k][:], sem_o[k], [b_x1t[k]], [Buf("ost%d" % k)])
            P.barrier()
            P.emit_block()
    return nc


def _consts(CAP):
    cf = np.zeros((128, K_END), np.float32)
    cf[:, K_ID:K_ID + 128] = np.eye(128, dtype=np.float32)
    s = np.arange(128)[:, None]
    t = np.arange(128)[None, :]
    cf[:, K_TRI:K_TRI + 128] = ((s <= t) & ((s // 64) == (t // 64))).astype(np.float32)
    cf[63, K_SEL2] = 1.0
    cf[127, K_SEL2 + 1] = 1.0
    invf = (500000.0 ** (-np.arange(0, 16, 2, dtype=np.float32) / 16)).astype(np.float32)
    cf[:, K_INVF:K_INVF + 8] = invf[None, :]
    cf[:, K_IOTA:K_IOTA + 32] = (np.arange(32, dtype=np.float32) * CAP)[None, :]
    cb = np.zeros((128, B_END), np.float32)
    cb[:, B_ID:B_ID + 128] = np.eye(128, dtype=np.float32)
    cb[:, B_STRI:B_STRI + 128] = (s < t).astype(np.float32)
    cb[:, B_ONES:B_ONES + 128] = 1.0
    mcur = np.where(s <= t, 0.0, NEG).astype(np.float32)
    mprev = np.where(s > t, 0.0, NEG).astype(np.float32)
    cb[:, B_MCUR:B_MCUR + 512] = np.tile(mcur, (1, 4))
    cb[:, B_MPREV:B_MPREV + 512] = np.tile(mprev, (1, 4))
    return cf, cb, mprev


def make_in_maps(inp, NT, CAP, n_cores=8):
    x = np.asarray(inp["x"], np.float32)
    B, S, _ = x.shape
    T = NT * 128
    assert S == 2 * T and B * 2 == n_cores
    pos = np.asarray(inp["positions"]).astype(np.int32)
    cf, cb0, mprev = _consts(CAP)
    w_in = np.asarray(inp["w_in"], np.float32)[0]
    qperm = np.concatenate([np.arange(64) + (g * 4 + j) * 64 for j in range(4) for g in range(2)])
    w_in_p = np.ascontiguousarray(np.concatenate([w_in[:, :512][:, qperm], w_in[:, 512:]], axis=1))
    f = lambda k: np.ascontiguousarray(np.asarray(inp[k], np.float32)[0])
    pp = np.zeros((128, Q_END), np.float32)
    pp[:, Q_N1C:Q_N1C + 8] = f("norm1_w").reshape(8, 128).T
    pp[:, Q_N2C:Q_N2C + 8] = f("norm2_w").reshape(8, 128).T
    pp[:, Q_N2:Q_N2 + 1024] = f("norm2_w")[None, :]
    pp[:, Q_QW:Q_QW + 512] = np.tile(f("q_norm_w"), 8)[None, :]
    pp[:, Q_KW:Q_KW + 128] = np.tile(f("k_norm_w"), 2)[None, :]
    pp[:, Q_HW:Q_HW + 512] = np.tile(f("hgrn_norm_w"), 4)[None, :]
    hlb = np.asarray(inp["hgrn_lower_bounds"], np.float32)
    hlbt = np.ascontiguousarray(np.tile(hlb.reshape(1, 1024), (128, 1)))
    pp[:, Q_SINK:Q_SINK + 8] = f("attn_sinks")[None, :]
    pp[:, Q_BRT:Q_BRT + 4] = f("b_router_group")[None, :]
    pp[:, Q_BRT + 4:Q_BRT + 36] = f("b_router_expert")[None, :]
    w_r = np.ascontiguousarray(np.concatenate([f("w_router_group"), f("w_router_expert")], axis=1))
    shared = {
        "cf32": cf, "pp32": pp, "hlb": hlbt, "w_in": w_in_p, "w_ba": f("w_branch_attn"), "w_bh": f("w_branch_hgrn"),
        "w_out": f("w_out"), "w_r": w_r, "w_g": f("w_gate_experts"), "w_u": f("w_up_experts"), "w_d": f("w_down_experts"),
    }
    maps = []
    for c in range(n_cores):
        b, half = c // 2, c % 2
        cb = cb0.copy()
        if half == 1:
            cb[:, B_MHALO:B_MHALO + 512] = np.tile(mprev, (1, 4))
            x_pre = np.ascontiguousarray(x[b, 0:T])
            halo_pos = pos[b, T - 128:T]
        else:
            cb[:, B_MHALO:B_MHALO + 512] = NEG
            x_pre = np.zeros((T, D), np.float32)
            halo_pos = np.zeros(128, np.int32)
        own = slice(half * T, (half + 1) * T)
        pt = np.concatenate([pos[b, own].reshape(NT, 128).T, halo_pos[:, None]], axis=1).astype(np.int32)
        m = dict(shared)
        m.update({"x_own": np.ascontiguousarray(x[b, own]), "x_pre": x_pre, "pos": np.ascontiguousarray(pt), "cb16": cb})
        maps.append(m)
    return maps


_NC_CACHE = {}


def run(inp, NT, CAP, mode=0, raw=False):
    key = (NT, CAP, mode)
    if key not in _NC_CACHE:
        _NC_CACHE[key] = build(NT, CAP, mode=mode)
    nc = _NC_CACHE[key]
    maps = make_in_maps(inp, NT, CAP)
    if mode != 0:
        for m in maps:
            for k in ("w_g", "w_u", "w_d"):
                m[k] = np.ascontiguousarray(m[k][:1])
    res = run_bass_kernel_spmd(nc, maps, core_ids=list(range(8)))
    if raw:
        return res.results
    x = np.asarray(inp["x"])
    B, S, _ = x.shape
    T = NT * 128
    out = np.empty((B, S, D), np.float32)
    for c in range(8):
        b, half = c // 2, c % 2
        out[b, half * T:(half + 1) * T] = res.results[c]["out"]
    return out


def kernel(**inputs):
    return run(inputs, 32, 384)
```

```python
import math
import os
from contextlib import ExitStack
import numpy as np
import concourse.bass as bass
import concourse.mybir as mybir
from concourse.bass_utils import run_bass_kernel_spmd

F32 = mybir.dt.float32
BF16 = mybir.dt.bfloat16
I32 = mybir.dt.int32
ALU = mybir.AluOpType
AF = mybir.ActivationFunctionType
AX = mybir.AxisListType

D = 1024
NEG = -30000.0
EPS = 1e-6
TWO_PI = 2.0 * math.pi


class Buf:
    __slots__ = ("name", "last_w", "readers", "excl")

    def __init__(self, name, excl=False):
        self.name = name
        self.last_w = None
        self.readers = []
        self.excl = excl


class DmaSem:
    def __init__(self, key):
        self.key = key
        self.count = 0


class Prog:
    ENGS = ("pe", "act", "dve", "pool", "sp")
    EMAP = {"pe": "tensor", "act": "scalar", "dve": "vector", "pool": "gpsimd", "sp": "sync"}

    def __init__(self, nc, stack, n_dma_sems=64):
        self.nc = nc
        self.ops = {e: [] for e in self.ENGS}
        self.n = {e: 0 for e in self.ENGS}
        self.waited = {e: {} for e in self.ENGS}
        self.dma_sems = [DmaSem("d%d" % i) for i in range(n_dma_sems)]
        self.sems = {}
        for e in self.ENGS:
            self.sems[e] = stack.enter_context(nc.semaphore("s_" + e))
        for d in self.dma_sems:
            self.sems[d.key] = stack.enter_context(nc.semaphore("s_" + d.key))
        self._next = 0
        self.tfree = {e: 0.0 for e in self.ENGS}
        self.hfin = {}
        self.step_max = 0.0
        self.act_grp = None

    def _time(self, eng, deps, h, cost):
        t = self.tfree[eng]
        for d in deps:
            f = self.hfin.get(d)
            if f is not None:
                f = f + (0.05 if d[0] == eng else 0.2)
                if f > t:
                    t = f
        fin = t + cost
        if h[0] in self.ENGS:
            self.tfree[eng] = fin
        else:
            self.tfree[eng] = t + 0.1
        self.hfin[h] = fin
        if fin > self.step_max:
            self.step_max = fin

    def newsem(self):
        s = self.dma_sems[self._next]
        self._next += 1
        return s

    def _deps(self, eng, reads, writes):
        deps = set()
        for b in reads:
            if b.last_w is not None:
                deps.add(b.last_w)
        for b in writes:
            if b.last_w is not None:
                deps.add(b.last_w)
            deps.update(b.readers)
        w = self.waited[eng]
        best = {}
        for (sk, v) in deps:
            if eng == "pe" and sk == "pe":
                continue
            if w.get(sk, 0) < v and best.get(sk, 0) < v:
                best[sk] = v
        for sk, v in best.items():
            self.ops[eng].append(("wait", sk, v))
            w[sk] = v
        return deps

    def _mark(self, h, reads, writes):
        for b in reads:
            b.readers.append(h)
            if len(b.readers) > 64:
                b.readers = b.readers[-48:]
        for b in writes:
            b.last_w = h
            b.readers = []

    def op(self, eng, fn, reads=(), writes=(), cost=0.3):
        ex = [b for b in reads if b.excl and b not in writes]
        if ex:
            writes = list(writes) + ex
        deps = self._deps(eng, reads, writes)
        self.n[eng] += 1
        h = (eng, self.n[eng])
        self.ops[eng].append(("op", fn))
        self._time(eng, deps, h, cost)
        self._mark(h, reads, writes)
        return h

    def dma(self, eng, fn, sem, reads=(), writes=(), cost=3.0):
        deps = self._deps(eng, reads, writes)
        sem.count += 16
        h = (sem.key, sem.count)
        self.ops[eng].append(("dma", fn, sem.key))
        self._time(eng, deps, h, cost)
        self._mark(h, reads, writes)
        return h

    def wait_all(self, eng, bufs):
        self._deps(eng, bufs, ())

    def barrier(self):
        for e in self.ENGS:
            w = self.waited[e]
            for f in self.ENGS:
                if f != e and self.n[f] > w.get(f, 0):
                    self.ops[e].append(("wait", f, self.n[f]))
                    w[f] = self.n[f]
            for d in self.dma_sems:
                if d.count > w.get(d.key, 0):
                    self.ops[e].append(("wait", d.key, d.count))
                    w[d.key] = d.count

    def emit_block(self):
        sems = self.sems
        with self.nc.Block() as block:
            def mk(e):
                items = self.ops[e]

                def body(engobj):
                    for item in items:
                        if item[0] == "wait":
                            engobj.wait_ge(sems[item[1]], item[2])
                        elif item[0] == "op":
                            item[1](engobj).then_inc(sems[e], 1)
                        else:
                            item[1](engobj).then_inc(sems[item[2]], 16)
                return body
            for e in self.ENGS:
                if self.ops[e]:
                    getattr(block, self.EMAP[e])(mk(e))
        self.ops = {e: [] for e in self.ENGS}


C_Q, C_KV, C_QH, C_FH, C_IH, C_GH, C_ZA, C_ZB = 0, 512, 768, 1280, 1792, 2304, 2816, 3840
NIN = 4864
K_ID, K_TRI, K_SEL2, K_INVF, K_IOTA, K_END = 0, 128, 256, 258, 266, 298
B_ID, B_STRI, B_ONES, B_MCUR, B_MPREV, B_MHALO, B_END = 0, 128, 256, 384, 896, 1408, 1920
Q_N2, Q_QW, Q_KW, Q_HW, Q_SINK, Q_BRT, Q_N1C, Q_N2C, Q_END = 0, 1024, 1536, 1664, 2176, 2184, 2220, 2228, 2236


def build(NT=32, CAP=384, NPRE=None, mode=0):
    if NPRE is None:
        NPRE = NT
    T = NT * 128
    NSLOT = 32 * CAP
    NB = CAP // 128
    nc = bass.Bass("TRN2", target_bir_lowering=False)

    def din(name, shape, dt=F32):
        return nc.dram_tensor(name, shape, dt, kind="ExternalInput").ap()

    x_own = din("x_own", [T, D])
    x_pre = din("x_pre", [NPRE * 128, D])
    pos_d = din("pos", [128, NT + 1], I32)
    cf_d = din("cf32", [128, K_END])
    cb_d = din("cb16", [128, B_END])
    pp_d = din("pp32", [128, Q_END])
    hlb_d = din("hlb", [128, 1024])
    win_d = din("w_in", [D, NIN])
    wba_d = din("w_ba", [512, D])
    wbh_d = din("w_bh", [512, D])
    wout_d = din("w_out", [D, D])
    wr_d = din("w_r", [D, 36])
    NEX = 32 if mode == 0 else 1
    wg_d = din("w_g", [NEX, D, 512])
    wu_d = din("w_u", [NEX, D, 512])
    wd_d = din("w_d", [NEX, 512, D])
    out_d = nc.dram_tensor("out", [T, D], F32, kind="ExternalOutput").ap()
    xbuf_d = nc.dram_tensor("xbuf", [NSLOT, D], BF16, kind="Internal").ap()
    ybuf_d = nc.dram_tensor("ybuf", [NSLOT, D], F32, kind="Internal").ap()
    wgb_d = nc.dram_tensor("wgb", [NEX, D, 512], BF16, kind="Internal").ap()
    wub_d = nc.dram_tensor("wub", [NEX, D, 512], BF16, kind="Internal").ap()
    wdb_d = nc.dram_tensor("wdb", [NEX, 512, D], BF16, kind="Internal").ap()

    with ExitStack() as top:
        P = Prog(nc, top)

        def SB(st, name, shape, dt):
            return st.enter_context(nc.sbuf_tensor(name, shape, dt))

        def PS(st, name, shape, dt):
            return st.enter_context(nc.psum_tensor(name, shape, dt))

        def nfree(ap):
            n = 1
            for d in ap.shape[1:]:
                n *= d
            return n

        def ecost(eng, ap):
            n = nfree(ap)
            if eng == "pool":
                return 0.12 + n * 0.0022
            if eng == "act":
                return 0.2 + n * 0.00095
            return 0.07 + n * 0.00123

        AGRP = {AF.Exp: 1, AF.Ln: 1, AF.Sigmoid: 2, AF.Silu: 3, AF.Sin: 4}

        def mm(out, lhsT, rhs, start, stop, r, w):
            c = max(0.064, 0.00042 * nfree(rhs))
            if lhsT.dtype == F32:
                c *= 4
            return P.op("pe", lambda e: e.matmul(out=out, lhsT=lhsT, rhs=rhs, start=start, stop=stop), r, w, cost=c)

        def tr(out, in_, ident, r, w):
            c = 0.11 * (4 if in_.dtype == F32 else 1)
            return P.op("pe", lambda e: e.transpose(out=out, in_=in_, identity=ident), r, w, cost=c)

        def act(out, in_, func, r, w, **kw):
            c = ecost("act", in_)
            g = AGRP.get(func)
            if g is not None and g != P.act_grp:
                c += 1.3
                P.act_grp = g
            return P.op("act", lambda e: e.activation(out=out, in_=in_, func=func, **kw), r, w, cost=c)

        def tt(eng, out, in0, in1, op, r, w):
            return P.op(eng, lambda e: e.tensor_tensor(out=out, in0=in0, in1=in1, op=op), r, w, cost=ecost(eng, out))

        def ts(eng, out, in0, s1, s2, op0, op1, r, w):
            if op1 is None:
                return P.op(eng, lambda e: e.tensor_scalar(out=out, in0=in0, scalar1=s1, scalar2=None, op0=op0), r, w, cost=ecost(eng, out))
            return P.op(eng, lambda e: e.tensor_scalar(out=out, in0=in0, scalar1=s1, scalar2=s2, op0=op0, op1=op1), r, w, cost=ecost(eng, out))

        def stt(eng, out, in0, scalar, in1, op0, op1, r, w):
            return P.op(eng, lambda e: e.scalar_tensor_tensor(out=out, in0=in0, scalar=scalar, in1=in1, op0=op0, op1=op1), r, w, cost=ecost(eng, out))

        def cp(eng, out, in_, r, w):
            if eng == "act":
                return P.op("act", lambda e: e.copy(out=out, in_=in_), r, w, cost=ecost(eng, out))
            return P.op(eng, lambda e: e.tensor_copy(out=out, in_=in_), r, w, cost=ecost(eng, out))

        def red(eng, out, in_, op, r, w):
            return P.op(eng, lambda e: e.tensor_reduce(out=out, in_=in_, axis=AX.X, op=op), r, w, cost=ecost(eng, in_))

        def mset(eng, ap, val, w):
            return P.op(eng, lambda e: e.memset(ap, val), (), w, cost=ecost(eng, ap))

        def dma(eng, out, in_, sem, r, w):
            return P.dma(eng, lambda e: e.dma_start(out=out, in_=in_), sem, r, w)

        def bc(ap, shape, axis):
            return ap.unsqueeze(axis).to_broadcast(shape)

        idb = SB(top, "idb", [128, B_END], BF16); b_cb = Buf("cb")
        cf = SB(top, "cf", [128, K_END], F32); b_cf = Buf("cf")
        dest = SB(top, "dest", [128, 2, NT], I32); b_dest = Buf("dest")
        gate = SB(top, "gate", [128, 2, NT], F32); b_gate = Buf("gate")
        b_xbuf = Buf("xbuf"); b_ybuf = Buf("ybuf"); b_out = Buf("out")
        ident_f = cf[:, K_ID:K_ID + 128]
        tri_f = cf[:, K_TRI:K_TRI + 128]
        sel2 = cf[:, K_SEL2:K_SEL2 + 2]
        invf = cf[:, K_INVF:K_INVF + 8]
        iotae = cf[:, K_IOTA:K_IOTA + 32]
        ident_b = idb[:, B_ID:B_ID + 128]
        stri_b = idb[:, B_STRI:B_STRI + 128]
        ones_b = idb[:, B_ONES:B_ONES + 128]
        mask_cur = idb[:, B_MCUR:B_MCUR + 512]
        mask_prev = idb[:, B_MPREV:B_MPREV + 512]
        mask_halo = idb[:, B_MHALO:B_MHALO + 512]

        sem_c = [P.newsem() for _ in range(4)]
        dma("sp", cf[:], cf_d, sem_c[0], [], [b_cf])
        dma("pool", idb[:], cb_d, sem_c[1], [], [b_cb])

        with ExitStack() as s1:
            Wp = SB(s1, "Wp", [128, 8, NIN], BF16); b_Wp = Buf("Wp")
            wba = SB(s1, "wba", [128, 4, D], BF16); b_wba = Buf("wba")
            wbh = SB(s1, "wbh", [128, 4, D], BF16); b_wbh = Buf("wbh")
            wout = SB(s1, "wout", [128, 8, D], BF16); b_wout = Buf("wout")
            wr = SB(s1, "wr", [128, 8, 36], F32); b_wr = Buf("wr")
            pp = SB(s1, "pp", [128, Q_END], F32); b_pp = Buf("pp")
            lbt = SB(s1, "lbt", [128, 2, 512], F32); b_lb = Buf("lb")
            esink = SB(s1, "esink", [128, 8], F32); b_esink = Buf("esink")
            posi = SB(s1, "posi", [128, NT + 1], I32); b_posi = Buf("posi")
            NA = (NT + 1) * 8
            trig = SB(s1, "trig", [128, 2, NA], F32); b_trig = Buf("trig")
            cst = SB(s1, "cst", [128, 4], F32); b_cst = Buf("cst")
            cos_t = trig[:, 0, :].rearrange("p (i j) -> p i j", j=8)
            sin_t = trig[:, 1, :].rearrange("p (i j) -> p i j", j=8)

            n2w = pp[:, Q_N2:Q_N2 + 1024]
            qw = pp[:, Q_QW:Q_QW + 512]
            kw = pp[:, Q_KW:Q_KW + 128]
            hw = pp[:, Q_HW:Q_HW + 512]
            brt = pp[:, Q_BRT:Q_BRT + 36]

            dma("sp", pp[:], pp_d, sem_c[2], [], [b_pp])
            dma("sp", posi[:], pos_d, sem_c[3], [], [b_posi])
            WG = [(C_FH, C_GH), (C_KV, C_QH), (C_Q, C_KV), (C_QH, C_FH), (C_GH, C_ZA), (C_ZA, NIN)]
            b_Wg = [Buf("Wg%d" % g) for g in range(len(WG))]
            win3 = win_d.rearrange("(c p) n -> p c n", p=128)
            for g, (c0, c1) in enumerate(WG):
                dma("pool", Wp[:, :, c0:c1], win3[:, :, c0:c1], P.newsem(), [], [b_Wg[g]])

            def wbuf(c0):
                for g, (a0, a1) in enumerate(WG):
                    if a0 <= c0 < a1:
                        return b_Wg[g]
                raise ValueError(c0)
            sem_w = P.newsem()
            dma("pool", wba[:], wba_d.rearrange("(c p) n -> p c n", p=128), sem_w, [], [Buf("wtmp")])
            dma("pool", wbh[:], wbh_d.rearrange("(c p) n -> p c n", p=128), sem_w, [], [Buf("wtmp")])
            hW = dma("pool", wout[:], wout_d.rearrange("(c p) n -> p c n", p=128), sem_w, [], [Buf("wtmp")])
            for b in (b_wba, b_wbh, b_wout):
                b.last_w = hW
            sem_wr = P.newsem()
            dma("sp", wr[:], wr_d.rearrange("(c p) n -> p c n", p=128), sem_wr, [], [b_wr])

            xt = [SB(s1, "xt%d" % k, [128, D], F32) for k in range(3)]; b_xt = [Buf("xt%d" % k) for k in range(3)]
            sem_x = [P.newsem() for _ in range(3)]
            junkF = SB(s1, "junkF", [128, D], BF16); b_junkF = Buf("junkF")
            junkY = SB(s1, "junkY", [128, D], BF16); b_junkY = Buf("junkY")
            stF = SB(s1, "stF", [128, 8], F32); b_stF = Buf("stF")
            stA = SB(s1, "stA", [128, 64], F32); b_stA = Buf("stA")
            stH = SB(s1, "stH", [128, 16], F32); b_stH = Buf("stH")
            stY = SB(s1, "stY", [128, 8], F32); b_stY = Buf("stY")
            xb = SB(s1, "xb", [128, D], BF16); b_xb = Buf("xb")
            xT = SB(s1, "xT", [128, 8, 128], BF16); b_xT = Buf("xT")
            qa = SB(s1, "qa", [128, 512], F32); b_qa = Buf("qa")
            kv = SB(s1, "kvf", [128, 128], F32); b_kv = Buf("kv")
            r16 = SB(s1, "r16", [128, 6, 8, 8], F32); b_r16 = Buf("r16")
            qr = SB(s1, "qr", [128, 512], BF16); b_qr = Buf("qr")
            kr = SB(s1, "kr", [128, 128], BF16); b_kr = Buf("kr")
            qT = SB(s1, "qT", [128, 4, 128], BF16); b_qT = Buf("qT")
            kTs = [SB(s1, "kT%d" % k, [128, 128], BF16) for k in range(2)]; b_kT = [Buf("kT0"), Buf("kT1")]
            vx = [SB(s1, "vx%d" % k, [128, 2, 80], BF16) for k in range(2)]; b_vx = [Buf("vx0"), Buf("vx1")]
            pTb = [SB(s1, "pT%d" % k, [128, 512], BF16) for k in range(2)]; b_pTb = [Buf("pT0"), Buf("pT1")]
            attn = SB(s1, "attn", [128, 512], BF16); b_attn = Buf("attn")
            aT = SB(s1, "aT", [128, 4, 128], BF16); b_aT = Buf("aT")
            qh = SB(s1, "qh", [128, 512], F32); b_qh = Buf("qh")
            hA = SB(s1, "hA", [128, 512], F32); b_hA = Buf("hA")
            hk = SB(s1, "hk", [128, 512], F32); b_hk = Buf("hk")
            eb = SB(s1, "eb", [128, 512], F32); b_eb = Buf("eb")
            iv = SB(s1, "iv", [128, 512], BF16); b_iv = Buf("iv")
            gs = SB(s1, "gs", [128, 512], F32); b_gs = Buf("gs")
            qt = SB(s1, "qt", [128, 512], BF16); b_qt = Buf("qt")
            kt = SB(s1, "kt", [128, 512], BF16); b_kt = Buf("kt")
            qlh = SB(s1, "qlh", [128, 4, 2, 128], BF16); b_qlh = Buf("qlh")
            ktT = SB(s1, "ktT", [128, 4, 128], BF16); b_ktT = Buf("ktT")
            Am = [SB(s1, "Am%d" % k, [128, 128], BF16) for k in range(2)]; b_Am = [Buf("Am0"), Buf("Am1")]
            S = SB(s1, "S", [128, 4, 128], F32); b_S = Buf("S")
            Sb = [SB(s1, "Sb%d" % k, [128, 4, 128], BF16) for k in range(2)]; b_Sb = [Buf("Sb0"), Buf("Sb1")]
            Dsb = SB(s1, "Dsb", [128, 4, 2], F32); b_Dsb = Buf("Dsb")
            hg = SB(s1, "hg", [128, 512], BF16); b_hg = Buf("hg")
            hgT = SB(s1, "hgT", [128, 4, 128], BF16); b_hgT = Buf("hgT")
            sza = SB(s1, "sza", [128, D], BF16); b_sza = Buf("sza")
            szb = SB(s1, "szb", [128, D], BF16); b_szb = Buf("szb")
            m1 = SB(s1, "m1", [128, D], F32); b_m1 = Buf("m1")
            mixed = SB(s1, "mixed", [128, D], BF16); b_mixed = Buf("mixed")
            mT = SB(s1, "mT", [128, 8, 128], BF16); b_mT = Buf("mT")
            hb = SB(s1, "hb", [128, D], BF16); b_hb = Buf("hb")
            rt = SB(s1, "rt", [128, 256], F32); b_rt = Buf("rt")
            cums = SB(s1, "cums", [128, 32], F32); b_cums = Buf("cums")
            cumsb = SB(s1, "cumsb", [128, 32], BF16); b_cumsb = Buf("cumsb")
            selb = SB(s1, "selb", [128, 32], BF16); b_selb = Buf("selb")
            sem_x1 = P.newsem()
            sem_sc = P.newsem()

            tp = PS(s1, "tp", [128, 1024], BF16); b_tp = Buf("tp", True)
            mmA = PS(s1, "mmA", [128, 512], F32); mmB = PS(s1, "mmB", [128, 512], F32)
            b_mm = [Buf("mmA", True), Buf("mmB", True)]; mmP = [mmA, mmB]
            sc = [PS(s1, "sc0", [128, 512], F32)]; b_sc = [Buf("sc0", True)]
            tpB = PS(s1, "tpB", [128, 1024], BF16); b_tpB = Buf("tpB", True)
            pv = [PS(s1, "pv0", [128, 512], F32), PS(s1, "pv1", [128, 512], F32)]; b_pv = [Buf("pv0", True), Buf("pv1", True)]
            misc = PS(s1, "misc", [128, 512], F32); b_misc = Buf("misc", True)

            mset("dve", qlh[:], 0.0, [b_qlh])
            mset("dve", S[:], 0.0, [b_S])
            mset("dve", Sb[0][:], 0.0, [b_Sb[0]])
            mset("dve", cums[:], 0.0, [b_cums])
            mset("dve", cumsb[:], 0.0, [b_cumsb])
            for k in range(2):
                mset("dve", vx[k][:], 0.0, [b_vx[k]])
                mset("dve", vx[k][:, :, 64:65], 1.0, [b_vx[k]])
            def scale_w(g):
                c0, c1 = WG[g]
                tt("dve", Wp[:, :, c0:c1], Wp[:, :, c0:c1], bc(pp[:, Q_N1C:Q_N1C + 8], [128, 8, c1 - c0], 2), ALU.mult,
                   [b_Wg[g], b_pp], [b_Wg[g]])
            scale_w(0)
            scale_w(1)
            pending_w = [2, 3, 4, 5]
            for c in range(8):
                ts("dve", wr[:, c, :], wr[:, c, :], pp[:, Q_N2C + c:Q_N2C + c + 1], None, ALU.mult, None, [b_wr, b_pp], [b_wr])
            zt = hb[:]; b_zt = b_hb
            ZC = 1024
            mset("pool", zt, 0.0, [b_zt])
            sem_z = P.newsem()
            xz = xbuf_d.rearrange("(p r) d -> p (r d)", p=128)
            tot = (NSLOT // 128) * D
            zchunks = [(k0, min(tot, k0 + ZC)) for k0 in range(0, tot, ZC)]

            def zero_some(n):
                for _ in range(n):
                    if zchunks:
                        k0, k1 = zchunks.pop(0)
                        b_xbuf.last_w = dma("sp", xz[:, k0:k1], zt[:, 0:k1 - k0], sem_z, [b_zt], [Buf("ztmp")])

            mset("dve", cst[:, 0:1], math.pi / 2, [b_cst])
            mset("dve", cst[:, 1:2], EPS, [b_cst])
            dma("sp", m1[:], hlb_d, P.newsem(), [], [b_m1])
            hl = m1[:].rearrange("p (a c) -> p a c", a=2)
            tt("dve", lbt[:, 0, :], hl[:, 0, :], hl[:, 1, :], ALU.subtract, [b_m1], [b_lb])
            act(lbt[:, 0, :], lbt[:, 0, :], AF.Sigmoid, [b_lb], [b_lb])
            ts("dve", lbt[:, 1, :], lbt[:, 0, :], -1.0, 1.0, ALU.mult, ALU.add, [b_lb], [b_lb])
            act(esink[:], pp[:, Q_SINK:Q_SINK + 8], AF.Exp, [b_pp], [b_esink])
            b_tg = Buf("trigtmp")
            tA = xt[1][:, 0:2 * NA].rearrange("p (a n) -> p a n", a=2)
            tB = xt[2][:, 0:2 * NA].rearrange("p (a n) -> p a n", a=2)
            TR = [b_tg, b_xt[1], b_xt[2]]
            trg = {0: tA[:, 0, :], 1: tA[:, 1, :], 2: tB[:, 0, :], 3: tB[:, 1, :], 4: trig[:, 0, :], 5: trig[:, 1, :]}
            posf = trg[0][:, 0:NT + 1]
            cp("dve", posf, posi[:], [b_posi], TR)
            ang = trg[1].rearrange("p (i j) -> p i j", j=8)
            tt("dve", ang, bc(posf, [128, NT + 1, 8], 2), bc(invf, [128, NT + 1, 8], 1), ALU.mult, TR + [b_cf], TR)
            kf = trg[2]
            ts("dve", kf, trg[1], 1.0 / TWO_PI, None, ALU.mult, None, TR, TR)
            ki = trg[3].bitcast(I32)
            cp("dve", ki, kf, TR, TR)
            cp("dve", kf, ki, TR, TR)
            r_ = trg[1]
            stt("dve", r_, kf, -TWO_PI, r_, ALU.mult, ALU.add, TR, TR)
            s4 = trg[2]
            c4 = trg[3]
            act(s4, r_, AF.Sin, TR, TR, scale=0.25)
            act(c4, r_, AF.Sin, TR + [b_cst], TR, scale=0.25, bias=cst[:, 0:1])
            s2 = trg[0]
            c2 = trg[1]
            stt("dve", s2, s4, 2.0, c4, ALU.mult, ALU.mult, TR, TR)
            tt("dve", c2, s4, s4, ALU.mult, TR, TR)
            ts("dve", c2, c2, -2.0, 1.0, ALU.mult, ALU.add, TR, TR)
            stt("dve", trg[5], s2, 2.0, c2, ALU.mult, ALU.mult, TR, [b_trig])
            tt("dve", trg[4], s2, s2, ALU.mult, TR, [b_trig])
            ts("dve", trg[4], trg[4], -2.0, 1.0, ALU.mult, ALU.add, [b_trig], [b_trig])


            mmc = [0]

            def next_mm():
                k = mmc[0] % 2
                mmc[0] += 1
                return mmP[k], b_mm[k]

            def rstd_of(stt_, b_s, col, src_col, scale=1.0):
                act(stt_[:, col], stt_[:, src_col], AF.Ln, [b_s, b_cst], [b_s], bias=cst[:, 1:2], scale=scale)
                act(stt_[:, col], stt_[:, col], AF.Exp, [b_s], [b_s], scale=-0.5)

            def load_x(src, i, slot):
                dma("sp", xt[slot][:], src[i * 128:(i + 1) * 128, :], sem_x[slot], [], [b_xt[slot]])

            rstd1 = stF[:, 1:2]

            def front(slot):
                mset("dve", stF[:, 0:1], 0.0, [b_stF])
                act(junkF[:], xt[slot][:], AF.Square, [b_xt[slot]], [b_junkF, b_stF], scale=1.0 / 32, accum_out=stF[:, 0:1])
                rstd_of(stF, b_stF, slice(1, 2), slice(0, 1))
                cp("act", xb[:], xt[slot][:], [b_xt[slot]], [b_xb])
                for c in range(8):
                    tr(tp[:, c * 128:(c + 1) * 128], xb[:, c * 128:(c + 1) * 128], ident_b, [b_xb, b_cb], [b_tp])
                cp("dve", xT[:].rearrange("p c t -> p (c t)"), tp[:], [b_tp], [b_xT])

            def proj(c0, n):
                ps_, b_ps = next_mm()
                for c in range(8):
                    mm(ps_[:, 0:n], xT[:, c, :], Wp[:, c, c0:c0 + n], c == 0, c == 7, [b_xT, wbuf(c0)], [b_ps])
                return ps_, b_ps

            def qknorm_rope(src, b_src, nh, wtile, dst, b_dst, ti):
                n = nh * 64
                s3 = src.rearrange("p (h d) -> p h d", d=64)
                d3 = dst.rearrange("p (h d) -> p h d", d=64)
                w3 = wtile.rearrange("p (h d) -> p h d", d=64)
                tt("pool", dst, src, src, ALU.mult, [b_src], [b_dst])
                red("dve", stA[:, 8:8 + nh], d3, ALU.add, [b_dst], [b_stA])
                rstd_of(stA, b_stA, slice(16, 16 + nh), slice(8, 8 + nh), scale=1.0 / 64)
                tt("dve", s3, s3, bc(stA[:, 16:16 + nh], [128, nh, 64], 2), ALU.mult, [b_src, b_stA], [b_src])
                tt("dve", dst, src, wtile, ALU.mult, [b_src, b_pp], [b_dst])
                x1 = r16[:, 0, 0:nh, :]; x2 = r16[:, 1, 0:nh, :]
                tt("dve", x1, s3[:, :, 0:8], w3[:, :, 0:8], ALU.mult, [b_src, b_pp], [b_r16])
                tt("dve", x2, s3[:, :, 8:16], w3[:, :, 8:16], ALU.mult, [b_src, b_pp], [b_r16])
                cs = bc(cos_t[:, ti, :], [128, nh, 8], 1)
                sn = bc(sin_t[:, ti, :], [128, nh, 8], 1)
                a_ = r16[:, 2, 0:nh, :]; b_ = r16[:, 3, 0:nh, :]; c_ = r16[:, 4, 0:nh, :]; d_ = r16[:, 5, 0:nh, :]
                tt("dve", a_, x1, cs, ALU.mult, [b_r16, b_trig], [b_r16])
                tt("dve", b_, x2, sn, ALU.mult, [b_r16, b_trig], [b_r16])
                tt("dve", c_, x2, cs, ALU.mult, [b_r16, b_trig], [b_r16])
                tt("dve", d_, x1, sn, ALU.mult, [b_r16, b_trig], [b_r16])
                tt("dve", d3[:, :, 0:8], a_, b_, ALU.subtract, [b_r16], [b_dst])
                tt("dve", d3[:, :, 8:16], c_, d_, ALU.add, [b_r16], [b_dst])

            def kv_proj(slot):
                ps_, b_ps = proj(C_KV, 256)
                act(kv[:], ps_[:, 0:128], AF.Copy, [b_ps, b_stF], [b_kv], scale=rstd1)
                act(vx[slot][:, :, 0:64], ps_[:, 128:256].rearrange("p (m d) -> p m d", d=64), AF.Copy, [b_ps, b_stF], [b_vx[slot]], scale=rstd1)

            def k_finish(ti, slot):
                qknorm_rope(kv[:], b_kv, 2, kw, kr[:], b_kr, ti)
                tr(tp[:, 0:128], kr[:], ident_b, [b_kr, b_cb], [b_tp])
                cp("dve", kTs[slot][:], tp[:, 0:128], [b_tp], [b_kT[slot]])

            HS = [(hA, b_hA, hk, b_hk, eb, b_eb, iv, b_iv, kt, b_kt),
                  (qa, b_qa, qh, b_qh, gs, b_gs, qr, b_qr, attn, b_attn)]

            def hgrn_proj(hs=0):
                hA, b_hA, hk, b_hk, eb, b_eb, iv, b_iv, kt, b_kt = HS[hs]
                ps_, b_ps = proj(C_FH, 512)
                act(hA[:], ps_[:], AF.Sigmoid, [b_ps, b_stF], [b_hA], scale=rstd1)
                ps2, b_ps2 = proj(C_IH, 512)
                act(iv[:], ps2[:], AF.Copy, [b_ps2, b_stF], [b_iv], scale=rstd1)

            def hgrn_chain(hs=0):
                hA, b_hA, hk, b_hk, eb, b_eb, iv, b_iv, kt, b_kt = HS[hs]
                tt("pool", hA[:], hA[:], lbt[:, 1, :], ALU.mult, [b_hA, b_lb], [b_hA])
                tt("pool", hA[:], hA[:], lbt[:, 0, :], ALU.add, [b_hA, b_lb], [b_hA])
                yield
                ts("dve", hk[:], hA[:], -1.0, 1.0, ALU.mult, ALU.add, [b_hA], [b_hk])
                act(hA[:], hA[:], AF.Ln, [b_hA], [b_hA])
                yield
                mm(pv[1][:], tri_f, hA[:], True, True, [b_cf, b_hA], [b_pv[1]])
                act(eb[:], pv[1][:], AF.Exp, [b_pv[1]], [b_eb])
                act(hA[:], pv[1][:], AF.Exp, [b_pv[1]], [b_hA], scale=-1.0)
                yield
                tt("dve", kt[:], hk[:], hA[:], ALU.mult, [b_hk, b_hA], [b_kt])
                for hh in range(4):
                    mm(misc[:, hh * 2:hh * 2 + 2], eb[:, hh * 128:(hh + 1) * 128], sel2, True, True, [b_eb, b_cf], [b_misc])
                cp("dve", Dsb[:].rearrange("p h c -> p (h c)"), misc[:, 0:8], [b_misc], [b_Dsb])
                yield

            def state_update(c, dst, hs=0):
                hA, b_hA, hk, b_hk, eb, b_eb, iv, b_iv, kt, b_kt = HS[hs]
                for hh in range(4):
                    mm(misc[:, hh * 128:(hh + 1) * 128], kt[c * 64:(c + 1) * 64, hh * 128:(hh + 1) * 128],
                       iv[c * 64:(c + 1) * 64, hh * 128:(hh + 1) * 128], True, True, [b_kt, b_iv], [b_misc])
                Sf = S[:].rearrange("p h d -> p (h d)")
                tt("dve", Sf, Sf, misc[:], ALU.add, [b_S, b_misc], [b_S])
                tt("dve", S[:], S[:], bc(Dsb[:, :, c], [128, 4, 128], 2), ALU.mult, [b_S, b_Dsb], [b_S])
                cp("act", Sb[dst][:], S[:], [b_S], [b_Sb[dst]])

            flags = set()

            def run_threads(threads):
                active = [[g, 0.0, None] for g in threads]
                while active:
                    cand = [a for a in active if a[2] is None or a[2] in flags]
                    assert cand, "scheduler deadlock"
                    a = cand[0]
                    active.remove(a)
                    active.append(a)
                    a[2] = None
                    P.step_max = 0.0
                    try:
                        r = next(a[0])
                    except StopIteration:
                        active.remove(a)
                        continue
                    if P.step_max > 0.0:
                        a[1] = P.step_max
                    if r is None:
                        continue
                    if isinstance(r, tuple):
                        if r[0] == "need":
                            a[2] = r[1]
                        else:
                            flags.add(r[1])
                    else:
                        active.append([r, a[1], None])

            load_x(x_pre if NPRE > 0 else x_own, 0, 0)
            def thr_PXF(j):
                front(j % 3)
                yield
                hgrn_proj(j % 2)
                yield
                if j == NPRE - 1:
                    kv_proj(1)
                    yield
                    k_finish(NT, 1)

            def thr_PH(j):
                for _ in hgrn_chain(j % 2):
                    yield
                state_update(0, 1, j % 2)
                yield
                state_update(1, 0, j % 2)

            for j in range(NPRE + 1):
                if j < NPRE:
                    if j + 1 < NPRE:
                        load_x(x_pre, j + 1, (j + 1) % 3)
                    else:
                        load_x(x_own, 0, (j + 1) % 3)
                thr = []
                if j > 0:
                    thr.append(thr_PH(j - 1))
                if j < NPRE:
                    thr.append(thr_PXF(j))
                run_threads(thr)
                if pending_w and j >= 3 and j % 2 == 1:
                    scale_w(pending_w.pop(0))
                if j >= 1:
                    zero_some(4)
            while pending_w:
                scale_w(pending_w.pop(0))
            zero_some(len(zchunks))

            def thr_A(i):
                ks = i % 2
                k_finish(i, ks)
                yield
                qknorm_rope(qa[:], b_qa, 8, qw, qr[:], b_qr, i)
                yield
                for j in range(4):
                    tr(tp[:, j * 128:(j + 1) * 128], qr[:, j * 128:(j + 1) * 128], ident_b, [b_qr, b_cb], [b_tp])
                cp("dve", qT[:].rearrange("p j t -> p (j t)"), tp[:, 0:512], [b_tp], [b_qT])
                yield
                for m in range(2):
                    for kb in range(2):
                        ksl = (1 - ks) if kb == 0 else ks
                        msk = (mask_halo if i == 0 else mask_prev) if kb == 0 else mask_cur
                        mm(sc[0][:], ident_b, msk, True, False, [b_cb], [b_sc[0]])
                        mm(sc[0][:], kTs[ksl][m * 64:(m + 1) * 64, :], qT[m * 64:(m + 1) * 64, :, :].rearrange("p j t -> p (j t)"),
                           False, True, [b_kT[ksl], b_qT], [b_sc[0]])
                        act(pTb[kb][:], sc[0][:], AF.Exp, [b_sc[0]], [b_pTb[kb]], scale=0.125)
                        yield
                    for j in range(4):
                        for kb in range(2):
                            ksl = (1 - ks) if kb == 0 else ks
                            mm(pv[0][:, j * 80:(j + 1) * 80], pTb[kb][:, j * 128:(j + 1) * 128], vx[ksl][:, m, :],
                               kb == 0, kb == 1, [b_pTb[kb], b_vx[ksl]], [b_pv[0]])
                    pv3 = pv[0][:, 0:320].rearrange("p (j d) -> p j d", d=80)
                    den = stA[:, 24 + m * 4:28 + m * 4]
                    tt("dve", den, pv3[:, :, 64], esink[:, m * 4:(m + 1) * 4], ALU.add, [b_pv[0], b_esink], [b_stA])
                    P.op("dve", lambda e, den=den: e.reciprocal(out=den, in_=den), [b_stA], [b_stA])
                    tt("dve", attn[:, m * 256:(m + 1) * 256].rearrange("p (j d) -> p j d", d=64), pv3[:, :, 0:64],
                       bc(den, [128, 4, 64], 2), ALU.mult, [b_pv[0], b_stA], [b_attn])
                    yield
                if i > 0:
                    yield ("need", ("aT", i - 1))
                for j in range(4):
                    tr(tp[:, j * 128:(j + 1) * 128], attn[:, j * 128:(j + 1) * 128], ident_b, [b_attn, b_cb], [b_tp])
                cp("dve", aT[:].rearrange("p j t -> p (j t)"), tp[:, 0:512], [b_tp], [b_aT])

            def thr_H(i):
                for _ in hgrn_chain():
                    yield
                tt("dve", qt[:], qh[:], eb[:], ALU.mult, [b_qh, b_eb], [b_qt])
                tt("pool", gs[:], gs[:], hw, ALU.mult, [b_gs, b_pp], [b_gs])
                yield
                for hh in range(4):
                    tr(tpB[:, hh * 128:(hh + 1) * 128], qt[:, hh * 128:(hh + 1) * 128], ident_b, [b_qt, b_cb], [b_tpB])
                    tr(tpB[:, 512 + hh * 128:512 + (hh + 1) * 128], kt[:, hh * 128:(hh + 1) * 128], ident_b, [b_kt, b_cb], [b_tpB])
                tq = tpB[:, 0:512].rearrange("p (h t) -> p h t", t=128)
                cp("dve", qlh[:, :, 0, 0:64], tq[:, :, 0:64], [b_tpB], [b_qlh])
                cp("dve", qlh[:, :, 1, 64:128], tq[:, :, 64:128], [b_tpB], [b_qlh])
                cp("act", ktT[:].rearrange("p h t -> p (h t)"), tpB[:, 512:1024], [b_tpB], [b_ktT])
                yield
                state_update(0, 1)
                yield
                for hh in range(4):
                    a_ = hh % 2
                    sca = misc[:, 128 + a_ * 128:256 + a_ * 128]
                    mm(sca, ktT[:, hh, :], qlh[:, hh, 0, :], True, False, [b_ktT, b_qlh], [b_misc])
                    mm(sca, ktT[:, hh, :], qlh[:, hh, 1, :], False, True, [b_ktT, b_qlh], [b_misc])
                    tt("dve", Am[a_][:], sca, tri_f, ALU.mult, [b_misc, b_cf], [b_Am[a_]])
                    o_ = pv[1][:, hh * 128:(hh + 1) * 128]
                    mm(o_, Am[a_][:], iv[:, hh * 128:(hh + 1) * 128], True, False, [b_Am[a_], b_iv], [b_pv[1]])
                    mm(o_, qlh[:, hh, 0, :], Sb[0][:, hh, :], False, False, [b_qlh, b_Sb[0]], [b_pv[1]])
                    mm(o_, qlh[:, hh, 1, :], Sb[1][:, hh, :], False, True, [b_qlh, b_Sb[1]], [b_pv[1]])
                    yield
                state_update(1, 0)
                yield
                o3 = pv[1][:].rearrange("p (h d) -> p h d", d=128)
                act(hk[:], pv[1][:], AF.Square, [b_pv[1]], [b_hk])
                red("dve", stH[:, 0:4], hk[:].rearrange("p (h d) -> p h d", d=128), ALU.add, [b_hk], [b_stH])
                rstd_of(stH, b_stH, slice(4, 8), slice(0, 4), scale=1.0 / 128)
                tt("dve", qh[:].rearrange("p (h d) -> p h d", d=128), o3, bc(stH[:, 4:8], [128, 4, 128], 2), ALU.mult,
                   [b_pv[1], b_stH], [b_qh])
                yield
                tt("dve", hg[:], qh[:], gs[:], ALU.mult, [b_qh, b_gs], [b_hg])
                if i > 0:
                    yield ("need", ("hgT", i - 1))
                for j in range(4):
                    tr(tpB[:, j * 128:(j + 1) * 128], hg[:, j * 128:(j + 1) * 128], ident_b, [b_hg, b_cb], [b_tpB])
                cp("dve", hgT[:].rearrange("p j t -> p (j t)"), tpB[:, 0:512], [b_tpB], [b_hgT])

            def thr_XF(i, slot):
                front(slot)
                yield
                ps_, b_ps = proj(C_Q, 512)
                act(qa[:], ps_[:], AF.Copy, [b_ps, b_stF], [b_qa], scale=rstd1)
                kv_proj(i % 2)
                yield thr_A(i)
                hgrn_proj()
                yield
                ps_, b_ps = proj(C_QH, 512)
                act(qh[:], ps_[:], AF.Silu, [b_ps, b_stF], [b_qh], scale=rstd1)
                ps_, b_ps = proj(C_GH, 512)
                act(gs[:], ps_[:], AF.Silu, [b_ps, b_stF], [b_gs], scale=rstd1)
                yield thr_H(i)
                if i > 0:
                    yield ("need", ("hgT", i - 1))
                for half in range(2):
                    ps_, b_ps = proj(C_ZA + half * 512, 512)
                    act(sza[:, half * 512:(half + 1) * 512], ps_[:], AF.Sigmoid, [b_ps, b_stF], [b_sza], scale=rstd1)
                    yield
                for half in range(2):
                    ps_, b_ps = proj(C_ZB + half * 512, 512)
                    act(szb[:, half * 512:(half + 1) * 512], ps_[:], AF.Sigmoid, [b_ps, b_stF], [b_szb], scale=rstd1)
                    yield

            def thr_Y(i, slot):
                for half in range(2):
                    hs = slice(half * 512, (half + 1) * 512)
                    for c in range(4):
                        mm(mmP[half][:], aT[:, c, :], wba[:, c, hs], c == 0, c == 3, [b_aT, b_wba], [b_mm[half]])
                    tt("dve", m1[:, hs], mmP[half][:], sza[:, hs], ALU.mult, [b_mm[half], b_sza], [b_m1])
                    yield
                yield ("set", ("aT", i))
                for half in range(2):
                    hs = slice(half * 512, (half + 1) * 512)
                    for c in range(4):
                        mm(mmP[half][:], hgT[:, c, :], wbh[:, c, hs], c == 0, c == 3, [b_hgT, b_wbh], [b_mm[half]])
                    tt("dve", junkY[:, hs], mmP[half][:], szb[:, hs], ALU.mult, [b_mm[half], b_szb], [b_junkY])
                    tt("pool", mixed[:, hs], junkY[:, hs], m1[:, hs], ALU.add, [b_junkY, b_m1], [b_mixed])
                    yield
                yield ("set", ("hgT", i))
                for c in range(8):
                    tr(tpB[:, c * 128:(c + 1) * 128], mixed[:, c * 128:(c + 1) * 128], ident_b, [b_mixed, b_cb], [b_tpB])
                cp("dve", mT[:].rearrange("p c t -> p (c t)"), tpB[:], [b_tpB], [b_mT])
                yield
                for half in range(2):
                    hs = slice(half * 512, (half + 1) * 512)
                    for c in range(8):
                        mm(mmP[half][:], mT[:, c, :], wout[:, c, hs], c == 0, c == 7, [b_mT, b_wout], [b_mm[half]])
                    tt("dve", xt[slot][:, hs], xt[slot][:, hs], mmP[half][:], ALU.add, [b_xt[slot], b_mm[half]], [b_xt[slot]])
                    yield
                dma("sp", out_d[i * 128:(i + 1) * 128, :], xt[slot][:], sem_x1, [b_xt[slot]], [b_out])
                mset("dve", stY[:, 0:1], 0.0, [b_stY])
                act(junkY[:], xt[slot][:], AF.Square, [b_xt[slot]], [b_junkY, b_stY], scale=1.0 / 32, accum_out=stY[:, 0:1])
                rstd_of(stY, b_stY, slice(1, 2), slice(0, 1))
                rstd2 = stY[:, 1:2]
                stt("dve", hb[:], xt[slot][:], rstd2, n2w, ALU.mult, ALU.mult, [b_xt[slot], b_stY, b_pp], [b_hb])
                yield
                x1T = m1[:].rearrange("p (c t) -> p c t", t=128)
                for g4 in range(2):
                    for c in range(4):
                        tr(mmP[g4][:, c * 128:(c + 1) * 128], xt[slot][:, (g4 * 4 + c) * 128:(g4 * 4 + c + 1) * 128], ident_f,
                           [b_xt[slot], b_cf], [b_mm[g4]])
                    cp("act" if g4 == 0 else "dve", m1[:, g4 * 512:(g4 + 1) * 512], mmP[g4][:], [b_mm[g4]], [b_m1])
                    yield
                for c in range(8):
                    mm(mmP[0][:, 0:36], x1T[:, c, :], wr[:, c, :], c == 0, c == 7, [b_m1, b_wr], [b_mm[0]])
                L = rt[:, 0:36]
                stt("dve", L, mmP[0][:, 0:36], rstd2, brt, ALU.mult, ALU.add, [b_mm[0], b_stY, b_pp], [b_rt])
                yield
                R = [b_rt]
                gmax = rt[:, 36:37]; ngmax = rt[:, 37:38]; gsum = rt[:, 38:39]; pg = rt[:, 39:40]
                red("dve", gmax, L[:, 0:4], ALU.max, R, R)
                ts("dve", ngmax, gmax, -1.0, None, ALU.mult, None, R, R)
                mset("dve", gsum, 0.0, R)
                act(rt[:, 40:44], L[:, 0:4], AF.Exp, R, R, bias=ngmax, accum_out=gsum)
                P.op("dve", lambda e, pg=pg, gsum=gsum: e.reciprocal(out=pg, in_=gsum), R, R)
                gm = rt[:, 44:48]
                ts("dve", gm, L[:, 0:4], gmax, None, ALU.is_ge, None, R, R)
                ts("dve", gm, gm, 1e30, -1e30, ALU.mult, ALU.add, R, R)
                yield
                lem = rt[:, 48:80]
                tt("dve", lem.rearrange("p (g j) -> p g j", j=8), L[:, 4:36].rearrange("p (g j) -> p g j", j=8),
                   bc(gm, [128, 4, 8], 2), ALU.add, R, R)
                top8 = rt[:, 80:88]
                P.op("dve", lambda e, top8=top8, lem=lem: e.max(out=top8, in_=lem), R, R)
                sel = rt[:, 88:120]; is1 = rt[:, 120:152]; isB = rt[:, 152:184]
                ts("dve", sel, lem, top8[:, 1:2], None, ALU.is_ge, None, R, R)
                ts("dve", is1, lem, top8[:, 0:1], None, ALU.is_ge, None, R, R)
                tt("dve", isB, sel, is1, ALU.subtract, R, R)
                yield
                dd = rt[:, 184:185]; s1_ = rt[:, 185:186]
                tt("dve", dd, top8[:, 0:1], top8[:, 1:2], ALU.subtract, R, R)
                act(s1_, dd, AF.Sigmoid, R, R)
                tt("dve", gate[:, 0, i:i + 1], pg, s1_, ALU.mult, R, [b_gate])
                tt("dve", gate[:, 1, i:i + 1], pg, gate[:, 0, i:i + 1], ALU.subtract, R + [b_gate], [b_gate])
                cp("dve", selb[:], sel, R, [b_selb])
                yield
                mm(mmP[1][:, 0:32], stri_b, selb[:], True, False, [b_cb, b_selb], [b_mm[1]])
                mm(mmP[1][:, 0:32], ones_b, cumsb[:], False, True, [b_cb, b_cumsb], [b_mm[1]])
                tt("dve", cums[:], cums[:], sel, ALU.add, R + [b_cums], [b_cums])
                cp("dve", cumsb[:], cums[:], [b_cums], [b_cumsb])
                posc = rt[:, 192:224]
                ts("dve", posc, mmP[1][:, 0:32], float(CAP - 1), None, ALU.min, None, [b_mm[1]], R)
                yield
                tt("dve", posc, posc, iotae, ALU.add, R + [b_cf], R)
                tmpm = rt[:, 224:256]
                dA = rt[:, 186:187]; dB = rt[:, 187:188]
                tt("dve", tmpm, posc, is1, ALU.mult, R, R)
                red("dve", dA, tmpm, ALU.add, R, R)
                tt("dve", tmpm, posc, isB, ALU.mult, R, R)
                red("dve", dB, tmpm, ALU.add, R, R)
                cp("dve", dest[:, 0, i:i + 1], dA, R, [b_dest])
                cp("dve", dest[:, 1, i:i + 1], dB, R, [b_dest])
                yield
                for k in range(2):
                    off = dest[:, k, i:i + 1]
                    P.dma("pool", lambda e, off=off: e.indirect_dma_start(
                        out=xbuf_d, out_offset=bass.IndirectOffsetOnAxis(ap=off, axis=0), in_=hb[:, :], in_offset=None),
                        sem_sc, [b_hb, b_dest, b_xbuf], [Buf("scat")])

            sem_cv = P.newsem()
            cv_next = [0]

            def convert_experts(upto):
                while cv_next[0] < min(upto, NEX):
                    e = cv_next[0]
                    cv_next[0] += 1
                    for src, dst in ((wg_d, wgb_d), (wu_d, wub_d), (wd_d, wdb_d)):
                        s2_ = src[e].rearrange("a b -> (a b)").rearrange("(p f) -> p f", p=128)
                        d2_ = dst[e].rearrange("a b -> (a b)").rearrange("(p f) -> p f", p=128)
                        dma("pool", d2_, s2_, sem_cv, [], [Buf("cv")])

            for i in range(NT):
                gi = NPRE + i
                if i + 1 < NT:
                    load_x(x_own, i + 1, (gi + 1) % 3)
                convert_experts(((i + 1) * 32 + NT - 1) // NT)
                thr = []
                if i > 0:
                    thr.append(thr_Y(i - 1, (gi - 1) % 3))
                thr.append(thr_XF(i, gi % 3))
                run_threads(thr)
            run_threads([thr_Y(NT - 1, (NPRE + NT - 1) % 3)])
            P.barrier()
            P.emit_block()
        if mode in (1, 2, 3, 4):
            return nc

        with ExitStack() as s2:
            NWB = 2
            wg = [SB(s2, "wg%d" % k, [128, 8, 512], BF16) for k in range(NWB)]
            wu = [SB(s2, "wu%d" % k, [128, 8, 512], BF16) for k in range(NWB)]
            wd = [SB(s2, "wd%d" % k, [128, 4, D], BF16) for k in range(NWB)]
            b_w = [Buf("w%d" % k) for k in range(NWB)]
            sem_e = [P.newsem() for _ in range(NWB)]
            xg = [SB(s2, "xg%d" % k, [128, NB, D], BF16) for k in range(2)]; b_xg = [Buf("xg0"), Buf("xg1")]
            sem_xg = [P.newsem(), P.newsem()]
            xgT = SB(s2, "xgT", [128, 8, CAP], BF16); b_xgT = Buf("xgT")
            sg = [SB(s2, "sg%d" % k, [128, CAP], F32) for k in range(2)]; b_sg = [Buf("sg0"), Buf("sg1")]
            hTs = [SB(s2, "hT%d" % k, [128, 4, CAP], BF16) for k in range(2)]; b_hTs = [Buf("hT0"), Buf("hT1")]
            NYB = 4
            ysb = [SB(s2, "ysb%d" % k, [128, D], F32) for k in range(NYB)]; b_ysb = [Buf("ysb%d" % k) for k in range(NYB)]
            sem_y = [P.newsem() for _ in range(NYB)]
            NP3 = 8
            x1t = [SB(s2, "x1t%d" % k, [128, D], F32) for k in range(NP3)]; b_x1t = [Buf("x1t%d" % k) for k in range(NP3)]
            yA = [SB(s2, "yA%d" % k, [128, D], F32) for k in range(NP3)]; b_yA = [Buf("yA%d" % k) for k in range(NP3)]
            yB = [SB(s2, "yB%d" % k, [128, D], F32) for k in range(NP3)]; b_yB = [Buf("yB%d" % k) for k in range(NP3)]
            sem_l = [P.newsem() for _ in range(NP3)]
            sem_ga = [P.newsem() for _ in range(NP3)]
            sem_gb = [P.newsem() for _ in range(NP3)]
            sem_o = [P.newsem() for _ in range(NP3)]
            tp2s = [PS(s2, "tp2%d" % k, [128, 1024], BF16) for k in range(2)]; b_tp2s = [Buf("tp20", True), Buf("tp21", True)]
            tcount = [0]
            pg_ = [PS(s2, "pg%d" % k, [128, 512], F32) for k in range(2)]; b_pg = [Buf("pg0", True), Buf("pg1", True)]
            pu_ = [PS(s2, "pu%d" % k, [128, 512], F32) for k in range(2)]; b_pu = [Buf("pu0", True), Buf("pu1", True)]
            py_ = [PS(s2, "py%d" % k, [128, 512], F32) for k in range(2)]; b_py = [Buf("py0", True), Buf("py1", True)]

            def load_w(e):
                k = e % NWB
                hws = []
                dma("pool", wg[k][:], wgb_d[e].rearrange("(c p) n -> p c n", p=128), sem_e[k], [], [b_w[k]])
                P.dma("pool", lambda en, k=k, e=e: en.dma_start(out=wu[k][:], in_=wub_d[e].rearrange("(c p) n -> p c n", p=128)), sem_e[k], [], [Buf("t")])
                h = P.dma("pool", lambda en, k=k, e=e: en.dma_start(out=wd[k][:], in_=wdb_d[e].rearrange("(c p) n -> p c n", p=128)), sem_e[k], [], [Buf("t")])
                b_w[k].last_w = h

            def load_xg(e):
                k = e % 2
                dma("sp", xg[k][:], xbuf_d[e * CAP:(e + 1) * CAP, :].rearrange("(b p) d -> p b d", p=128), sem_xg[k], [b_xbuf], [b_xg[k]])

            load_w(0)
            load_xg(0)
            ycount = 0
            for e in range(32):
                k = e % NWB
                kx = e % 2
                hT = hTs[e % 2]; b_hT = b_hTs[e % 2]
                if e + 1 < 32:
                    load_w(e + 1)
                    load_xg(e + 1)
                for sb_ in range(NB):
                    tp2 = tp2s[tcount[0] % 2]; b_tp2 = b_tp2s[tcount[0] % 2]
                    tcount[0] += 1
                    for c in range(8):
                        tr(tp2[:, c * 128:(c + 1) * 128], xg[kx][:, sb_, c * 128:(c + 1) * 128], ident_b, [b_xg[kx], b_cb], [b_tp2])
                    cp("dve" if sb_ % 2 == 0 else "act", xgT[:, :, sb_ * 128:(sb_ + 1) * 128], tp2[:].rearrange("p (c t) -> p c t", t=128), [b_tp2], [b_xgT])
                for fc in range(4):
                    a_ = fc % 2
                    for c in range(8):
                        mm(pg_[a_][:, 0:CAP], wg[k][:, c, fc * 128:(fc + 1) * 128], xgT[:, c, :], c == 0, c == 7, [b_w[k], b_xgT], [b_pg[a_]])
                    for c in range(8):
                        mm(pu_[a_][:, 0:CAP], wu[k][:, c, fc * 128:(fc + 1) * 128], xgT[:, c, :], c == 0, c == 7, [b_w[k], b_xgT], [b_pu[a_]])
                    act(sg[a_][:], pg_[a_][:, 0:CAP], AF.Silu, [b_pg[a_]], [b_sg[a_]])
                    tt("dve", hT[:, fc, :], sg[a_][:], pu_[a_][:, 0:CAP], ALU.mult, [b_sg[a_], b_pu[a_]], [b_hT])
                for sb_ in range(NB):
                    ky = ycount % NYB
                    ycount += 1
                    for nh in range(2):
                        for fc in range(4):
                            mm(py_[nh][:], hT[:, fc, sb_ * 128:(sb_ + 1) * 128], wd[k][:, fc, nh * 512:(nh + 1) * 512], fc == 0, fc == 3,
                               [b_hT, b_w[k]], [b_py[nh]])
                        cp("act" if nh == 0 else "dve", ysb[ky][:, nh * 512:(nh + 1) * 512], py_[nh][:], [b_py[nh]], [b_ysb[ky]])
                    r0 = e * CAP + sb_ * 128
                    dma("sp", ybuf_d[r0:r0 + 128, :], ysb[ky][:], sem_y[ky], [b_ysb[ky]], [Buf("yst%d" % ky)])
            P.barrier()

            def p3_fetch(i):
                k = i % NP3
                dma("sp", x1t[k][:], out_d[i * 128:(i + 1) * 128, :], sem_l[k], [b_out], [b_x1t[k]])
                offA = dest[:, 0, i:i + 1]
                offB = dest[:, 1, i:i + 1]
                P.dma("pool", lambda en, off=offA, dst=yA[k]: en.indirect_dma_start(
                    out=dst[:, :], out_offset=None, in_=ybuf_d, in_offset=bass.IndirectOffsetOnAxis(ap=off, axis=0)),
                    sem_ga[k], [b_dest], [b_yA[k]])
                P.dma("pool", lambda en, off=offB, dst=yB[k]: en.indirect_dma_start(
                    out=dst[:, :], out_offset=None, in_=ybuf_d, in_offset=bass.IndirectOffsetOnAxis(ap=off, axis=0)),
                    sem_gb[k], [b_dest], [b_yB[k]])

            for i in range(min(NP3 - 1, NT)):
                p3_fetch(i)
            for i in range(NT):
                k = i % NP3
                if i + NP3 - 1 < NT:
                    p3_fetch(i + NP3 - 1)
                stt("dve", x1t[k][:], yA[k][:], gate[:, 0, i:i + 1], x1t[k][:], ALU.mult, ALU.add, [b_yA[k], b_gate, b_x1t[k]], [b_x1t[k]])
                stt("dve", x1t[k][:], yB[k][:], gate[:, 1, i:i + 1], x1t[k][:], ALU.mult, ALU.add, [b_yB[k], b_gate, b_x1t[k]], [b_x1t[k]])
                dma("act", out_d[i * 128:(i + 1) * 128, :], x1t[k][:], sem_o[k], [b_x1t[k]], [Buf("ost%d" % k)])
            P.barrier()
            P.emit_block()
    return nc


def _consts(CAP):
    cf = np.zeros((128, K_END), np.float32)
    cf[:, K_ID:K_ID + 128] = np.eye(128, dtype=np.float32)
    s = np.arange(128)[:, None]
    t = np.arange(128)[None, :]
    cf[:, K_TRI:K_TRI + 128] = ((s <= t) & ((s // 64) == (t // 64))).astype(np.float32)
    cf[63, K_SEL2] = 1.0
    cf[127, K_SEL2 + 1] = 1.0
    invf = (500000.0 ** (-np.arange(0, 16, 2, dtype=np.float32) / 16)).astype(np.float32)
    cf[:, K_INVF:K_INVF + 8] = invf[None, :]
    cf[:, K_IOTA:K_IOTA + 32] = (np.arange(32, dtype=np.float32) * CAP)[None, :]
    cb = np.zeros((128, B_END), np.float32)
    cb[:, B_ID:B_ID + 128] = np.eye(128, dtype=np.float32)
    cb[:, B_STRI:B_STRI + 128] = (s < t).astype(np.float32)
    cb[:, B_ONES:B_ONES + 128] = 1.0
    mcur = np.where(s <= t, 0.0, NEG).astype(np.float32)
    mprev = np.where(s > t, 0.0, NEG).astype(np.float32)
    cb[:, B_MCUR:B_MCUR + 512] = np.tile(mcur, (1, 4))
    cb[:, B_MPREV:B_MPREV + 512] = np.tile(mprev, (1, 4))
    return cf, cb, mprev


def make_in_maps(inp, NT, CAP, n_cores=8):
    x = np.asarray(inp["x"], np.float32)
    B, S, _ = x.shape
    T = NT * 128
    assert S == 2 * T and B * 2 == n_cores
    pos = np.asarray(inp["positions"]).astype(np.int32)
    cf, cb0, mprev = _consts(CAP)
    w_in = np.asarray(inp["w_in"], np.float32)[0]
    qperm = np.concatenate([np.arange(64) + (g * 4 + j) * 64 for j in range(4) for g in range(2)])
    w_in_p = np.ascontiguousarray(np.concatenate([w_in[:, :512][:, qperm], w_in[:, 512:]], axis=1))
    f = lambda k: np.ascontiguousarray(np.asarray(inp[k], np.float32)[0])
    pp = np.zeros((128, Q_END), np.float32)
    pp[:, Q_N1C:Q_N1C + 8] = f("norm1_w").reshape(8, 128).T
    pp[:, Q_N2C:Q_N2C + 8] = f("norm2_w").reshape(8, 128).T
    pp[:, Q_N2:Q_N2 + 1024] = f("norm2_w")[None, :]
    pp[:, Q_QW:Q_QW + 512] = np.tile(f("q_norm_w"), 8)[None, :]
    pp[:, Q_KW:Q_KW + 128] = np.tile(f("k_norm_w"), 2)[None, :]
    pp[:, Q_HW:Q_HW + 512] = np.tile(f("hgrn_norm_w"), 4)[None, :]
    hlb = np.asarray(inp["hgrn_lower_bounds"], np.float32)
    hlbt = np.ascontiguousarray(np.tile(hlb.reshape(1, 1024), (128, 1)))
    pp[:, Q_SINK:Q_SINK + 8] = f("attn_sinks")[None, :]
    pp[:, Q_BRT:Q_BRT + 4] = f("b_router_group")[None, :]
    pp[:, Q_BRT + 4:Q_BRT + 36] = f("b_router_expert")[None, :]
    w_r = np.ascontiguousarray(np.concatenate([f("w_router_group"), f("w_router_expert")], axis=1))
    shared = {
        "cf32": cf, "pp32": pp, "hlb": hlbt, "w_in": w_in_p, "w_ba": f("w_branch_attn"), "w_bh": f("w_branch_hgrn"),
        "w_out": f("w_out"), "w_r": w_r, "w_g": f("w_gate_experts"), "w_u": f("w_up_experts"), "w_d": f("w_down_experts"),
    }
    maps = []
    for c in range(n_cores):
        b, half = c // 2, c % 2
        cb = cb0.copy()
        if half == 1:
            cb[:, B_MHALO:B_MHALO + 512] = np.tile(mprev, (1, 4))
            x_pre = np.ascontiguousarray(x[b, 0:T])
            halo_pos = pos[b, T - 128:T]
        else:
            cb[:, B_MHALO:B_MHALO + 512] = NEG
            x_pre = np.zeros((T, D), np.float32)
            halo_pos = np.zeros(128, np.int32)
        own = slice(half * T, (half + 1) * T)
        pt = np.concatenate([pos[b, own].reshape(NT, 128).T, halo_pos[:, None]], axis=1).astype(np.int32)
        m = dict(shared)
        m.update({"x_own": np.ascontiguousarray(x[b, own]), "x_pre": x_pre, "pos": np.ascontiguousarray(pt), "cb16": cb})
        maps.append(m)
    return maps


_NC_CACHE = {}


def run(inp, NT, CAP, mode=0, raw=False):
    key = (NT, CAP, mode)
    if key not in _NC_CACHE:
        _NC_CACHE[key] = build(NT, CAP, mode=mode)
    nc = _NC_CACHE[key]
    maps = make_in_maps(inp, NT, CAP)
    if mode != 0:
        for m in maps:
            for k in ("w_g", "w_u", "w_d"):
                m[k] = np.ascontiguousarray(m[k][:1])
    res = run_bass_kernel_spmd(nc, maps, core_ids=list(range(8)))
    if raw:
        return res.results
    x = np.asarray(inp["x"])
    B, S, _ = x.shape
    T = NT * 128
    out = np.empty((B, S, D), np.float32)
    for c in range(8):
        b, half = c // 2, c % 2
        out[b, half * T:(half + 1) * T] = res.results[c]["out"]
    return out


def kernel(**inputs):
    return run(inputs, 32, 384)
```
